# Optimizing a Trainium2 kernel written in Bass

```python
import jax, jax.numpy as jnp
from jax import lax
import numpy as np

D_MODEL = 2048
BATCH = 2
SEQ = 4096
DEPTH = 1

CHUNK = 64
EPS = 1e-6

GM_GROUPS = 8
GM_DIM = 1024
GM_GROUP_DIM = GM_DIM // GM_GROUPS
GM_BLOCK = 128

RET_HEADS = 8
RET_DK = 128
RET_DV = 256
RET_QK = RET_HEADS * RET_DK
RET_V = RET_HEADS * RET_DV
ROPE_BASE = 10000.0

IN_WIDTH = 2 * GM_DIM + 2 * RET_QK + 2 * RET_V
N_BRANCH = 2
N_MOD = 6

PEER_HEADS = 8
PEER_NKEYS = 128
PEER_NEXPERTS = PEER_NKEYS * PEER_NKEYS
PEER_DKEY = 256
PEER_DHALF = PEER_DKEY // 2
PEER_TOPK = 16
PEER_TOKEN_BLOCK = 128

kernel_name = "chunk_causal_gmlp_retention_peer_hybrid"


def rmsnorm(x, g):
    xf = x.astype(jnp.float32)
    y = xf * lax.rsqrt(jnp.mean(xf * xf, axis=-1, keepdims=True) + EPS)
    return (y * g.astype(jnp.float32)).astype(x.dtype)


def modulate(h, shift, scale):
    return h * (1.0 + scale[:, None, :]) + shift[:, None, :]


def rotary(t, positions):
    half = t.shape[-1] // 2
    freqs = ROPE_BASE ** (-jnp.arange(half, dtype=jnp.float32) / half)
    ang = positions.astype(jnp.float32)[:, :, None, None] * freqs
    cos, sin = jnp.cos(ang), jnp.sin(ang)
    t1, t2 = t[..., :half], t[..., half:]
    return jnp.concatenate([t1 * cos - t2 * sin, t1 * sin + t2 * cos], axis=-1)


def gmlp_mixer(za, v_gain, ws, bias):
    b_, s_ = za.shape[0], za.shape[1]
    za = jax.nn.gelu(za, approximate=False)
    u, v = jnp.split(za, 2, axis=-1)
    v = rmsnorm(v.reshape(b_, s_, GM_GROUPS, GM_GROUP_DIM),
                v_gain.reshape(GM_GROUPS, GM_GROUP_DIM))
    vb = v.reshape(b_, s_ // GM_BLOCK, GM_BLOCK, GM_GROUPS, GM_GROUP_DIM)
    cid = jnp.arange(GM_BLOCK) // CHUNK
    mask = cid[:, None] >= cid[None, :]
    wsm = jnp.where(mask[None], ws, jnp.zeros_like(ws))
    s = jnp.einsum('gij,bnjgd->bnigd', wsm, vb) + bias.T[None, None, :, :, None]
    return u * s.reshape(b_, s_, GM_DIM)


def retention(q, k, v, g, positions, gn_gain):
    b_, s_ = q.shape[0], q.shape[1]
    nc = s_ // CHUNK
    f32 = jnp.float32
    qf = rotary(q.reshape(b_, s_, RET_HEADS, RET_DK).astype(f32), positions)
    kf = rotary(k.reshape(b_, s_, RET_HEADS, RET_DK).astype(f32), positions) * (RET_DK ** -0.5)
    vf = v.reshape(b_, s_, RET_HEADS, RET_DV).astype(f32)

    def to_chunks(t):
        return t.reshape(b_, nc, CHUNK, RET_HEADS, t.shape[-1]).transpose(1, 0, 3, 2, 4)

    log_g = jnp.log1p(-jnp.exp2(-5.0 - jnp.arange(RET_HEADS, dtype=f32)))
    idx = jnp.arange(CHUNK, dtype=f32)
    d_intra = jnp.exp(log_g[:, None, None] * jnp.abs(idx[:, None] - idx[None, :]))
    q_dec = jnp.exp(log_g[:, None] * (idx + 1.0))
    k_dec = jnp.exp(log_g[:, None] * (CHUNK - 1.0 - idx))
    c_dec = jnp.exp(log_g * CHUNK)

    def step(state, inp):
        qc, kc, vc = inp
        att = jnp.einsum('bhid,bhjd->bhij', qc, kc) * d_intra
        o = jnp.einsum('bhij,bhjv->bhiv', att, vc) + jnp.einsum(
            'bhid,bhdv->bhiv', qc * q_dec[None, :, :, None], state)
        state = state * c_dec[None, :, None, None] + jnp.einsum(
            'bhjd,bhjv->bhdv', kc * k_dec[None, :, :, None], vc)
        return state, o

    s0 = jnp.zeros((b_, RET_HEADS, RET_DK, RET_DV), f32)
    _, o = lax.scan(step, s0, (to_chunks(qf), to_chunks(kf), to_chunks(vf)))
    o = o.transpose(1, 0, 3, 2, 4).reshape(b_, s_, RET_HEADS, RET_DV)
    mu = jnp.mean(o, axis=-1, keepdims=True)
    var = jnp.mean(jnp.square(o - mu), axis=-1, keepdims=True)
    on = (o - mu) * lax.rsqrt(var + EPS) * gn_gain.astype(f32).reshape(RET_HEADS, RET_DV)
    return jax.nn.silu(g) * on.reshape(b_, s_, RET_V).astype(g.dtype)


def peer(h, wq, subkeys, emb_u, emb_v):
    b_, s_, d_ = h.shape
    t_ = b_ * s_
    hf = h.reshape(t_, d_)
    q = (hf @ wq).reshape(t_, PEER_HEADS, 2, PEER_DHALF).astype(jnp.float32)
    sc = jnp.einsum('thpd,hpnd->thpn', q, subkeys.astype(jnp.float32))
    v1, i1 = lax.top_k(sc[:, :, 0], PEER_TOPK)
    v2, i2 = lax.top_k(sc[:, :, 1], PEER_TOPK)
    cand = (v1[..., :, None] + v2[..., None, :]).reshape(t_, PEER_HEADS, PEER_TOPK * PEER_TOPK)
    cidx = (i1[..., :, None] * PEER_NKEYS + i2[..., None, :]).reshape(t_, PEER_HEADS, PEER_TOPK * PEER_TOPK)
    top, sel = lax.top_k(cand, PEER_TOPK)
    eidx = jnp.take_along_axis(cidx, sel, axis=-1)
    gate = jax.nn.softmax(top, axis=-1).astype(h.dtype)

    nb = t_ // PEER_TOKEN_BLOCK
    xb = hf.reshape(nb, PEER_TOKEN_BLOCK, d_)
    ib = eidx.reshape(nb, PEER_TOKEN_BLOCK, PEER_HEADS, PEER_TOPK)
    gb = gate.reshape(nb, PEER_TOKEN_BLOCK, PEER_HEADS, PEER_TOPK)

    def block(args):
        xt, it, gt = args
        u = emb_u[it]
        a = jnp.einsum('td,thkd->thk', xt, u)
        w = jax.nn.gelu(a, approximate=False) * gt
        vv = emb_v[it]
        return jnp.einsum('thk,thkd->td', w, vv)

    y = lax.map(block, (xb, ib, gb))
    return y.reshape(b_, s_, d_)


def setup_inputs(seed: int = 0) -> dict:
    key = jax.random.key(seed)
    ks = jax.random.split(key, 24)
    n = jax.random.normal
    f32 = jnp.float32
    D = D_MODEL
    x = n(ks[0], (BATCH, SEQ, D), f32)
    c = n(ks[1], (BATCH, D), f32)
    offset = jax.random.randint(ks[2], (BATCH, 1), 0, 64, dtype=jnp.int32) * CHUNK
    positions = jnp.arange(SEQ, dtype=jnp.int32)[None, :] + offset
    return {
        "x": x,
        "c": c,
        "positions": positions,
        "w_ada": n(ks[3], (DEPTH, D, N_MOD * D), f32) * (0.5 * D ** -0.5),
        "b_ada": n(ks[4], (DEPTH, N_MOD * D), f32) * 0.02,
        "norm1_g": 1.0 + 0.02 * n(ks[5], (DEPTH, D), f32),
        "w_in": n(ks[6], (DEPTH, D, IN_WIDTH), f32) * D ** -0.5,
        "w_branch_gate": n(ks[7], (DEPTH, D, N_BRANCH * D), f32) * D ** -0.5,
        "b_branch_gate": n(ks[8], (DEPTH, N_BRANCH * D), f32) * 0.02,
        "gm_v_g": 1.0 + 0.02 * n(ks[9], (DEPTH, GM_DIM), f32),
        "gm_ws": n(ks[10], (DEPTH, GM_GROUPS, GM_BLOCK, GM_BLOCK), f32) * (0.5 * GM_BLOCK ** -0.5),
        "gm_b": 1.0 + 0.1 * n(ks[11], (DEPTH, GM_GROUPS, GM_BLOCK), f32),
        "ret_gn_g": 1.0 + 0.02 * n(ks[12], (DEPTH, RET_V), f32),
        "w_a_out": n(ks[13], (DEPTH, GM_DIM, D), f32) * GM_DIM ** -0.5,
        "w_b_out": n(ks[14], (DEPTH, RET_V, D), f32) * RET_V ** -0.5,
        "w_o": n(ks[15], (DEPTH, D, D), f32) * D ** -0.5,
        "norm2_g": 1.0 + 0.02 * n(ks[16], (DEPTH, D), f32),
        "peer_wq": n(ks[17], (DEPTH, D, PEER_HEADS * PEER_DKEY), f32) * D ** -0.5,
        "peer_subkeys": n(ks[18], (DEPTH, PEER_HEADS, 2, PEER_NKEYS, PEER_DHALF), f32) * PEER_DHALF ** -0.5,
        "peer_u": n(ks[19], (DEPTH, PEER_NEXPERTS, D), f32) * D ** -0.5,
        "peer_v": n(ks[20], (DEPTH, PEER_NEXPERTS, D), f32) * 0.5,
        "norm_f_g": 1.0 + 0.02 * n(ks[21], (D,), f32),
    }


def reference(x, c, positions, w_ada, b_ada, norm1_g, w_in, w_branch_gate, b_branch_gate,
              gm_v_g, gm_ws, gm_b, ret_gn_g, w_a_out, w_b_out, w_o, norm2_g,
              peer_wq, peer_subkeys, peer_u, peer_v, norm_f_g):
    for l in range(DEPTH):
        ada = jax.nn.silu(c) @ w_ada[l] + b_ada[l]
        sh1, sc1, ga1, sh2, sc2, ga2 = jnp.split(ada, N_MOD, axis=-1)

        h = modulate(rmsnorm(x, norm1_g[l]), sh1, sc1)
        z = h @ w_in[l]
        za, zq, zk, zv, zg = jnp.split(
            z, [2 * GM_DIM, 2 * GM_DIM + RET_QK, 2 * GM_DIM + 2 * RET_QK,
                2 * GM_DIM + 2 * RET_QK + RET_V], axis=-1)
        ya = gmlp_mixer(za, gm_v_g[l], gm_ws[l], gm_b[l]) @ w_a_out[l]
        yb = retention(zq, zk, zv, zg, positions, ret_gn_g[l]) @ w_b_out[l]
        gates = jax.nn.sigmoid(h @ w_branch_gate[l] + b_branch_gate[l])
        gate_a, gate_b = jnp.split(gates, N_BRANCH, axis=-1)
        mix = (gate_a * ya + gate_b * yb) @ w_o[l]
        x = x + ga1[:, None, :] * mix

        h2 = modulate(rmsnorm(x, norm2_g[l]), sh2, sc2)
        x = x + ga2[:, None, :] * peer(h2, peer_wq[l], peer_subkeys[l], peer_u[l], peer_v[l])
    return rmsnorm(x, norm_f_g)
```

```python
import os
from contextlib import ExitStack
import numpy as np
import concourse.bass as bass
import concourse.mybir as mybir
from concourse.bass_utils import run_bass_kernel_spmd

F32 = mybir.dt.float32
BF16 = mybir.dt.bfloat16
I32 = mybir.dt.int32
U32 = mybir.dt.uint32
AF = mybir.ActivationFunctionType
ALU = mybir.AluOpType
AX = mybir.AxisListType

D = 2048
NPRE = 24
NOWN = 8
EPS = 1e-6
H = 8
ENGS = ("pe", "act", "dve", "pool", "sp")
NDMA_SEMS = 28
TWO_PI = float(2 * np.pi)


class Sched:
    def __init__(self, nc, stack):
        self.nc = nc
        self.stack = stack
        self.ops = {e: [] for e in ENGS}
        self.cnt = {e: 0 for e in ENGS}
        self.sem = {e: stack.enter_context(nc.semaphore("s_" + e)) for e in ENGS if e != "sp"}
        self.dsem = [stack.enter_context(nc.semaphore("d%d" % i)) for i in range(NDMA_SEMS)]
        self.dcnt = [0] * NDMA_SEMS
        half = NDMA_SEMS // 2
        self.dpool = {"sp": list(range(0, half)), "act": list(range(0, half)), "pool": list(range(half, NDMA_SEMS))}
        self.dnext = {"sp": 0, "act": 0, "pool": 0}
        self.waited = {e: {} for e in ENGS}
        self.lastw = {}
        self.readers = {}
        self.alias = {}

    def _x(self, names):
        out = []
        for n in names:
            out.extend(self.alias.get(n, [n]))
        return out

    def sb(self, name, shape, dtype=F32):
        return self.stack.enter_context(self.nc.sbuf_tensor("sb_" + name, list(shape), dtype))

    def ps(self, name, shape, dtype=F32):
        return self.stack.enter_context(self.nc.psum_tensor("ps_" + name, list(shape), dtype))

    def _semobj(self, key):
        return self.sem[key] if isinstance(key, str) else self.dsem[key]

    def _deps(self, eng, reads, writes):
        need = {}

        def add(k, v, same_ok):
            if k == eng and not same_ok and eng == "pe":
                return
            if need.get(k, 0) < v:
                need[k] = v

        for r in reads:
            if r in self.lastw:
                k, v = self.lastw[r]
                add(k, v, True)
        for w in writes:
            if w in self.lastw:
                k, v = self.lastw[w]
                add(k, v, False)
            for k, v in self.readers.get(w, {}).items():
                add(k, v, False)
        waits = []
        wd = self.waited[eng]
        for k, v in need.items():
            if wd.get(k, 0) >= v:
                continue
            wd[k] = v
            waits.append((k, v))
        return waits

    def _record(self, key, val, reads, writes):
        for r in reads:
            self.readers.setdefault(r, {})[key] = val
        for w in writes:
            self.lastw[w] = (key, val)
            self.readers[w] = {}

    def op(self, eng, fn, reads=(), writes=()):
        reads, writes = self._x(reads), self._x(writes)
        waits = self._deps(eng, reads, writes)
        self.cnt[eng] += 1
        self.ops[eng].append((waits, fn, (eng, 1)))
        self._record(eng, self.cnt[eng], reads, writes)

    def dma(self, fn, reads=(), writes=(), queue="sp"):
        reads, writes = self._x(reads), self._x(writes)
        qp = self.dpool[queue]
        i = qp[self.dnext[queue] % len(qp)]
        self.dnext[queue] += 1
        waits = self._deps(queue, reads, writes)
        if self.dcnt[i] > 0 and self.waited[queue].get(i, 0) < self.dcnt[i]:
            self.waited[queue][i] = self.dcnt[i]
            waits.append((i, self.dcnt[i]))
        self.dcnt[i] += 16
        self.ops[queue].append((waits, fn, (i, 16)))
        self._record(i, self.dcnt[i], reads, writes)

    def final_wait(self, eng, reslist):
        waits = self._deps(eng, self._x(reslist), ())
        self.ops[eng].append((waits, None, None))

    def emit(self):
        nc = self.nc
        with nc.Block() as block:
            def run(ename):
                def body(e):
                    for waits, fn, inc in self.ops[ename]:
                        for k, v in waits:
                            e.wait_ge(self._semobj(k), v)
                        if fn is None:
                            continue
                        ins = fn(e)
                        ins.then_inc(self._semobj(inc[0]), inc[1])
                return body
            block.tensor(run("pe"))
            block.scalar(run("act"))
            block.vector(run("dve"))
            block.gpsimd(run("pool"))
            block.sync(run("sp"))


def build_program(dbg=False):
    nc = bass.Bass("TRN2", target_bir_lowering=False)

    def din(name, shape, dt=F32):
        return nc.dram_tensor(name, list(shape), dt, kind="ExternalInput").ap()

    xs = din("xs", [NPRE + NOWN, 128, D])
    posi = din("posi", [128, NPRE + NOWN], I32)
    cT_d = din("cT", [128, 16])
    w_ada = din("w_ada", [D, 6 * D])
    b_ada = din("b_ada", [1, 6 * D])
    norm1_g = din("norm1_g", [1, D])
    w_in = din("w_in", [D, 8192])
    w_bg = din("w_bg", [D, 4096])
    b_bg = din("b_bg", [1, 4096])
    gm_v_g = din("gm_v_g", [1, 1024])
    wsT_d = din("wsT", [128, 8, 128])
    gm_bT_d = din("gm_bT", [128, 8])
    ret_gn_g = din("ret_gn_g", [1, D])
    w_a = din("w_a", [1024, D])
    w_b = din("w_b", [D, D])
    w_o = din("w_o", [D, D])
    norm2_g = din("norm2_g", [1, D])
    wq = din("wq", [D, D])
    subkT_d = din("subkT", [128, 16, 128])
    peer_u = din("peer_u", [16384, D])
    peer_v = din("peer_v", [16384, D])
    norm_f_g = din("norm_f_g", [1, D])
    ident_d = din("ident", [128, 128])
    freq_d = din("freq", [128, 64])
    iota16_d = din("iota16", [128, 16])
    MT_d = din("MT", [128, 8, 128])
    qdecT_d = din("qdecT", [128, 8, 128])
    kdec_d = din("kdec", [128, 8])
    kdecP_d = din("kdecP", [128, NPRE, 8])
    out_d = nc.dram_tensor("out", [NOWN, 128, D], F32, kind="ExternalOutput").ap()
    ada_d = nc.dram_tensor("ada_scr", [1, 6 * D], F32, kind="Internal").ap()
    WSPEC = {"w_in": (w_in, 16, 16), "w_bg": (w_bg, 16, 8), "w_a": (w_a, 8, 4), "w_b": (w_b, 16, 4), "w_o": (w_o, 16, 4), "wq": (wq, 16, 4)}
    wscr = {k: nc.dram_tensor("wb_" + k, [v[2], 128, v[1] * 512], BF16, kind="Internal").ap() for k, v in WSPEC.items()}
    if dbg:
        dbg_x1 = nc.dram_tensor("dbg_x1", [NOWN, 128, D], F32, kind="ExternalOutput").ap()
        dbg_y = nc.dram_tensor("dbg_y", [NOWN, 128, D], F32, kind="ExternalOutput").ap()

    GAM = [1.0 - 2.0 ** (-5.0 - h) for h in range(H)]

    with ExitStack() as st:
        S = Sched(nc, st)
        sb, ps = S.sb, S.ps

        ident = sb("ident", [128, 128]); identb = sb("identb", [128, 128], BF16)
        freq = sb("freq", [128, 64]); iota16 = sb("iota16", [128, 16])
        MT = sb("MT", [128, 8, 128]); qdecT = sb("qdecT", [128, 8, 128])
        kdec = sb("kdec", [128, 8]); kdecP = sb("kdecP", [128, NPRE, 8])
        wsT = sb("wsT", [128, 8, 128], BF16)
        gmbT = sb("gmbT", [128, 8]); subkT = sb("subkT", [128, 16, 128])
        posI = sb("posI", [128, NPRE + NOWN], I32); posF = sb("posF", [128, NPRE + NOWN])
        cT = sb("cT", [128, 16]); scT = sb("scT", [128, 16])

        NW = 3
        wring = [sb("w%d" % i, [128, 8192], BF16) for i in range(NW)]
        wr_i = [0]

        def wnext():
            i = wr_i[0]; wr_i[0] = (i + 1) % NW
            return wring[i], ["w%da" % i, "w%db" % i]

        NBC = 2
        bcring = [sb("bc%d" % i, [128, D]) for i in range(NBC)]
        bc_i = [0]

        def bcnext():
            i = bc_i[0]; bc_i[0] = (i + 1) % NBC
            return bcring[i], "bc%d" % i

        NP = 4
        pring = [ps("P%d" % i, [128, 1024]) for i in range(NP)]
        p_i = [0]

        def pnext():
            i = p_i[0]; p_i[0] = (i + 1) % NP
            return pring[i], "P%d" % i

        xt = sb("xt", [128, D])
        ss = sb("ss", [128, 8]); rstd = sb("rstd", [128, 8])
        hb = sb("hb", [128, D], BF16); hT = sb("hT", [128, 16, 128], BF16)
        junk = hb; S.alias["junk"] = ["hb"]
        cs = sb("cs", [128, 4, 64])
        ki = sb("ki", [128, 64], I32); kf = sb("kf", [128, 2, 64])
        pi32 = sb("pi32", [128, 8, 16], I32); pcor = sb("pcor", [128, 8, 16])
        Sf = sb("Sf", [128, 8, 256]); Sb = sb("Sb", [128, 8, 256], BF16)
        st8 = sb("st8", [128, 4, 8])
        wk = sb("wk", [128, 256])
        v12 = sb("v12", [128, 16, 16]); i12 = sb("i12", [128, 16, 16], U32); i12f = sb("i12f", [128, 16, 16])
        top = sb("top", [128, 8, 16]); pos = sb("pos", [128, 8, 16], U32)
        paf = sb("paf", [128, 8, 16]); pbf = sb("pbf", [128, 8, 16])
        i1s = sb("i1s", [128, 8, 16]); i2s = sb("i2s", [128, 8, 16])
        eidx = sb("eidx", [128, 128], I32)
        gate = sb("gate", [128, 8, 16]); gsum = sb("gsum", [128, 8])
        acol = sb("acol", [128, 128]); wgt = sb("wgt", [128, 128])

        ARENA = 80 * 1024
        GRAN = 2048
        arena = sb("arena", [128, ARENA // 4])
        DSZ = {F32: 4, BF16: 2, I32: 4, U32: 4}

        def carve(off, name, shape, dtype=F32, parts=128):
            nel = int(np.prod(shape[1:]))
            nbytes = nel * DSZ[dtype]
            assert off % 4 == 0 and off + nbytes <= ARENA, (name, off, nbytes)
            v = arena[0:parts, off // 4:(off + nbytes) // 4]
            if dtype != F32:
                v = v.bitcast(dtype)
            if len(shape) == 3:
                v = v.rearrange("p (a b) -> p a b", a=shape[1])
            S.alias[name] = ["ar%d" % g for g in range(off // GRAN, (off + nbytes + GRAN - 1) // GRAN)]
            return v, off + ((nbytes + GRAN - 1) // GRAN) * GRAN

        K = 1024
        sg, o = carve(0, "sg", [128, D])
        rotA, o = carve(o, "rotA", [128, 8, 128]); rotB, o2_off = carve(o, "rotB", [128, 8, 128])
        o_sb, _ = carve(o - 4 * K, "o_sb", [128, 8, 256])
        o = o2_off
        u_sb, o = carve(o, "u_sb", [128, 8, 128]); v_f, o = carve(o, "v_f", [128, 8, 128])
        o2, _ = carve(o - 8 * K, "o2", [128, 8, 256])
        qb, o = carve(o, "qb", [128, 8, 128], BF16); kb, o = carve(o, "kb", [128, 8, 128], BF16)
        kd, o = carve(o, "kd", [128, 8, 128], BF16); qT, o = carve(o, "qT", [128, 8, 128], BF16)
        qdT, o = carve(o, "qdT", [128, 8, 128], BF16); kT, o = carve(o, "kT", [128, 8, 128], BF16)
        vr, o = carve(o, "vr", [128, 8, 256], BF16)
        v_b, o = carve(o, "v_b", [128, 8, 128], BF16); preA, o = carve(o, "preA", [128, 8, 128], BF16)
        preAT, o = carve(o, "preAT", [128, 8, 128], BF16); attm, o = carve(o, "attm", [128, 8, 128], BF16)
        retb, o = carve(o, "retb", [128, D], BF16); retT, o = carve(o, "retT", [128, 16, 128], BF16)
        mb, o = carve(o, "mb", [128, D], BF16); mT, o = carve(o, "mT", [128, 16, 128], BF16)
        gA, o = carve(o, "gA", [128, 512]); gB, o = carve(o, "gB", [128, 512])
        xt2, _ = carve(40 * K, "xt2", [128, D]); hb2, _ = carve(48 * K, "hb2", [128, D], BF16)
        hT2, _ = carve(52 * K, "hT2", [128, 16, 128], BF16)
        A1p, _ = carve(56 * K, "A1p", [128, D]); sh1p, _ = carve(64 * K, "sh1p", [128, D])
        cvS = [carve(0, "cvS0", [128, 8, 512], BF16)[0], carve(16 * K, "cvS1", [128, 8, 512], BF16)[0]]
        brow, _ = carve(0, "brow", [1, 512], parts=1); arow, _ = carve(2 * K, "arow", [1, 512], parts=1)
        h2, o = carve(0, "h2", [128, D]); y, o = carve(o, "y", [128, D])
        qpT, o = carve(o, "qpT", [128, 16, 128]); sc_sb, o = carve(o, "sc_sb", [128, 16, 128])
        cand, o = carve(o, "cand", [128, 8, 256]); oh, o = carve(o, "oh", [128, 8, 256])
        NG = 4
        gring = []
        for gi in range(NG):
            gv, o = carve(o, "g%d" % gi, [128, D])
            gring.append(gv)
        g_i = [0]

        def gnext():
            i = g_i[0]; g_i[0] = (i + 1) % NG
            return gring[i], "g%d" % i

        def V(fn, r, w):
            S.op("dve", fn, r, w)

        def A(fn, r, w):
            S.op("act", fn, r, w)

        def P(fn, r, w):
            S.op("pe", fn, r, w)

        def G(fn, r, w):
            S.op("pool", fn, r, w)

        def ld(out_ap, in_ap, w, queue="sp", r=()):
            S.dma(lambda e: e.dma_start(out=out_ap, in_=in_ap), reads=r, writes=w, queue=queue)

        def bcload(src_row, r=()):
            t, n = bcnext()
            width = src_row.shape[1]
            ld(t[:, 0:width], src_row.partition_broadcast(128)[:, 0, :], [n], r=r)
            return t, n

        def adaload(off):
            return bcload(ada_d[0:1, off:off + D], r=["ada%d" % k for k in range(off // 512, off // 512 + 4)])

        def wload_cast(src, K, N=512):
            t, n = wnext()
            view = t[:, 0:K * N].rearrange("p (k n) -> p k n", k=K)
            ld(view, src.rearrange("(k p) n -> p k n", p=128), n, queue="pool")
            return view, n

        def wload(name, j):
            K_ = WSPEC[name][1]
            t, n = wnext()
            ld(t[:, 0:K_ * 512], wscr[name][j], n, r=wb_res[(name, j)])
            return t[:, 0:K_ * 512].rearrange("p (k n) -> p k n", k=K_), n

        ld(ident[:], ident_d, ["ident"]); ld(freq[:], freq_d, ["freq"]); ld(iota16[:], iota16_d, ["iota16"])
        ld(MT[:], MT_d, ["MT"]); ld(qdecT[:], qdecT_d, ["qdecT"]); ld(kdec[:], kdec_d, ["kdec"])
        ld(kdecP[:], kdecP_d, ["kdecP"]); ld(wsT[:], wsT_d, ["wsT"], queue="pool"); ld(gmbT[:], gm_bT_d, ["gmbT"])
        ld(subkT[:], subkT_d, ["subkT"]); ld(posI[:], posi, ["posI"]); ld(cT[:], cT_d, ["cT"])
        V(lambda e: e.tensor_copy(identb[:], ident[:]), ["ident"], ["identb"])
        V(lambda e: e.tensor_copy(posF[:], posI[:]), ["posI"], ["posF"])
        V(lambda e: e.memset(wsT[64:128, :, 0:64], 0.0), ["wsT"], ["wsT"])
        V(lambda e: e.memset(Sf[:], 0.0), [], ["Sf"])

        wb_res = {}
        EARLY = [("w_in", j) for j in range(6, 12)]
        for name, j in EARLY:
            wsrc_, K_, nch = WSPEC[name]
            view, n = wload_cast(wsrc_[:, j * 512:(j + 1) * 512], K_)
            wb_res[(name, j)] = ["wb_%s_%d" % (name, j)]
            ld(wscr[name][j], view.rearrange("p k n -> p (k n)"), wb_res[(name, j)], r=n)
        late_jobs = []
        for name, (wsrc_, K_, nch) in WSPEC.items():
            for j in range(nch):
                if (name, j) in EARLY:
                    continue
                wb_res[(name, j)] = ["wb_%s_%d_%d" % (name, j, h) for h in range(K_ // 8)]
                for h in range(K_ // 8):
                    late_jobs.append((name, j, h))
        cv_i = [0]

        def emit_late(count):
            for _ in range(count):
                if not late_jobs:
                    return
                name, j, h = late_jobs.pop(0)
                wsrc_ = WSPEC[name][0]
                buf = cvS[cv_i[0] % 2]; bn = "cvS%d" % (cv_i[0] % 2); cv_i[0] += 1
                ld(buf[:], wsrc_[h * 1024:(h + 1) * 1024, j * 512:(j + 1) * 512].rearrange("(k p) n -> p k n", p=128), [bn], queue="pool")
                ld(wscr[name][j][:, h * 4096:(h + 1) * 4096], buf[:].rearrange("p k n -> p (k n)"), ["wb_%s_%d_%d" % (name, j, h)], r=[bn])

        A(lambda e: e.activation(scT[:], cT[:], AF.Silu), ["cT"], ["scT"])
        for n in range(24):
            pt, pn = pnext()
            for half in range(2):
                t, wn = wnext()
                wv = t[:].bitcast(F32).rearrange("p (k n) -> p k n", k=8)
                ld(wv, w_ada[half * 1024:(half + 1) * 1024, n * 512:(n + 1) * 512].rearrange("(k p) n -> p k n", p=128), wn)
                for k in range(8):
                    kc = half * 8 + k
                    P(lambda e, wv=wv, k=k, kc=kc, pt=pt: e.matmul(pt[0:1, 0:512], scT[:, kc:kc + 1], wv[:, k, :],
                                                                  start=(kc == 0), stop=(kc == 15)), ["scT"] + wn, [pn])
            ld(brow[:], b_ada[0:1, n * 512:(n + 1) * 512], ["brow"])
            V(lambda e, pt=pt: e.tensor_tensor(arow[:], pt[0:1, 0:512], brow[:], op=ALU.add), [pn, "brow"], ["arow"])
            ld(ada_d[0:1, n * 512:(n + 1) * 512], arow[:], ["ada%d" % n], r=["arow"])

        def load_x_norm(tile_idx, g_row, sc_off, sh_off, out_f32=None):
            ld(xt[:], xs[tile_idx], ["xt"])
            norm_from(xt, "xt", g_row, sc_off, sh_off, out_f32)

        def prefix_norm(tile_idx, par):
            x_, xn_, hb_, hbn_, hT_, hTn_ = (xt, "xt", hb, "hb", hT, "hT") if par == 0 else (xt2, "xt2", hb2, "hb2", hT2, "hT2")
            ld(x_[:], xs[tile_idx], [xn_])
            c0 = 4 * par
            A(lambda e: e.activation(hb_[:], x_[:], AF.Square, accum_out=ss[:, c0:c0 + 1]), [xn_], [hbn_, "ss"])
            V(lambda e: e.tensor_scalar(rstd[:, c0:c0 + 1], ss[:, c0:c0 + 1], 1.0 / D, EPS, op0=ALU.mult, op1=ALU.add), ["ss"], ["rstd"])
            A(lambda e: e.activation(rstd[:, c0 + 1:c0 + 2], rstd[:, c0:c0 + 1], AF.Sqrt), ["rstd"], ["rstd"])
            V(lambda e: e.reciprocal(rstd[:, c0 + 2:c0 + 3], rstd[:, c0 + 1:c0 + 2]), ["rstd"], ["rstd"])
            tmp, tmpn = bcnext()
            V(lambda e: e.scalar_tensor_tensor(tmp[:], x_[:], rstd[:, c0 + 2:c0 + 3], A1p[:], op0=ALU.mult, op1=ALU.mult), [xn_, "rstd", "A1p"], [tmpn])
            V(lambda e: e.tensor_tensor(hb_[:], tmp[:], sh1p[:], op=ALU.add), [tmpn, "sh1p"], [hbn_])
            transpose16(hb_, hbn_, hT_, hTn_)
            return hT_, hTn_

        def norm_from(src, srcn, g_row, sc_off, sh_off, out_f32=None):
            A(lambda e: e.activation(junk[:], src[:], AF.Square, accum_out=ss[:, 0:1]), [srcn], ["junk", "ss"])
            V(lambda e: e.tensor_scalar(rstd[:, 0:1], ss[:, 0:1], 1.0 / D, EPS, op0=ALU.mult, op1=ALU.add), ["ss"], ["rstd"])
            A(lambda e: e.activation(rstd[:, 1:2], rstd[:, 0:1], AF.Sqrt), ["rstd"], ["rstd"])
            V(lambda e: e.reciprocal(rstd[:, 2:3], rstd[:, 1:2]), ["rstd"], ["rstd"])
            gt, gn = bcload(g_row)
            sct, scn = adaload(sc_off)
            V(lambda e: e.scalar_tensor_tensor(sct[:], sct[:], 1.0, gt[:], op0=ALU.add, op1=ALU.mult), [gn, scn], [scn])
            sht, shn = adaload(sh_off)
            V(lambda e: e.scalar_tensor_tensor(sct[:], src[:], rstd[:, 2:3], sct[:], op0=ALU.mult, op1=ALU.mult), [srcn, "rstd", scn], [scn])
            if out_f32 is not None:
                V(lambda e: e.tensor_tensor(out_f32[0][:], sct[:], sht[:], op=ALU.add), [scn, shn], [out_f32[1]])
                A(lambda e: e.copy(hb[:], out_f32[0][:]), [out_f32[1]], ["hb"])
            else:
                V(lambda e: e.tensor_tensor(hb[:], sct[:], sht[:], op=ALU.add), [scn, shn], ["hb"])
            transpose16(hb, "hb", hT, "hT")

        def transpose16(src, srcn, dst, dstn, nchunks=16):
            for half in range((nchunks + 7) // 8):
                pt, pn = pnext()
                pv = pt[:].bitcast(BF16)
                cnt = min(8, nchunks - half * 8)
                for j in range(cnt):
                    c = half * 8 + j
                    P(lambda e, pv=pv, j=j, c=c: e.transpose(pv[:, j * 128:(j + 1) * 128], src[:, c * 128:(c + 1) * 128], identb[:]),
                      [srcn, "identb"], [pn])
                A(lambda e, pv=pv, half=half, cnt=cnt: e.copy(dst[:, half * 8:half * 8 + cnt, :].rearrange("p a b -> p (a b)"), pv[:, 0:cnt * 128]),
                  [pn], [dstn])

        def rope_tables(tile_idx):
            pcol = posF[:, tile_idx:tile_idx + 1]
            PI = float(np.pi)
            for (shift, dsti) in ((0.0, 1), (PI / 2, 0)):
                V(lambda e, shift=shift: e.tensor_scalar(cs[:, 3, :], freq[:], pcol, shift, op0=ALU.mult, op1=ALU.add), ["freq", "posF", "cs"], ["cs3"])
                V(lambda e: e.tensor_scalar(ki[:], cs[:, 3, :], 1.0 / TWO_PI, None, op0=ALU.mult), ["cs3"], ["ki"])
                V(lambda e: e.tensor_copy(kf[:, 0, :], ki[:]), ["ki"], ["kf"])
                V(lambda e: e.scalar_tensor_tensor(cs[:, 3, :], kf[:, 0, :], -TWO_PI, cs[:, 3, :], op0=ALU.mult, op1=ALU.add), ["kf", "cs3"], ["cs3"])
                V(lambda e: e.tensor_scalar(kf[:, 1, :], cs[:, 3, :], PI, -TWO_PI, op0=ALU.is_gt, op1=ALU.mult), ["cs3"], ["kf"])
                V(lambda e: e.tensor_tensor(cs[:, 3, :], cs[:, 3, :], kf[:, 1, :], op=ALU.add), ["kf", "cs3"], ["cs3"])
                V(lambda e: e.tensor_scalar(cs[:, 3, :], cs[:, 3, :], -PI, PI, op0=ALU.max, op1=ALU.min), ["cs3"], ["cs3"])
                A(lambda e, dsti=dsti: e.activation(cs[:, dsti, :], cs[:, 3, :], AF.Sin), ["cs3"], ["cs"])
            V(lambda e: e.tensor_scalar(cs[:, 2, :], cs[:, 1, :], -1.0, None, op0=ALU.mult), ["cs"], ["cs"])

        def rope(pt, pn, nh, dst, dstn):
            pv = pt[:, 0:nh * 128].rearrange("p (h d) -> p h d", h=nh)
            cosb = cs[:, 0, :].unsqueeze(1).to_broadcast([128, nh, 64])
            sinb = cs[:, 1, :].unsqueeze(1).to_broadcast([128, nh, 64])
            nsinb = cs[:, 2, :].unsqueeze(1).to_broadcast([128, nh, 64])
            V(lambda e: e.tensor_tensor(rotA[:, 0:nh, 0:64], pv[:, :, 0:64], cosb, op=ALU.mult), [pn, "cs"], ["rotA"])
            V(lambda e: e.tensor_tensor(rotA[:, 0:nh, 64:128], pv[:, :, 64:128], cosb, op=ALU.mult), [pn, "cs"], ["rotA"])
            V(lambda e: e.tensor_tensor(rotB[:, 0:nh, 0:64], pv[:, :, 64:128], nsinb, op=ALU.mult), [pn, "cs"], ["rotB"])
            V(lambda e: e.tensor_tensor(rotB[:, 0:nh, 64:128], pv[:, :, 0:64], sinb, op=ALU.mult), [pn, "cs"], ["rotB"])
            V(lambda e: e.tensor_tensor(dst[:, 0:nh, :], rotA[:, 0:nh, :], rotB[:, 0:nh, :], op=ALU.add), ["rotA", "rotB"], [dstn])

        def proj(pt, pn, col, wname, c0, K=16, lhs=None, lhsn="hT"):
            lhs = hT if lhs is None else lhs
            wv, wn = wload(wname, c0 // 512)
            for kc in range(K):
                P(lambda e, wv=wv, kc=kc: e.matmul(pt[:, col * 512:(col + 1) * 512], lhs[:, kc, :], wv[:, kc, :],
                                                   start=(kc == 0), stop=(kc == K - 1)), [lhsn] + wn, [pn])

        gt_, gn_ = bcload(norm1_g)
        ld(A1p[:], ada_d[0:1, D:2 * D].partition_broadcast(128)[:, 0, :], ["A1p"], r=["ada%d" % k for k in range(4, 8)])
        V(lambda e: e.scalar_tensor_tensor(A1p[:], A1p[:], 1.0, gt_[:], op0=ALU.add, op1=ALU.mult), ["A1p", gn_], ["A1p"])
        ld(sh1p[:], ada_d[0:1, 0:D].partition_broadcast(128)[:, 0, :], ["sh1p"], r=["ada%d" % k for k in range(0, 4)])
        for hh in range(2):
            wk_v, wk_n = wload("w_in", 6 + hh)
            wv0, wv0n = wload("w_in", 8 + hh * 2)
            wv1, wv1n = wload("w_in", 9 + hh * 2)
            for p in range(NPRE):
                hT_, hTn_ = prefix_norm(p, p % 2)
                rope_tables(p)
                pk, pkn = pnext()
                for kc in range(16):
                    P(lambda e, kc=kc, pk=pk, hT_=hT_: e.matmul(pk[:, 0:512], hT_[:, kc, :], wk_v[:, kc, :], start=(kc == 0), stop=(kc == 15)),
                      [hTn_] + wk_n, [pkn])
                pv_, pvn = pnext()
                for j, (wv, wn) in enumerate(((wv0, wv0n), (wv1, wv1n))):
                    for kc in range(16):
                        P(lambda e, kc=kc, j=j, wv=wv, pv_=pv_, hT_=hT_: e.matmul(pv_[:, j * 512:(j + 1) * 512], hT_[:, kc, :], wv[:, kc, :],
                                                                         start=(kc == 0), stop=(kc == 15)), [hTn_] + wn, [pvn])
                rope(pk, pkn, 4, kb, "kb")
                V(lambda e, p=p, hh=hh: e.tensor_tensor(kd[:, 0:4, :], kb[:, 0:4, :],
                                                        kdecP[:, p, hh * 4:(hh + 1) * 4].unsqueeze(2).to_broadcast([128, 4, 128]), op=ALU.mult),
                  ["kb", "kdecP"], ["kd"])
                A(lambda e, pv_=pv_: e.copy(vr[:, 0:4, :].rearrange("p a b -> p (a b)"), pv_[:, :]), [pvn], ["vr"])
                pst, pstn = pnext()
                for h4 in range(4):
                    P(lambda e, h4=h4, pst=pst: e.matmul(pst[:, h4 * 256:(h4 + 1) * 256], kd[:, h4, :], vr[:, h4, :], start=True, stop=True),
                      ["kd", "vr"], [pstn])
                V(lambda e, hh=hh, pst=pst: e.tensor_tensor(Sf[:, hh * 4:(hh + 1) * 4, :].rearrange("p a b -> p (a b)"),
                                                            Sf[:, hh * 4:(hh + 1) * 4, :].rearrange("p a b -> p (a b)"), pst[:, :], op=ALU.add),
                  [pstn, "Sf"], ["Sf"])
                emit_late(2)
        emit_late(len(late_jobs))
        A(lambda e: e.copy(Sb[:], Sf[:]), ["Sf"], ["Sb"])

        for i in range(NOWN):
            ti = NPRE + i
            load_x_norm(ti, norm1_g, 1 * D, 0)
            rope_tables(ti)
            pu, pun = pnext(); proj(pu, pun, 0, "w_in", 0); proj(pu, pun, 1, "w_in", 512)
            A(lambda e, pu=pu: e.activation(u_sb[:].rearrange("p a b -> p (a b)"), pu[:, :], AF.Gelu), [pun], ["u_sb"])
            pvv, pvvn = pnext(); proj(pvv, pvvn, 0, "w_in", 1024); proj(pvv, pvvn, 1, "w_in", 1536)
            A(lambda e, pvv=pvv: e.activation(v_f[:].rearrange("p a b -> p (a b)"), pvv[:, :], AF.Gelu), [pvvn], ["v_f"])
            V(lambda e: e.tensor_tensor(rotA[:], v_f[:], v_f[:], op=ALU.mult), ["v_f"], ["rotA"])
            V(lambda e: e.tensor_reduce(ss[:, 0:8], rotA[:], axis=AX.X, op=ALU.add), ["rotA"], ["ss"])
            V(lambda e: e.tensor_scalar(rstd[:, 0:8], ss[:, 0:8], 1.0 / 128, EPS, op0=ALU.mult, op1=ALU.add), ["ss"], ["rstd"])
            A(lambda e: e.activation(ss[:, 0:8], rstd[:, 0:8], AF.Sqrt), ["rstd"], ["ss"])
            V(lambda e: e.reciprocal(rstd[:, 0:8], ss[:, 0:8]), ["ss"], ["rstd"])
            vg, vgn = bcload(gm_v_g)
            V(lambda e: e.tensor_tensor(rotA[:], v_f[:], rstd[:, 0:8].unsqueeze(2).to_broadcast([128, 8, 128]), op=ALU.mult), ["v_f", "rstd"], ["rotA"])
            V(lambda e, vg=vg: e.tensor_tensor(v_b[:].rearrange("p a b -> p (a b)"), rotA[:].rearrange("p a b -> p (a b)"), vg[:, 0:1024], op=ALU.mult),
              ["rotA", vgn], ["v_b"])
            psg, psgn = pnext()
            for g in range(8):
                P(lambda e, g=g, psg=psg: e.matmul(psg[:, g * 128:(g + 1) * 128], wsT[:, g, :], v_b[:, g, :], start=True, stop=True),
                  ["wsT", "v_b"], [psgn])
            V(lambda e, psg=psg: e.tensor_tensor(rotA[:], psg[:, :].rearrange("p (a b) -> p a b", a=8),
                                                 gmbT[:, :].unsqueeze(2).to_broadcast([128, 8, 128]), op=ALU.add), [psgn, "gmbT"], ["rotA"])
            V(lambda e: e.tensor_tensor(preA[:], rotA[:], u_sb[:], op=ALU.mult), ["rotA", "u_sb"], ["preA"])
            transpose16(preA[:].rearrange("p a b -> p (a b)"), "preA", preAT, "preAT", nchunks=8)
            pq, pqn = pnext(); proj(pq, pqn, 0, "w_in", 2048); proj(pq, pqn, 1, "w_in", 2560)
            rope(pq, pqn, 8, qb, "qb")
            pk, pkn = pnext(); proj(pk, pkn, 0, "w_in", 3072); proj(pk, pkn, 1, "w_in", 3584)
            rope(pk, pkn, 8, kb, "kb")
            V(lambda e: e.tensor_tensor(kd[:], kb[:], kdec[:, :].unsqueeze(2).to_broadcast([128, 8, 128]), op=ALU.mult), ["kb", "kdec"], ["kd"])
            pt, pn = pnext(); pv = pt[:].bitcast(BF16)
            for h in range(8):
                P(lambda e, h=h, pv=pv: e.transpose(pv[:, h * 128:(h + 1) * 128], qb[:, h, :], identb[:]), ["qb", "identb"], [pn])
            A(lambda e, pv=pv: e.copy(qT[:].rearrange("p a b -> p (a b)"), pv[:, 0:1024]), [pn], ["qT"])
            V(lambda e, pv=pv: e.tensor_tensor(qdT[:].rearrange("p a b -> p (a b)"), pv[:, 0:1024], qdecT[:].rearrange("p a b -> p (a b)"), op=ALU.mult),
              [pn, "qdecT"], ["qdT"])
            pt, pn = pnext(); pv = pt[:].bitcast(BF16)
            for h in range(8):
                P(lambda e, h=h, pv=pv: e.transpose(pv[:, h * 128:(h + 1) * 128], kb[:, h, :], identb[:]), ["kb", "identb"], [pn])
            A(lambda e, pv=pv: e.copy(kT[:].rearrange("p a b -> p (a b)"), pv[:, 0:1024]), [pn], ["kT"])
            for j in range(2):
                pvr, pvrn = pnext(); proj(pvr, pvrn, 0, "w_in", 4096 + j * 1024); proj(pvr, pvrn, 1, "w_in", 4096 + j * 1024 + 512)
                A(lambda e, j=j, pvr=pvr: e.copy(vr[:, j * 4:(j + 1) * 4, :].rearrange("p a b -> p (a b)"), pvr[:, :]), [pvrn], ["vr"])
            for j in range(2):
                pg, pgn = pnext(); proj(pg, pgn, 0, "w_in", 6144 + j * 1024); proj(pg, pgn, 1, "w_in", 6144 + j * 1024 + 512)
                A(lambda e, j=j, pg=pg: e.activation(sg[:, j * 1024:(j + 1) * 1024], pg[:, :], AF.Silu), [pgn], ["sg"])
            pat, patn = pnext()
            for h in range(8):
                P(lambda e, h=h, pat=pat: e.matmul(pat[:, h * 128:(h + 1) * 128], kT[:, h, :], qT[:, h, :], start=True, stop=True),
                  ["kT", "qT"], [patn])
            V(lambda e, pat=pat: e.tensor_tensor(attm[:].rearrange("p a b -> p (a b)"), pat[:, :], MT[:].rearrange("p a b -> p (a b)"), op=ALU.mult),
              [patn, "MT"], ["attm"])
            for j in range(2):
                po, pon = pnext()
                for h4 in range(4):
                    h = j * 4 + h4
                    P(lambda e, h=h, h4=h4, po=po: e.matmul(po[:, h4 * 256:(h4 + 1) * 256], attm[:, h, :], vr[:, h, :], start=True, stop=False),
                      ["attm", "vr"], [pon])
                    P(lambda e, h=h, h4=h4, po=po: e.matmul(po[:, h4 * 256:(h4 + 1) * 256], qdT[:, h, :], Sb[:, h, :], start=False, stop=True),
                      ["qdT", "Sb"], [pon])
                A(lambda e, j=j, po=po: e.copy(o_sb[:, j * 4:(j + 1) * 4, :].rearrange("p a b -> p (a b)"), po[:, :]), [pon], ["o_sb"])
            for j in range(2):
                pst, pstn = pnext()
                for h4 in range(4):
                    h = j * 4 + h4
                    P(lambda e, h=h, h4=h4, pst=pst: e.matmul(pst[:, h4 * 256:(h4 + 1) * 256], kd[:, h, :], vr[:, h, :], start=True, stop=True),
                      ["kd", "vr"], [pstn])
                for h4 in range(4):
                    h = j * 4 + h4
                    V(lambda e, h=h, h4=h4, pst=pst: e.scalar_tensor_tensor(Sf[:, h, :], Sf[:, h, :], float(GAM[h] ** 128), pst[:, h4 * 256:(h4 + 1) * 256],
                                                                            op0=ALU.mult, op1=ALU.add), [pstn, "Sf"], ["Sf"])
            A(lambda e: e.copy(Sb[:], Sf[:]), ["Sf"], ["Sb"])
            V(lambda e: e.tensor_reduce(st8[:, 0, :], o_sb[:], axis=AX.X, op=ALU.add), ["o_sb"], ["st8"])
            V(lambda e: e.tensor_tensor(o2[:], o_sb[:], o_sb[:], op=ALU.mult), ["o_sb"], ["o2"])
            V(lambda e: e.tensor_reduce(st8[:, 1, :], o2[:], axis=AX.X, op=ALU.add), ["o2"], ["st8"])
            V(lambda e: e.tensor_scalar(st8[:, 0, :], st8[:, 0, :], 1.0 / 256, None, op0=ALU.mult), ["st8"], ["st8"])
            V(lambda e: e.tensor_tensor(st8[:, 2, :], st8[:, 0, :], st8[:, 0, :], op=ALU.mult), ["st8"], ["st8"])
            V(lambda e: e.scalar_tensor_tensor(st8[:, 1, :], st8[:, 1, :], 1.0 / 256, st8[:, 2, :], op0=ALU.mult, op1=ALU.subtract), ["st8"], ["st8"])
            V(lambda e: e.tensor_scalar(st8[:, 1, :], st8[:, 1, :], EPS, None, op0=ALU.add), ["st8"], ["st8"])
            A(lambda e: e.activation(st8[:, 2, :], st8[:, 1, :], AF.Sqrt), ["st8"], ["st8"])
            V(lambda e: e.reciprocal(st8[:, 3, :], st8[:, 2, :]), ["st8"], ["st8"])
            V(lambda e: e.tensor_tensor(o2[:], o_sb[:], st8[:, 0, :].unsqueeze(2).to_broadcast([128, 8, 256]), op=ALU.subtract), ["o_sb", "st8"], ["o2"])
            V(lambda e: e.tensor_tensor(o2[:], o2[:], st8[:, 3, :].unsqueeze(2).to_broadcast([128, 8, 256]), op=ALU.mult), ["o2", "st8"], ["o2"])
            gnb, gnn = bcload(ret_gn_g)
            V(lambda e, gnb=gnb: e.tensor_tensor(o2[:].rearrange("p a b -> p (a b)"), o2[:].rearrange("p a b -> p (a b)"), gnb[:], op=ALU.mult), ["o2", gnn], ["o2"])
            V(lambda e: e.tensor_tensor(retb[:], o2[:].rearrange("p a b -> p (a b)"), sg[:], op=ALU.mult), ["o2", "sg"], ["retb"])
            transpose16(retb, "retb", retT, "retT")
            for n in range(4):
                pga, pgan = pnext()
                proj(pga, pgan, 0, "w_bg", n * 512); proj(pga, pgan, 1, "w_bg", 2048 + n * 512)
                bb, bbn = bcload(b_bg[0:1, n * 512:(n + 1) * 512])
                bb2, bb2n = bcload(b_bg[0:1, 2048 + n * 512:2048 + (n + 1) * 512])
                V(lambda e, pga=pga, bb=bb: e.tensor_tensor(gA[:], pga[:, 0:512], bb[:, 0:512], op=ALU.add), [pgan, bbn], ["gA"])
                V(lambda e, pga=pga, bb2=bb2: e.tensor_tensor(gB[:], pga[:, 512:1024], bb2[:, 0:512], op=ALU.add), [pgan, bb2n], ["gB"])
                A(lambda e: e.activation(gA[:], gA[:], AF.Sigmoid), ["gA"], ["gA"])
                A(lambda e: e.activation(gB[:], gB[:], AF.Sigmoid), ["gB"], ["gB"])
                pyy, pyyn = pnext()
                proj(pyy, pyyn, 0, "w_a", n * 512, K=8, lhs=preAT, lhsn="preAT")
                proj(pyy, pyyn, 1, "w_b", n * 512, K=16, lhs=retT, lhsn="retT")
                V(lambda e, pyy=pyy: e.tensor_tensor(gA[:], gA[:], pyy[:, 0:512], op=ALU.mult), ["gA", pyyn], ["gA"])
                V(lambda e, pyy=pyy: e.tensor_tensor(gB[:], gB[:], pyy[:, 512:1024], op=ALU.mult), ["gB", pyyn], ["gB"])
                V(lambda e, n=n: e.tensor_tensor(mb[:, n * 512:(n + 1) * 512], gA[:], gB[:], op=ALU.add), ["gA", "gB"], ["mb"])
            transpose16(mb, "mb", mT, "mT")
            ga1, ga1n = adaload(2 * D)
            for j in range(2):
                pm, pmn = pnext()
                proj(pm, pmn, 0, "w_o", j * 1024, lhs=mT, lhsn="mT"); proj(pm, pmn, 1, "w_o", j * 1024 + 512, lhs=mT, lhsn="mT")
                V(lambda e, j=j, pm=pm, ga1=ga1: e.tensor_tensor(ga1[:, j * 1024:(j + 1) * 1024], ga1[:, j * 1024:(j + 1) * 1024], pm[:, :], op=ALU.mult),
                  [pmn, ga1n], [ga1n])
            V(lambda e, ga1=ga1: e.tensor_tensor(xt[:], xt[:], ga1[:], op=ALU.add), ["xt", ga1n], ["xt"])
            if dbg:
                ld(dbg_x1[i], xt[:], ["dbgx1_%d" % i], r=["xt"])

            norm_from(xt, "xt", norm2_g, 4 * D, 3 * D, out_f32=(h2, "h2"))
            for half in range(2):
                pq2, pq2n = pnext()
                for cc in range(2):
                    wv, wn = wload("wq", half * 2 + cc)
                    for c4 in range(4):
                        j = cc * 4 + c4
                        for kc in range(16):
                            P(lambda e, wv=wv, kc=kc, c4=c4, j=j, pq2=pq2: e.matmul(pq2[:, j * 128:(j + 1) * 128], wv[:, kc, c4 * 128:(c4 + 1) * 128], hT[:, kc, :],
                                                                                   start=(kc == 0), stop=(kc == 15)), ["hT"] + wn, [pq2n])
                A(lambda e, half=half, pq2=pq2: e.copy(qpT[:, half * 8:(half + 1) * 8, :].rearrange("p a b -> p (a b)"), pq2[:, :]), [pq2n], ["qpT"])
            for half in range(2):
                psc, pscn = pnext()
                for j in range(8):
                    c = half * 8 + j
                    P(lambda e, c=c, j=j, psc=psc: e.matmul(psc[:, j * 128:(j + 1) * 128], qpT[:, c, :], subkT[:, c, :], start=True, stop=True),
                      ["qpT", "subkT"], [pscn])
                A(lambda e, half=half, psc=psc: e.copy(sc_sb[:, half * 8:(half + 1) * 8, :].rearrange("p a b -> p (a b)"), psc[:, :]), [pscn], ["sc_sb"])
            for c in range(16):
                V(lambda e, c=c: e.max(out=v12[:, c, 0:8], in_=sc_sb[:, c, :]), ["sc_sb"], ["v12"])
                V(lambda e, c=c: e.max_index(out=i12[:, c, 0:8], in_max=v12[:, c, 0:8], in_values=sc_sb[:, c, :]), ["sc_sb", "v12"], ["i12"])
                V(lambda e, c=c: e.match_replace(out=wk[:, 0:128], in_to_replace=v12[:, c, 0:8], in_values=sc_sb[:, c, :], imm_value=-1e30), ["sc_sb", "v12"], ["wk"])
                V(lambda e, c=c: e.max(out=v12[:, c, 8:16], in_=wk[:, 0:128]), ["wk"], ["v12"])
                V(lambda e, c=c: e.max_index(out=i12[:, c, 8:16], in_max=v12[:, c, 8:16], in_values=wk[:, 0:128]), ["wk", "v12"], ["i12"])
            V(lambda e: e.tensor_copy(i12f[:], i12[:]), ["i12"], ["i12f"])
            v12v = v12[:].rearrange("p (h two) k -> p h two k", two=2)
            i12v = i12f[:].rearrange("p (h two) k -> p h two k", two=2)
            for h in range(8):
                V(lambda e, h=h: e.tensor_tensor(cand[:, h, :].rearrange("p (a b) -> p a b", a=16),
                                                 v12v[:, h, 0, :].unsqueeze(2).to_broadcast([128, 16, 16]),
                                                 v12v[:, h, 1, :].unsqueeze(1).to_broadcast([128, 16, 16]), op=ALU.add), ["v12"], ["cand"])
            for h in range(8):
                V(lambda e, h=h: e.max(out=top[:, h, 0:8], in_=cand[:, h, :]), ["cand"], ["top"])
                V(lambda e, h=h: e.max_index(out=pos[:, h, 0:8], in_max=top[:, h, 0:8], in_values=cand[:, h, :]), ["cand", "top"], ["pos"])
                V(lambda e, h=h: e.match_replace(out=wk[:], in_to_replace=top[:, h, 0:8], in_values=cand[:, h, :], imm_value=-1e30), ["cand", "top"], ["wk"])
                V(lambda e, h=h: e.max(out=top[:, h, 8:16], in_=wk[:]), ["wk"], ["top"])
                V(lambda e, h=h: e.max_index(out=pos[:, h, 8:16], in_max=top[:, h, 8:16], in_values=wk[:]), ["wk", "top"], ["pos"])
            V(lambda e: e.tensor_copy(pcor[:], pos[:]), ["pos"], ["pcor"])
            V(lambda e: e.tensor_scalar(pi32[:], pcor[:], 0.0625, None, op0=ALU.mult), ["pcor"], ["pi32"])
            V(lambda e: e.tensor_copy(paf[:], pi32[:]), ["pi32"], ["paf"])
            V(lambda e: e.scalar_tensor_tensor(pbf[:].rearrange("p h k -> p (h k)"), paf[:].rearrange("p h k -> p (h k)"), -16.0,
                                               pcor[:].rearrange("p h k -> p (h k)"), op0=ALU.mult, op1=ALU.add), ["paf", "pcor"], ["pbf"])
            V(lambda e: e.tensor_scalar(pcor[:], pbf[:], 0.0, None, op0=ALU.is_lt), ["pbf"], ["pcor"])
            V(lambda e: e.tensor_tensor(paf[:], paf[:], pcor[:], op=ALU.subtract), ["paf", "pcor"], ["paf"])
            V(lambda e: e.scalar_tensor_tensor(pbf[:].rearrange("p h k -> p (h k)"), pcor[:].rearrange("p h k -> p (h k)"), 16.0,
                                               pbf[:].rearrange("p h k -> p (h k)"), op0=ALU.mult, op1=ALU.add), ["pcor", "pbf"], ["pbf"])
            for (pf, pfn, two, dst, dstn) in ((paf, "paf", 0, i1s, "i1s"), (pbf, "pbf", 1, i2s, "i2s")):
                for h in range(8):
                    ohv = oh[:, h, :].rearrange("p (k a) -> p k a", k=16)
                    V(lambda e, h=h, pf=pf, ohv=ohv: e.tensor_tensor(ohv, iota16[:, :].unsqueeze(1).to_broadcast([128, 16, 16]),
                                                                     pf[:, h, :].unsqueeze(2).to_broadcast([128, 16, 16]), op=ALU.is_equal),
                      ["iota16", pfn], ["oh"])
                    V(lambda e, h=h, two=two, ohv=ohv: e.tensor_tensor(ohv, ohv, i12v[:, h, two, :].unsqueeze(1).to_broadcast([128, 16, 16]), op=ALU.mult),
                      ["oh", "i12f"], ["oh"])
                V(lambda e, dst=dst: e.tensor_reduce(dst[:].rearrange("p h k -> p (h k)"), oh[:].rearrange("p h (k a) -> p (h k) a", a=16), axis=AX.X, op=ALU.add),
                  ["oh"], [dstn])
            V(lambda e: e.scalar_tensor_tensor(i1s[:], i1s[:], 128.0, i2s[:], op0=ALU.mult, op1=ALU.add), ["i1s", "i2s"], ["i1s"])
            V(lambda e: e.tensor_copy(eidx[:], i1s[:].rearrange("p h k -> p (h k)")), ["i1s"], ["eidx"])
            V(lambda e: e.tensor_tensor(gate[:], top[:], top[:, :, 0:1].to_broadcast([128, 8, 16]), op=ALU.subtract), ["top"], ["gate"])
            A(lambda e: e.activation(gate[:], gate[:], AF.Exp), ["gate"], ["gate"])
            V(lambda e: e.tensor_reduce(gsum[:], gate[:], axis=AX.X, op=ALU.add), ["gate"], ["gsum"])
            V(lambda e: e.reciprocal(gsum[:], gsum[:]), ["gsum"], ["gsum"])
            V(lambda e: e.tensor_tensor(gate[:], gate[:], gsum[:, :].unsqueeze(2).to_broadcast([128, 8, 16]), op=ALU.mult), ["gate", "gsum"], ["gate"])
            for hk in range(128):
                gt, gn = gnext()
                S.dma(lambda e, gt=gt, hk=hk: e.indirect_dma_start(out=gt[:], out_offset=None, in_=peer_u,
                                                                  in_offset=bass.IndirectOffsetOnAxis(ap=eidx[:, hk:hk + 1], axis=0)),
                      reads=["eidx"], writes=[gn], queue="pool")
                V(lambda e, gt=gt, hk=hk: e.scalar_tensor_tensor(junk[:], gt[:], 1.0, h2[:], op0=ALU.mult, op1=ALU.mult, accum_out=acol[:, hk:hk + 1]),
                  [gn, "h2"], ["junk", "acol"])
            A(lambda e: e.activation(wgt[:], acol[:], AF.Gelu), ["acol"], ["wgt"])
            V(lambda e: e.tensor_tensor(wgt[:], wgt[:], gate[:].rearrange("p h k -> p (h k)"), op=ALU.mult), ["wgt", "gate"], ["wgt"])
            for hk in range(128):
                gt, gn = gnext()
                S.dma(lambda e, gt=gt, hk=hk: e.indirect_dma_start(out=gt[:], out_offset=None, in_=peer_v,
                                                                  in_offset=bass.IndirectOffsetOnAxis(ap=eidx[:, hk:hk + 1], axis=0)),
                      reads=["eidx"], writes=[gn], queue="pool")
                if hk == 0:
                    V(lambda e, gt=gt: e.tensor_scalar(y[:], gt[:], wgt[:, 0:1], None, op0=ALU.mult), [gn, "wgt"], ["y"])
                else:
                    V(lambda e, gt=gt, hk=hk: e.scalar_tensor_tensor(y[:], gt[:], wgt[:, hk:hk + 1], y[:], op0=ALU.mult, op1=ALU.add), [gn, "wgt", "y"], ["y"])
            if dbg:
                ld(dbg_y[i], y[:], ["dbgy_%d" % i], r=["y"])
            ga2, ga2n = adaload(5 * D)
            V(lambda e, ga2=ga2: e.tensor_tensor(y[:], y[:], ga2[:], op=ALU.mult), ["y", ga2n], ["y"])
            V(lambda e: e.tensor_tensor(xt[:], xt[:], y[:], op=ALU.add), ["xt", "y"], ["xt"])
            A(lambda e: e.activation(junk[:], xt[:], AF.Square, accum_out=ss[:, 0:1]), ["xt"], ["junk", "ss"])
            V(lambda e: e.tensor_scalar(rstd[:, 0:1], ss[:, 0:1], 1.0 / D, EPS, op0=ALU.mult, op1=ALU.add), ["ss"], ["rstd"])
            A(lambda e: e.activation(rstd[:, 1:2], rstd[:, 0:1], AF.Sqrt), ["rstd"], ["rstd"])
            V(lambda e: e.reciprocal(rstd[:, 2:3], rstd[:, 1:2]), ["rstd"], ["rstd"])
            gf, gfn = bcload(norm_f_g)
            V(lambda e, gf=gf: e.scalar_tensor_tensor(y[:], xt[:], rstd[:, 2:3], gf[:], op0=ALU.mult, op1=ALU.mult), ["xt", "rstd", gfn], ["y"])
            ld(out_d[i], y[:], ["out%d" % i], r=["y"])
        fin = ["out%d" % i for i in range(NOWN)] + ([n % i for i in range(NOWN) for n in ("dbgx1_%d", "dbgy_%d")] if dbg else [])
        S.final_wait("sp", fin)
        S.emit()
    return nc


def _consts(seg):
    gam = np.array([1.0 - 2.0 ** (-5.0 - h) for h in range(H)], dtype=np.float64)
    scale = 128.0 ** -0.5
    i = np.arange(128)
    MT = np.zeros((128, H, 128), dtype=np.float64)
    ci, cj = i[None, :] // 64, i[:, None] // 64
    dist = np.abs(i[None, :] - i[:, None]).astype(np.float64)
    allowed = (ci >= cj)
    for h in range(H):
        MT[:, h, :] = np.where(allowed, gam[h] ** dist, 0.0) * scale
    qdecT = np.broadcast_to((gam[None, :, None] ** (i[None, None, :] + 1.0)), (128, H, 128))
    kdec = (gam[None, :] ** (127.0 - i[:, None])) * scale
    kdecP = np.zeros((128, NPRE, H), dtype=np.float64)
    P0 = seg * 1024
    for p in range(NPRE):
        gt = seg * 8 - NPRE + p
        if gt < 0:
            continue
        tok = gt * 128 + i
        kdecP[:, p, :] = (gam[None, :] ** (P0 - 1.0 - tok[:, None])) * scale
    f32 = lambda a: np.ascontiguousarray(a, dtype=np.float32)
    return f32(MT), f32(qdecT), f32(kdec), f32(kdecP)


def _in_maps(inputs):
    x = np.asarray(inputs["x"], dtype=np.float32)
    c = np.asarray(inputs["c"], dtype=np.float32)
    positions = np.asarray(inputs["positions"]).astype(np.int32)
    g = lambda k: np.asarray(inputs[k], dtype=np.float32)
    shared = {
        "w_ada": np.ascontiguousarray(g("w_ada")[0]),
        "b_ada": np.ascontiguousarray(g("b_ada")[0][None, :]),
        "norm1_g": np.ascontiguousarray(g("norm1_g")[0][None, :]),
        "w_in": np.ascontiguousarray(g("w_in")[0]),
        "w_bg": np.ascontiguousarray(g("w_branch_gate")[0]),
        "b_bg": np.ascontiguousarray(g("b_branch_gate")[0][None, :]),
        "gm_v_g": np.ascontiguousarray(g("gm_v_g")[0][None, :]),
        "wsT": np.ascontiguousarray(g("gm_ws")[0].transpose(2, 0, 1)),
        "gm_bT": np.ascontiguousarray(g("gm_b")[0].T),
        "ret_gn_g": np.ascontiguousarray(g("ret_gn_g")[0][None, :]),
        "w_a": np.ascontiguousarray(g("w_a_out")[0]),
        "w_b": np.ascontiguousarray(g("w_b_out")[0]),
        "w_o": np.ascontiguousarray(g("w_o")[0]),
        "norm2_g": np.ascontiguousarray(g("norm2_g")[0][None, :]),
        "wq": np.ascontiguousarray(g("peer_wq")[0]),
        "subkT": np.ascontiguousarray(g("peer_subkeys")[0].reshape(16, 128, 128).transpose(2, 0, 1)),
        "peer_u": np.ascontiguousarray(g("peer_u")[0]),
        "peer_v": np.ascontiguousarray(g("peer_v")[0]),
        "norm_f_g": np.ascontiguousarray(g("norm_f_g")[None, :]),
        "ident": np.eye(128, dtype=np.float32),
        "freq": np.ascontiguousarray(np.broadcast_to((10000.0 ** (-np.arange(64, dtype=np.float32) / 64)).astype(np.float32)[None, :], (128, 64))),
        "iota16": np.ascontiguousarray(np.broadcast_to(np.arange(16, dtype=np.float32)[None, :], (128, 16))),
    }
    maps = []
    for core in range(8):
        b, seg = core // 4, core % 4
        xsl = np.zeros((NPRE + NOWN, 128, D), dtype=np.float32)
        pos = np.zeros((128, NPRE + NOWN), dtype=np.int32)
        for p in range(NPRE + NOWN):
            gt = seg * 8 - NPRE + p
            if gt < 0:
                continue
            xsl[p] = x[b, gt * 128:(gt + 1) * 128]
            pos[:, p] = positions[b, gt * 128:(gt + 1) * 128]
        MT, qdecT, kdec, kdecP = _consts(seg)
        m = dict(shared)
        m.update({"xs": xsl, "posi": pos, "cT": np.ascontiguousarray(c[b].reshape(16, 128).T),
                  "MT": MT, "qdecT": qdecT, "kdec": kdec, "kdecP": kdecP})
        maps.append(m)
    return maps


_DBG = bool(int(os.environ.get("KDBG", "0")))
_last = {}


def kernel(**inputs):
    nc = build_program(dbg=_DBG)
    maps = _in_maps(inputs)
    res = run_bass_kernel_spmd(nc, maps, core_ids=list(range(8)))
    out = np.zeros((2, 4096, D), dtype=np.float32)
    for core in range(8):
        b, seg = core // 4, core % 4
        out[b, seg * 1024:(seg + 1) * 1024] = res.results[core]["out"].reshape(1024, D)
    if _DBG:
        _last["res"] = res.results
    return out
```

```python
import os
from contextlib import ExitStack
import numpy as np
import concourse.bass as bass
import concourse.mybir as mybir
from concourse.bass_utils import run_bass_kernel_spmd

F32 = mybir.dt.float32
BF16 = mybir.dt.bfloat16
I32 = mybir.dt.int32
U32 = mybir.dt.uint32
AF = mybir.ActivationFunctionType
ALU = mybir.AluOpType
AX = mybir.AxisListType

D = 2048
NPRE = 24
NOWN = 8
EPS = 1e-6
H = 8
ENGS = ("pe", "act", "dve", "pool", "sp")
NDMA_SEMS = 28
TWO_PI = float(2 * np.pi)


class Sched:
    def __init__(self, nc, stack):
        self.nc = nc
        self.stack = stack
        self.ops = {e: [] for e in ENGS}
        self.cnt = {e: 0 for e in ENGS}
        self.sem = {e: stack.enter_context(nc.semaphore("s_" + e)) for e in ENGS if e != "sp"}
        self.dsem = [stack.enter_context(nc.semaphore("d%d" % i)) for i in range(NDMA_SEMS)]
        self.dcnt = [0] * NDMA_SEMS
        half = NDMA_SEMS // 2
        self.dpool = {"sp": list(range(0, half)), "act": list(range(0, half)), "pool": list(range(half, NDMA_SEMS))}
        self.dnext = {"sp": 0, "act": 0, "pool": 0}
        self.waited = {e: {} for e in ENGS}
        self.lastw = {}
        self.readers = {}
        self.alias = {}

    def _x(self, names):
        out = []
        for n in names:
            out.extend(self.alias.get(n, [n]))
        return out

    def sb(self, name, shape, dtype=F32):
        return self.stack.enter_context(self.nc.sbuf_tensor("sb_" + name, list(shape), dtype))

    def ps(self, name, shape, dtype=F32):
        return self.stack.enter_context(self.nc.psum_tensor("ps_" + name, list(shape), dtype))

    def _semobj(self, key):
        return self.sem[key] if isinstance(key, str) else self.dsem[key]

    def _deps(self, eng, reads, writes):
        need = {}

        def add(k, v, same_ok):
            if k == eng and not same_ok and eng == "pe":
                return
            if need.get(k, 0) < v:
                need[k] = v

        for r in reads:
            if r in self.lastw:
                k, v = self.lastw[r]
                add(k, v, True)
        for w in writes:
            if w in self.lastw:
                k, v = self.lastw[w]
                add(k, v, False)
            for k, v in self.readers.get(w, {}).items():
                add(k, v, False)
        waits = []
        wd = self.waited[eng]
        for k, v in need.items():
            if wd.get(k, 0) >= v:
                continue
            wd[k] = v
            waits.append((k, v))
        return waits

    def _record(self, key, val, reads, writes):
        for r in reads:
            self.readers.setdefault(r, {})[key] = val
        for w in writes:
            self.lastw[w] = (key, val)
            self.readers[w] = {}

    def op(self, eng, fn, reads=(), writes=()):
        reads, writes = self._x(reads), self._x(writes)
        waits = self._deps(eng, reads, writes)
        self.cnt[eng] += 1
        self.ops[eng].append((waits, fn, (eng, 1)))
        self._record(eng, self.cnt[eng], reads, writes)

    def dma(self, fn, reads=(), writes=(), queue="sp"):
        reads, writes = self._x(reads), self._x(writes)
        qp = self.dpool[queue]
        i = qp[self.dnext[queue] % len(qp)]
        self.dnext[queue] += 1
        waits = self._deps(queue, reads, writes)
        if self.dcnt[i] > 0 and self.waited[queue].get(i, 0) < self.dcnt[i]:
            self.waited[queue][i] = self.dcnt[i]
            waits.append((i, self.dcnt[i]))
        self.dcnt[i] += 16
        self.ops[queue].append((waits, fn, (i, 16)))
        self._record(i, self.dcnt[i], reads, writes)

    def final_wait(self, eng, reslist):
        waits = self._deps(eng, self._x(reslist), ())
        self.ops[eng].append((waits, None, None))

    def emit(self):
        nc = self.nc
        with nc.Block() as block:
            def run(ename):
                def body(e):
                    for waits, fn, inc in self.ops[ename]:
                        for k, v in waits:
                            e.wait_ge(self._semobj(k), v)
                        if fn is None:
                            continue
                        ins = fn(e)
                        ins.then_inc(self._semobj(inc[0]), inc[1])
                return body
            block.tensor(run("pe"))
            block.scalar(run("act"))
            block.vector(run("dve"))
            block.gpsimd(run("pool"))
            block.sync(run("sp"))


def build_program(dbg=False):
    nc = bass.Bass("TRN2", target_bir_lowering=False)

    def din(name, shape, dt=F32):
        return nc.dram_tensor(name, list(shape), dt, kind="ExternalInput").ap()

    xs = din("xs", [NPRE + NOWN, 128, D])
    posi = din("posi", [128, NPRE + NOWN], I32)
    cT_d = din("cT", [128, 16])
    w_ada = din("w_ada", [D, 6 * D])
    b_ada = din("b_ada", [1, 6 * D])
    norm1_g = din("norm1_g", [1, D])
    w_in = din("w_in", [D, 8192])
    w_bg = din("w_bg", [D, 4096])
    b_bg = din("b_bg", [1, 4096])
    gm_v_g = din("gm_v_g", [1, 1024])
    wsT_d = din("wsT", [128, 8, 128])
    gm_bT_d = din("gm_bT", [128, 8])
    ret_gn_g = din("ret_gn_g", [1, D])
    w_a = din("w_a", [1024, D])
    w_b = din("w_b", [D, D])
    w_o = din("w_o", [D, D])
    norm2_g = din("norm2_g", [1, D])
    wq = din("wq", [D, D])
    subkT_d = din("subkT", [128, 16, 128])
    peer_u = din("peer_u", [16384, D])
    peer_v = din("peer_v", [16384, D])
    norm_f_g = din("norm_f_g", [1, D])
    ident_d = din("ident", [128, 128])
    freq_d = din("freq", [128, 64])
    iota16_d = din("iota16", [128, 16])
    MT_d = din("MT", [128, 8, 128])
    qdecT_d = din("qdecT", [128, 8, 128])
    kdec_d = din("kdec", [128, 8])
    kdecP_d = din("kdecP", [128, NPRE, 8])
    out_d = nc.dram_tensor("out", [NOWN, 128, D], F32, kind="ExternalOutput").ap()
    ada_d = nc.dram_tensor("ada_scr", [1, 6 * D], F32, kind="Internal").ap()
    WSPEC = {"w_in": (w_in, 16, 16), "w_bg": (w_bg, 16, 8), "w_a": (w_a, 8, 4), "w_b": (w_b, 16, 4), "w_o": (w_o, 16, 4), "wq": (wq, 16, 4)}
    wscr = {k: nc.dram_tensor("wb_" + k, [v[2], 128, v[1] * 512], BF16, kind="Internal").ap() for k, v in WSPEC.items()}
    if dbg:
        dbg_x1 = nc.dram_tensor("dbg_x1", [NOWN, 128, D], F32, kind="ExternalOutput").ap()
        dbg_y = nc.dram_tensor("dbg_y", [NOWN, 128, D], F32, kind="ExternalOutput").ap()

    GAM = [1.0 - 2.0 ** (-5.0 - h) for h in range(H)]

    with ExitStack() as st:
        S = Sched(nc, st)
        sb, ps = S.sb, S.ps

        ident = sb("ident", [128, 128]); identb = sb("identb", [128, 128], BF16)
        freq = sb("freq", [128, 64]); iota16 = sb("iota16", [128, 16])
        MT = sb("MT", [128, 8, 128]); qdecT = sb("qdecT", [128, 8, 128])
        kdec = sb("kdec", [128, 8]); kdecP = sb("kdecP", [128, NPRE, 8])
        wsT = sb("wsT", [128, 8, 128], BF16)
        gmbT = sb("gmbT", [128, 8]); subkT = sb("subkT", [128, 16, 128])
        posI = sb("posI", [128, NPRE + NOWN], I32); posF = sb("posF", [128, NPRE + NOWN])
        cT = sb("cT", [128, 16]); scT = sb("scT", [128, 16], BF16)

        NW = 3
        wring = [sb("w%d" % i, [128, 8192], BF16) for i in range(NW)]
        wr_i = [0]

        def wnext():
            i = wr_i[0]; wr_i[0] = (i + 1) % NW
            return wring[i], ["w%da" % i, "w%db" % i]

        NBC = 2
        bcring = [sb("bc%d" % i, [128, D]) for i in range(NBC)]
        bc_i = [0]

        def bcnext():
            i = bc_i[0]; bc_i[0] = (i + 1) % NBC
            return bcring[i], "bc%d" % i

        NP = 4
        pring = [ps("P%d" % i, [128, 1024]) for i in range(NP)]
        p_i = [0]

        def pnext():
            i = p_i[0]; p_i[0] = (i + 1) % NP
            return pring[i], "P%d" % i

        xt = sb("xt", [128, D])
        ss = sb("ss", [128, 8]); rstd = sb("rstd", [128, 8])
        hb = sb("hb", [128, D], BF16); hT = sb("hT", [128, 16, 128], BF16)
        junk = hb; S.alias["junk"] = ["hb"]
        cs = sb("cs", [128, 4, 64])
        ki = sb("ki", [128, 64], I32); kf = sb("kf", [128, 2, 64])
        pi32 = sb("pi32", [128, 8, 16], I32); pcor = sb("pcor", [128, 8, 16])
        Sf = sb("Sf", [128, 8, 256]); Sb = sb("Sb", [128, 8, 256], BF16)
        st8 = sb("st8", [128, 4, 8])
        wk = sb("wk", [128, 256])
        v12 = sb("v12", [128, 16, 16]); i12 = sb("i12", [128, 16, 16], U32); i12f = sb("i12f", [128, 16, 16])
        top = sb("top", [128, 8, 16]); pos = sb("pos", [128, 8, 16], U32)
        paf = sb("paf", [128, 8, 16]); pbf = sb("pbf", [128, 8, 16])
        i1s = sb("i1s", [128, 8, 16]); i2s = sb("i2s", [128, 8, 16])
        eidx = sb("eidx", [128, 128], I32)
        gate = sb("gate", [128, 8, 16]); gsum = sb("gsum", [128, 8])
        acol = sb("acol", [128, 128]); wgt = sb("wgt", [128, 128])

        ARENA = 80 * 1024
        GRAN = 2048
        arena = sb("arena", [128, ARENA // 4])
        DSZ = {F32: 4, BF16: 2, I32: 4, U32: 4}

        def carve(off, name, shape, dtype=F32, parts=128):
            nel = int(np.prod(shape[1:]))
            nbytes = nel * DSZ[dtype]
            assert off % 4 == 0 and off + nbytes <= ARENA, (name, off, nbytes)
            v = arena[0:parts, off // 4:(off + nbytes) // 4]
            if dtype != F32:
                v = v.bitcast(dtype)
            if len(shape) == 3:
                v = v.rearrange("p (a b) -> p a b", a=shape[1])
            S.alias[name] = ["ar%d" % g for g in range(off // GRAN, (off + nbytes + GRAN - 1) // GRAN)]
            return v, off + ((nbytes + GRAN - 1) // GRAN) * GRAN

        K = 1024
        sg, o = carve(0, "sg", [128, D])
        rotA, o = carve(o, "rotA", [128, 8, 128]); rotB, o2_off = carve(o, "rotB", [128, 8, 128])
        o_sb, _ = carve(o - 4 * K, "o_sb", [128, 8, 256])
        o = o2_off
        u_sb, o = carve(o, "u_sb", [128, 8, 128]); v_f, o = carve(o, "v_f", [128, 8, 128])
        o2, _ = carve(o - 8 * K, "o2", [128, 8, 256])
        qb, o = carve(o, "qb", [128, 8, 128], BF16); kb, o = carve(o, "kb", [128, 8, 128], BF16)
        kd, o = carve(o, "kd", [128, 8, 128], BF16); qT, o = carve(o, "qT", [128, 8, 128], BF16)
        qdT, o = carve(o, "qdT", [128, 8, 128], BF16); kT, o = carve(o, "kT", [128, 8, 128], BF16)
        vr, o = carve(o, "vr", [128, 8, 256], BF16)
        v_b, o = carve(o, "v_b", [128, 8, 128], BF16); preA, o = carve(o, "preA", [128, 8, 128], BF16)
        preAT, o = carve(o, "preAT", [128, 8, 128], BF16); attm, o = carve(o, "attm", [128, 8, 128], BF16)
        retb, o = carve(o, "retb", [128, D], BF16); retT, o = carve(o, "retT", [128, 16, 128], BF16)
        mb, o = carve(o, "mb", [128, D], BF16); mT, o = carve(o, "mT", [128, 16, 128], BF16)
        gA, o = carve(o, "gA", [128, 512]); gB, o = carve(o, "gB", [128, 512])
        xt2, _ = carve(40 * K, "xt2", [128, D]); hb2, _ = carve(48 * K, "hb2", [128, D], BF16)
        hT2, _ = carve(52 * K, "hT2", [128, 16, 128], BF16)
        A1p, _ = carve(56 * K, "A1p", [128, D]); sh1p, _ = carve(64 * K, "sh1p", [128, D])
        cvS = [carve(0, "cvS0", [128, 8, 512], BF16)[0], carve(16 * K, "cvS1", [128, 8, 512], BF16)[0]]
        brow, _ = carve(0, "brow", [1, 512], parts=1); arow, _ = carve(2 * K, "arow", [1, 512], parts=1)
        h2, o = carve(0, "h2", [128, D]); y, o = carve(o, "y", [128, D])
        qpT, o = carve(o, "qpT", [128, 16, 128]); sc_sb, o = carve(o, "sc_sb", [128, 16, 128])
        cand, o = carve(o, "cand", [128, 8, 256]); oh, o = carve(o, "oh", [128, 8, 256])
        NG = 4
        gring = []
        for gi in range(NG):
            gv, o = carve(o, "g%d" % gi, [128, D])
            gring.append(gv)
        g_i = [0]

        def gnext():
            i = g_i[0]; g_i[0] = (i + 1) % NG
            return gring[i], "g%d" % i

        def V(fn, r, w):
            S.op("dve", fn, r, w)

        def A(fn, r, w):
            S.op("act", fn, r, w)

        def P(fn, r, w):
            S.op("pe", fn, r, w)

        def G(fn, r, w):
            S.op("pool", fn, r, w)

        def ld(out_ap, in_ap, w, queue="sp", r=()):
            S.dma(lambda e: e.dma_start(out=out_ap, in_=in_ap), reads=r, writes=w, queue=queue)

        def bcload(src_row, r=()):
            t, n = bcnext()
            width = src_row.shape[1]
            ld(t[:, 0:width], src_row.partition_broadcast(128)[:, 0, :], [n], r=r)
            return t, n

        def adaload(off):
            return bcload(ada_d[0:1, off:off + D], r=["ada%d" % k for k in range(off // 512, off // 512 + 4)])

        def wload_cast(src, K, N=512):
            t, n = wnext()
            view = t[:, 0:K * N].rearrange("p (k n) -> p k n", k=K)
            ld(view, src.rearrange("(k p) n -> p k n", p=128), n, queue="pool")
            return view, n

        def wload(name, j):
            K_ = WSPEC[name][1]
            t, n = wnext()
            ld(t[:, 0:K_ * 512], wscr[name][j], n, r=wb_res[(name, j)])
            return t[:, 0:K_ * 512].rearrange("p (k n) -> p k n", k=K_), n

        ld(ident[:], ident_d, ["ident"]); ld(freq[:], freq_d, ["freq"]); ld(iota16[:], iota16_d, ["iota16"])
        ld(MT[:], MT_d, ["MT"]); ld(qdecT[:], qdecT_d, ["qdecT"]); ld(kdec[:], kdec_d, ["kdec"])
        ld(kdecP[:], kdecP_d, ["kdecP"]); ld(wsT[:], wsT_d, ["wsT"], queue="pool"); ld(gmbT[:], gm_bT_d, ["gmbT"])
        ld(subkT[:], subkT_d, ["subkT"]); ld(posI[:], posi, ["posI"]); ld(cT[:], cT_d, ["cT"])
        V(lambda e: e.tensor_copy(identb[:], ident[:]), ["ident"], ["identb"])
        V(lambda e: e.tensor_copy(posF[:], posI[:]), ["posI"], ["posF"])
        V(lambda e: e.memset(wsT[64:128, :, 0:64], 0.0), ["wsT"], ["wsT"])
        V(lambda e: e.memset(Sf[:], 0.0), [], ["Sf"])

        wb_res = {}
        EARLY = [("w_in", j) for j in range(6, 12)]
        for name, j in EARLY:
            wsrc_, K_, nch = WSPEC[name]
            view, n = wload_cast(wsrc_[:, j * 512:(j + 1) * 512], K_)
            wb_res[(name, j)] = ["wb_%s_%d" % (name, j)]
            ld(wscr[name][j], view.rearrange("p k n -> p (k n)"), wb_res[(name, j)], r=n)
        late_jobs = []
        for name, (wsrc_, K_, nch) in WSPEC.items():
            for j in range(nch):
                if (name, j) in EARLY:
                    continue
                wb_res[(name, j)] = ["wb_%s_%d_%d" % (name, j, h) for h in range(K_ // 8)]
                for h in range(K_ // 8):
                    late_jobs.append((name, j, h))
        cv_i = [0]

        def emit_late(count):
            for _ in range(count):
                if not late_jobs:
                    return
                name, j, h = late_jobs.pop(0)
                wsrc_ = WSPEC[name][0]
                buf = cvS[cv_i[0] % 2]; bn = "cvS%d" % (cv_i[0] % 2); cv_i[0] += 1
                ld(buf[:], wsrc_[h * 1024:(h + 1) * 1024, j * 512:(j + 1) * 512].rearrange("(k p) n -> p k n", p=128), [bn], queue="pool")
                ld(wscr[name][j][:, h * 4096:(h + 1) * 4096], buf[:].rearrange("p k n -> p (k n)"), ["wb_%s_%d_%d" % (name, j, h)], r=[bn])

        A(lambda e: e.activation(scT[:], cT[:], AF.Silu), ["cT"], ["scT"])
        for n in range(24):
            pt, pn = pnext()
            wv, wn = wload_cast(w_ada[:, n * 512:(n + 1) * 512], 16)
            for kc in range(16):
                P(lambda e, wv=wv, kc=kc, pt=pt: e.matmul(pt[0:1, 0:512], scT[:, kc:kc + 1], wv[:, kc, :],
                                                          start=(kc == 0), stop=(kc == 15)), ["scT"] + wn, [pn])
            ld(brow[:], b_ada[0:1, n * 512:(n + 1) * 512], ["brow"])
            V(lambda e, pt=pt: e.tensor_tensor(arow[:], pt[0:1, 0:512], brow[:], op=ALU.add), [pn, "brow"], ["arow"])
            ld(ada_d[0:1, n * 512:(n + 1) * 512], arow[:], ["ada%d" % n], r=["arow"])

        def load_x_norm(tile_idx, g_row, sc_off, sh_off, out_f32=None):
            ld(xt[:], xs[tile_idx], ["xt"])
            norm_from(xt, "xt", g_row, sc_off, sh_off, out_f32)

        def prefix_norm(tile_idx, par):
            x_, xn_, hb_, hbn_, hT_, hTn_ = (xt, "xt", hb, "hb", hT, "hT") if par == 0 else (xt2, "xt2", hb2, "hb2", hT2, "hT2")
            ld(x_[:], xs[tile_idx], [xn_])
            c0 = 4 * par
            A(lambda e: e.activation(hb_[:], x_[:], AF.Square, accum_out=ss[:, c0:c0 + 1]), [xn_], [hbn_, "ss"])
            V(lambda e: e.tensor_scalar(rstd[:, c0:c0 + 1], ss[:, c0:c0 + 1], 1.0 / D, EPS, op0=ALU.mult, op1=ALU.add), ["ss"], ["rstd"])
            A(lambda e: e.activation(rstd[:, c0 + 1:c0 + 2], rstd[:, c0:c0 + 1], AF.Sqrt), ["rstd"], ["rstd"])
            V(lambda e: e.reciprocal(rstd[:, c0 + 2:c0 + 3], rstd[:, c0 + 1:c0 + 2]), ["rstd"], ["rstd"])
            tmp, tmpn = bcnext()
            V(lambda e: e.scalar_tensor_tensor(tmp[:], x_[:], rstd[:, c0 + 2:c0 + 3], A1p[:], op0=ALU.mult, op1=ALU.mult), [xn_, "rstd", "A1p"], [tmpn])
            V(lambda e: e.tensor_tensor(hb_[:], tmp[:], sh1p[:], op=ALU.add), [tmpn, "sh1p"], [hbn_])
            transpose16(hb_, hbn_, hT_, hTn_)
            return hT_, hTn_

        def norm_from(src, srcn, g_row, sc_off, sh_off, out_f32=None):
            A(lambda e: e.activation(junk[:], src[:], AF.Square, accum_out=ss[:, 0:1]), [srcn], ["junk", "ss"])
            V(lambda e: e.tensor_scalar(rstd[:, 0:1], ss[:, 0:1], 1.0 / D, EPS, op0=ALU.mult, op1=ALU.add), ["ss"], ["rstd"])
            A(lambda e: e.activation(rstd[:, 1:2], rstd[:, 0:1], AF.Sqrt), ["rstd"], ["rstd"])
            V(lambda e: e.reciprocal(rstd[:, 2:3], rstd[:, 1:2]), ["rstd"], ["rstd"])
            gt, gn = bcload(g_row)
            sct, scn = adaload(sc_off)
            V(lambda e: e.scalar_tensor_tensor(sct[:], sct[:], 1.0, gt[:], op0=ALU.add, op1=ALU.mult), [gn, scn], [scn])
            sht, shn = adaload(sh_off)
            V(lambda e: e.scalar_tensor_tensor(sct[:], src[:], rstd[:, 2:3], sct[:], op0=ALU.mult, op1=ALU.mult), [srcn, "rstd", scn], [scn])
            if out_f32 is not None:
                V(lambda e: e.tensor_tensor(out_f32[0][:], sct[:], sht[:], op=ALU.add), [scn, shn], [out_f32[1]])
                A(lambda e: e.copy(hb[:], out_f32[0][:]), [out_f32[1]], ["hb"])
            else:
                V(lambda e: e.tensor_tensor(hb[:], sct[:], sht[:], op=ALU.add), [scn, shn], ["hb"])
            transpose16(hb, "hb", hT, "hT")

        def transpose16(src, srcn, dst, dstn, nchunks=16):
            for half in range((nchunks + 7) // 8):
                pt, pn = pnext()
                pv = pt[:].bitcast(BF16)
                cnt = min(8, nchunks - half * 8)
                for j in range(cnt):
                    c = half * 8 + j
                    P(lambda e, pv=pv, j=j, c=c: e.transpose(pv[:, j * 128:(j + 1) * 128], src[:, c * 128:(c + 1) * 128], identb[:]),
                      [srcn, "identb"], [pn])
                A(lambda e, pv=pv, half=half, cnt=cnt: e.copy(dst[:, half * 8:half * 8 + cnt, :].rearrange("p a b -> p (a b)"), pv[:, 0:cnt * 128]),
                  [pn], [dstn])

        def rope_tables(tile_idx):
            pcol = posF[:, tile_idx:tile_idx + 1]
            PI = float(np.pi)
            for (shift, dsti) in ((0.0, 1), (PI / 2, 0)):
                V(lambda e, shift=shift: e.tensor_scalar(cs[:, 3, :], freq[:], pcol, shift, op0=ALU.mult, op1=ALU.add), ["freq", "posF", "cs"], ["cs3"])
                V(lambda e: e.tensor_scalar(ki[:], cs[:, 3, :], 1.0 / TWO_PI, None, op0=ALU.mult), ["cs3"], ["ki"])
                V(lambda e: e.tensor_copy(kf[:, 0, :], ki[:]), ["ki"], ["kf"])
                V(lambda e: e.scalar_tensor_tensor(cs[:, 3, :], kf[:, 0, :], -TWO_PI, cs[:, 3, :], op0=ALU.mult, op1=ALU.add), ["kf", "cs3"], ["cs3"])
                V(lambda e: e.tensor_scalar(kf[:, 1, :], cs[:, 3, :], PI, -TWO_PI, op0=ALU.is_gt, op1=ALU.mult), ["cs3"], ["kf"])
                V(lambda e: e.tensor_tensor(cs[:, 3, :], cs[:, 3, :], kf[:, 1, :], op=ALU.add), ["kf", "cs3"], ["cs3"])
                V(lambda e: e.tensor_scalar(cs[:, 3, :], cs[:, 3, :], -PI, PI, op0=ALU.max, op1=ALU.min), ["cs3"], ["cs3"])
                A(lambda e, dsti=dsti: e.activation(cs[:, dsti, :], cs[:, 3, :], AF.Sin), ["cs3"], ["cs"])
            V(lambda e: e.tensor_scalar(cs[:, 2, :], cs[:, 1, :], -1.0, None, op0=ALU.mult), ["cs"], ["cs"])

        def rope(pt, pn, nh, dst, dstn):
            pv = pt[:, 0:nh * 128].rearrange("p (h d) -> p h d", h=nh)
            cosb = cs[:, 0, :].unsqueeze(1).to_broadcast([128, nh, 64])
            sinb = cs[:, 1, :].unsqueeze(1).to_broadcast([128, nh, 64])
            nsinb = cs[:, 2, :].unsqueeze(1).to_broadcast([128, nh, 64])
            V(lambda e: e.tensor_tensor(rotA[:, 0:nh, 0:64], pv[:, :, 0:64], cosb, op=ALU.mult), [pn, "cs"], ["rotA"])
            V(lambda e: e.tensor_tensor(rotA[:, 0:nh, 64:128], pv[:, :, 64:128], cosb, op=ALU.mult), [pn, "cs"], ["rotA"])
            V(lambda e: e.tensor_tensor(rotB[:, 0:nh, 0:64], pv[:, :, 64:128], nsinb, op=ALU.mult), [pn, "cs"], ["rotB"])
            V(lambda e: e.tensor_tensor(rotB[:, 0:nh, 64:128], pv[:, :, 0:64], sinb, op=ALU.mult), [pn, "cs"], ["rotB"])
            V(lambda e: e.tensor_tensor(dst[:, 0:nh, :], rotA[:, 0:nh, :], rotB[:, 0:nh, :], op=ALU.add), ["rotA", "rotB"], [dstn])

        def proj(pt, pn, col, wname, c0, K=16, lhs=None, lhsn="hT"):
            lhs = hT if lhs is None else lhs
            wv, wn = wload(wname, c0 // 512)
            for kc in range(K):
                P(lambda e, wv=wv, kc=kc: e.matmul(pt[:, col * 512:(col + 1) * 512], lhs[:, kc, :], wv[:, kc, :],
                                                   start=(kc == 0), stop=(kc == K - 1)), [lhsn] + wn, [pn])

        gt_, gn_ = bcload(norm1_g)
        ld(A1p[:], ada_d[0:1, D:2 * D].partition_broadcast(128)[:, 0, :], ["A1p"], r=["ada%d" % k for k in range(4, 8)])
        V(lambda e: e.scalar_tensor_tensor(A1p[:], A1p[:], 1.0, gt_[:], op0=ALU.add, op1=ALU.mult), ["A1p", gn_], ["A1p"])
        ld(sh1p[:], ada_d[0:1, 0:D].partition_broadcast(128)[:, 0, :], ["sh1p"], r=["ada%d" % k for k in range(0, 4)])
        for hh in range(2):
            wk_v, wk_n = wload("w_in", 6 + hh)
            wv0, wv0n = wload("w_in", 8 + hh * 2)
            wv1, wv1n = wload("w_in", 9 + hh * 2)
            for p in range(NPRE):
                hT_, hTn_ = prefix_norm(p, p % 2)
                rope_tables(p)
                pk, pkn = pnext()
                for kc in range(16):
                    P(lambda e, kc=kc, pk=pk, hT_=hT_: e.matmul(pk[:, 0:512], hT_[:, kc, :], wk_v[:, kc, :], start=(kc == 0), stop=(kc == 15)),
                      [hTn_] + wk_n, [pkn])
                pv_, pvn = pnext()
                for j, (wv, wn) in enumerate(((wv0, wv0n), (wv1, wv1n))):
                    for kc in range(16):
                        P(lambda e, kc=kc, j=j, wv=wv, pv_=pv_, hT_=hT_: e.matmul(pv_[:, j * 512:(j + 1) * 512], hT_[:, kc, :], wv[:, kc, :],
                                                                         start=(kc == 0), stop=(kc == 15)), [hTn_] + wn, [pvn])
                rope(pk, pkn, 4, kb, "kb")
                V(lambda e, p=p, hh=hh: e.tensor_tensor(kd[:, 0:4, :], kb[:, 0:4, :],
                                                        kdecP[:, p, hh * 4:(hh + 1) * 4].unsqueeze(2).to_broadcast([128, 4, 128]), op=ALU.mult),
                  ["kb", "kdecP"], ["kd"])
                A(lambda e, pv_=pv_: e.copy(vr[:, 0:4, :].rearrange("p a b -> p (a b)"), pv_[:, :]), [pvn], ["vr"])
                pst, pstn = pnext()
                for h4 in range(4):
                    P(lambda e, h4=h4, pst=pst: e.matmul(pst[:, h4 * 256:(h4 + 1) * 256], kd[:, h4, :], vr[:, h4, :], start=True, stop=True),
                      ["kd", "vr"], [pstn])
                V(lambda e, hh=hh, pst=pst: e.tensor_tensor(Sf[:, hh * 4:(hh + 1) * 4, :].rearrange("p a b -> p (a b)"),
                                                            Sf[:, hh * 4:(hh + 1) * 4, :].rearrange("p a b -> p (a b)"), pst[:, :], op=ALU.add),
                  [pstn, "Sf"], ["Sf"])
                emit_late(2)
        emit_late(len(late_jobs))
        A(lambda e: e.copy(Sb[:], Sf[:]), ["Sf"], ["Sb"])

        for i in range(NOWN):
            ti = NPRE + i
            load_x_norm(ti, norm1_g, 1 * D, 0)
            rope_tables(ti)
            pu, pun = pnext(); proj(pu, pun, 0, "w_in", 0); proj(pu, pun, 1, "w_in", 512)
            A(lambda e, pu=pu: e.activation(u_sb[:].rearrange("p a b -> p (a b)"), pu[:, :], AF.Gelu), [pun], ["u_sb"])
            pvv, pvvn = pnext(); proj(pvv, pvvn, 0, "w_in", 1024); proj(pvv, pvvn, 1, "w_in", 1536)
            A(lambda e, pvv=pvv: e.activation(v_f[:].rearrange("p a b -> p (a b)"), pvv[:, :], AF.Gelu), [pvvn], ["v_f"])
            V(lambda e: e.tensor_tensor(rotA[:], v_f[:], v_f[:], op=ALU.mult), ["v_f"], ["rotA"])
            V(lambda e: e.tensor_reduce(ss[:, 0:8], rotA[:], axis=AX.X, op=ALU.add), ["rotA"], ["ss"])
            V(lambda e: e.tensor_scalar(rstd[:, 0:8], ss[:, 0:8], 1.0 / 128, EPS, op0=ALU.mult, op1=ALU.add), ["ss"], ["rstd"])
            A(lambda e: e.activation(ss[:, 0:8], rstd[:, 0:8], AF.Sqrt), ["rstd"], ["ss"])
            V(lambda e: e.reciprocal(rstd[:, 0:8], ss[:, 0:8]), ["ss"], ["rstd"])
            vg, vgn = bcload(gm_v_g)
            V(lambda e: e.tensor_tensor(rotA[:], v_f[:], rstd[:, 0:8].unsqueeze(2).to_broadcast([128, 8, 128]), op=ALU.mult), ["v_f", "rstd"], ["rotA"])
            V(lambda e, vg=vg: e.tensor_tensor(v_b[:].rearrange("p a b -> p (a b)"), rotA[:].rearrange("p a b -> p (a b)"), vg[:, 0:1024], op=ALU.mult),
              ["rotA", vgn], ["v_b"])
            psg, psgn = pnext()
            for g in range(8):
                P(lambda e, g=g, psg=psg: e.matmul(psg[:, g * 128:(g + 1) * 128], wsT[:, g, :], v_b[:, g, :], start=True, stop=True),
                  ["wsT", "v_b"], [psgn])
            V(lambda e, psg=psg: e.tensor_tensor(rotA[:], psg[:, :].rearrange("p (a b) -> p a b", a=8),
                                                 gmbT[:, :].unsqueeze(2).to_broadcast([128, 8, 128]), op=ALU.add), [psgn, "gmbT"], ["rotA"])
            V(lambda e: e.tensor_tensor(preA[:], rotA[:], u_sb[:], op=ALU.mult), ["rotA", "u_sb"], ["preA"])
            transpose16(preA[:].rearrange("p a b -> p (a b)"), "preA", preAT, "preAT", nchunks=8)
            pq, pqn = pnext(); proj(pq, pqn, 0, "w_in", 2048); proj(pq, pqn, 1, "w_in", 2560)
            rope(pq, pqn, 8, qb, "qb")
            pk, pkn = pnext(); proj(pk, pkn, 0, "w_in", 3072); proj(pk, pkn, 1, "w_in", 3584)
            rope(pk, pkn, 8, kb, "kb")
            V(lambda e: e.tensor_tensor(kd[:], kb[:], kdec[:, :].unsqueeze(2).to_broadcast([128, 8, 128]), op=ALU.mult), ["kb", "kdec"], ["kd"])
            pt, pn = pnext(); pv = pt[:].bitcast(BF16)
            for h in range(8):
                P(lambda e, h=h, pv=pv: e.transpose(pv[:, h * 128:(h + 1) * 128], qb[:, h, :], identb[:]), ["qb", "identb"], [pn])
            A(lambda e, pv=pv: e.copy(qT[:].rearrange("p a b -> p (a b)"), pv[:, 0:1024]), [pn], ["qT"])
            V(lambda e, pv=pv: e.tensor_tensor(qdT[:].rearrange("p a b -> p (a b)"), pv[:, 0:1024], qdecT[:].rearrange("p a b -> p (a b)"), op=ALU.mult),
              [pn, "qdecT"], ["qdT"])
            pt, pn = pnext(); pv = pt[:].bitcast(BF16)
            for h in range(8):
                P(lambda e, h=h, pv=pv: e.transpose(pv[:, h * 128:(h + 1) * 128], kb[:, h, :], identb[:]), ["kb", "identb"], [pn])
            A(lambda e, pv=pv: e.copy(kT[:].rearrange("p a b -> p (a b)"), pv[:, 0:1024]), [pn], ["kT"])
            for j in range(2):
                pvr, pvrn = pnext(); proj(pvr, pvrn, 0, "w_in", 4096 + j * 1024); proj(pvr, pvrn, 1, "w_in", 4096 + j * 1024 + 512)
                A(lambda e, j=j, pvr=pvr: e.copy(vr[:, j * 4:(j + 1) * 4, :].rearrange("p a b -> p (a b)"), pvr[:, :]), [pvrn], ["vr"])
            for j in range(2):
                pg, pgn = pnext(); proj(pg, pgn, 0, "w_in", 6144 + j * 1024); proj(pg, pgn, 1, "w_in", 6144 + j * 1024 + 512)
                A(lambda e, j=j, pg=pg: e.activation(sg[:, j * 1024:(j + 1) * 1024], pg[:, :], AF.Silu), [pgn], ["sg"])
            pat, patn = pnext()
            for h in range(8):
                P(lambda e, h=h, pat=pat: e.matmul(pat[:, h * 128:(h + 1) * 128], kT[:, h, :], qT[:, h, :], start=True, stop=True),
                  ["kT", "qT"], [patn])
            V(lambda e, pat=pat: e.tensor_tensor(attm[:].rearrange("p a b -> p (a b)"), pat[:, :], MT[:].rearrange("p a b -> p (a b)"), op=ALU.mult),
              [patn, "MT"], ["attm"])
            for j in range(2):
                po, pon = pnext()
                for h4 in range(4):
                    h = j * 4 + h4
                    P(lambda e, h=h, h4=h4, po=po: e.matmul(po[:, h4 * 256:(h4 + 1) * 256], attm[:, h, :], vr[:, h, :], start=True, stop=False),
                      ["attm", "vr"], [pon])
                    P(lambda e, h=h, h4=h4, po=po: e.matmul(po[:, h4 * 256:(h4 + 1) * 256], qdT[:, h, :], Sb[:, h, :], start=False, stop=True),
                      ["qdT", "Sb"], [pon])
                A(lambda e, j=j, po=po: e.copy(o_sb[:, j * 4:(j + 1) * 4, :].rearrange("p a b -> p (a b)"), po[:, :]), [pon], ["o_sb"])
            for j in range(2):
                pst, pstn = pnext()
                for h4 in range(4):
                    h = j * 4 + h4
                    P(lambda e, h=h, h4=h4, pst=pst: e.matmul(pst[:, h4 * 256:(h4 + 1) * 256], kd[:, h, :], vr[:, h, :], start=True, stop=True),
                      ["kd", "vr"], [pstn])
                for h4 in range(4):
                    h = j * 4 + h4
                    V(lambda e, h=h, h4=h4, pst=pst: e.scalar_tensor_tensor(Sf[:, h, :], Sf[:, h, :], float(GAM[h] ** 128), pst[:, h4 * 256:(h4 + 1) * 256],
                                                                            op0=ALU.mult, op1=ALU.add), [pstn, "Sf"], ["Sf"])
            A(lambda e: e.copy(Sb[:], Sf[:]), ["Sf"], ["Sb"])
            V(lambda e: e.tensor_reduce(st8[:, 0, :], o_sb[:], axis=AX.X, op=ALU.add), ["o_sb"], ["st8"])
            V(lambda e: e.tensor_tensor(o2[:], o_sb[:], o_sb[:], op=ALU.mult), ["o_sb"], ["o2"])
            V(lambda e: e.tensor_reduce(st8[:, 1, :], o2[:], axis=AX.X, op=ALU.add), ["o2"], ["st8"])
            V(lambda e: e.tensor_scalar(st8[:, 0, :], st8[:, 0, :], 1.0 / 256, None, op0=ALU.mult), ["st8"], ["st8"])
            V(lambda e: e.tensor_tensor(st8[:, 2, :], st8[:, 0, :], st8[:, 0, :], op=ALU.mult), ["st8"], ["st8"])
            V(lambda e: e.scalar_tensor_tensor(st8[:, 1, :], st8[:, 1, :], 1.0 / 256, st8[:, 2, :], op0=ALU.mult, op1=ALU.subtract), ["st8"], ["st8"])
            V(lambda e: e.tensor_scalar(st8[:, 1, :], st8[:, 1, :], EPS, None, op0=ALU.add), ["st8"], ["st8"])
            A(lambda e: e.activation(st8[:, 2, :], st8[:, 1, :], AF.Sqrt), ["st8"], ["st8"])
            V(lambda e: e.reciprocal(st8[:, 3, :], st8[:, 2, :]), ["st8"], ["st8"])
            V(lambda e: e.tensor_tensor(o2[:], o_sb[:], st8[:, 0, :].unsqueeze(2).to_broadcast([128, 8, 256]), op=ALU.subtract), ["o_sb", "st8"], ["o2"])
            V(lambda e: e.tensor_tensor(o2[:], o2[:], st8[:, 3, :].unsqueeze(2).to_broadcast([128, 8, 256]), op=ALU.mult), ["o2", "st8"], ["o2"])
            gnb, gnn = bcload(ret_gn_g)
            V(lambda e, gnb=gnb: e.tensor_tensor(o2[:].rearrange("p a b -> p (a b)"), o2[:].rearrange("p a b -> p (a b)"), gnb[:], op=ALU.mult), ["o2", gnn], ["o2"])
            V(lambda e: e.tensor_tensor(retb[:], o2[:].rearrange("p a b -> p (a b)"), sg[:], op=ALU.mult), ["o2", "sg"], ["retb"])
            transpose16(retb, "retb", retT, "retT")
            for n in range(4):
                pga, pgan = pnext()
                proj(pga, pgan, 0, "w_bg", n * 512); proj(pga, pgan, 1, "w_bg", 2048 + n * 512)
                bb, bbn = bcload(b_bg[0:1, n * 512:(n + 1) * 512])
                bb2, bb2n = bcload(b_bg[0:1, 2048 + n * 512:2048 + (n + 1) * 512])
                V(lambda e, pga=pga, bb=bb: e.tensor_tensor(gA[:], pga[:, 0:512], bb[:, 0:512], op=ALU.add), [pgan, bbn], ["gA"])
                V(lambda e, pga=pga, bb2=bb2: e.tensor_tensor(gB[:], pga[:, 512:1024], bb2[:, 0:512], op=ALU.add), [pgan, bb2n], ["gB"])
                A(lambda e: e.activation(gA[:], gA[:], AF.Sigmoid), ["gA"], ["gA"])
                A(lambda e: e.activation(gB[:], gB[:], AF.Sigmoid), ["gB"], ["gB"])
                pyy, pyyn = pnext()
                proj(pyy, pyyn, 0, "w_a", n * 512, K=8, lhs=preAT, lhsn="preAT")
                proj(pyy, pyyn, 1, "w_b", n * 512, K=16, lhs=retT, lhsn="retT")
                V(lambda e, pyy=pyy: e.tensor_tensor(gA[:], gA[:], pyy[:, 0:512], op=ALU.mult), ["gA", pyyn], ["gA"])
                V(lambda e, pyy=pyy: e.tensor_tensor(gB[:], gB[:], pyy[:, 512:1024], op=ALU.mult), ["gB", pyyn], ["gB"])
                V(lambda e, n=n: e.tensor_tensor(mb[:, n * 512:(n + 1) * 512], gA[:], gB[:], op=ALU.add), ["gA", "gB"], ["mb"])
            transpose16(mb, "mb", mT, "mT")
            ga1, ga1n = adaload(2 * D)
            for j in range(2):
                pm, pmn = pnext()
                proj(pm, pmn, 0, "w_o", j * 1024, lhs=mT, lhsn="mT"); proj(pm, pmn, 1, "w_o", j * 1024 + 512, lhs=mT, lhsn="mT")
                V(lambda e, j=j, pm=pm, ga1=ga1: e.tensor_tensor(ga1[:, j * 1024:(j + 1) * 1024], ga1[:, j * 1024:(j + 1) * 1024], pm[:, :], op=ALU.mult),
                  [pmn, ga1n], [ga1n])
            V(lambda e, ga1=ga1: e.tensor_tensor(xt[:], xt[:], ga1[:], op=ALU.add), ["xt", ga1n], ["xt"])
            if dbg:
                ld(dbg_x1[i], xt[:], ["dbgx1_%d" % i], r=["xt"])

            norm_from(xt, "xt", norm2_g, 4 * D, 3 * D, out_f32=(h2, "h2"))
            for half in range(2):
                pq2, pq2n = pnext()
                for cc in range(2):
                    wv, wn = wload("wq", half * 2 + cc)
                    for c4 in range(4):
                        j = cc * 4 + c4
                        for kc in range(16):
                            P(lambda e, wv=wv, kc=kc, c4=c4, j=j, pq2=pq2: e.matmul(pq2[:, j * 128:(j + 1) * 128], wv[:, kc, c4 * 128:(c4 + 1) * 128], hT[:, kc, :],
                                                                                   start=(kc == 0), stop=(kc == 15)), ["hT"] + wn, [pq2n])
                A(lambda e, half=half, pq2=pq2: e.copy(qpT[:, half * 8:(half + 1) * 8, :].rearrange("p a b -> p (a b)"), pq2[:, :]), [pq2n], ["qpT"])
            for half in range(2):
                psc, pscn = pnext()
                for j in range(8):
                    c = half * 8 + j
                    P(lambda e, c=c, j=j, psc=psc: e.matmul(psc[:, j * 128:(j + 1) * 128], qpT[:, c, :], subkT[:, c, :], start=True, stop=True),
                      ["qpT", "subkT"], [pscn])
                A(lambda e, half=half, psc=psc: e.copy(sc_sb[:, half * 8:(half + 1) * 8, :].rearrange("p a b -> p (a b)"), psc[:, :]), [pscn], ["sc_sb"])
            for c in range(16):
                V(lambda e, c=c: e.max(out=v12[:, c, 0:8], in_=sc_sb[:, c, :]), ["sc_sb"], ["v12"])
                V(lambda e, c=c: e.max_index(out=i12[:, c, 0:8], in_max=v12[:, c, 0:8], in_values=sc_sb[:, c, :]), ["sc_sb", "v12"], ["i12"])
                V(lambda e, c=c: e.match_replace(out=wk[:, 0:128], in_to_replace=v12[:, c, 0:8], in_values=sc_sb[:, c, :], imm_value=-1e30), ["sc_sb", "v12"], ["wk"])
                V(lambda e, c=c: e.max(out=v12[:, c, 8:16], in_=wk[:, 0:128]), ["wk"], ["v12"])
                V(lambda e, c=c: e.max_index(out=i12[:, c, 8:16], in_max=v12[:, c, 8:16], in_values=wk[:, 0:128]), ["wk", "v12"], ["i12"])
            V(lambda e: e.tensor_copy(i12f[:], i12[:]), ["i12"], ["i12f"])
            v12v = v12[:].rearrange("p (h two) k -> p h two k", two=2)
            i12v = i12f[:].rearrange("p (h two) k -> p h two k", two=2)
            for h in range(8):
                V(lambda e, h=h: e.tensor_tensor(cand[:, h, :].rearrange("p (a b) -> p a b", a=16),
                                                 v12v[:, h, 0, :].unsqueeze(2).to_broadcast([128, 16, 16]),
                                                 v12v[:, h, 1, :].unsqueeze(1).to_broadcast([128, 16, 16]), op=ALU.add), ["v12"], ["cand"])
            for h in range(8):
                V(lambda e, h=h: e.max(out=top[:, h, 0:8], in_=cand[:, h, :]), ["cand"], ["top"])
                V(lambda e, h=h: e.max_index(out=pos[:, h, 0:8], in_max=top[:, h, 0:8], in_values=cand[:, h, :]), ["cand", "top"], ["pos"])
                V(lambda e, h=h: e.match_replace(out=wk[:], in_to_replace=top[:, h, 0:8], in_values=cand[:, h, :], imm_value=-1e30), ["cand", "top"], ["wk"])
                V(lambda e, h=h: e.max(out=top[:, h, 8:16], in_=wk[:]), ["wk"], ["top"])
                V(lambda e, h=h: e.max_index(out=pos[:, h, 8:16], in_max=top[:, h, 8:16], in_values=wk[:]), ["wk", "top"], ["pos"])
            V(lambda e: e.tensor_copy(pcor[:], pos[:]), ["pos"], ["pcor"])
            V(lambda e: e.tensor_scalar(pi32[:], pcor[:], 0.0625, None, op0=ALU.mult), ["pcor"], ["pi32"])
            V(lambda e: e.tensor_copy(paf[:], pi32[:]), ["pi32"], ["paf"])
            V(lambda e: e.scalar_tensor_tensor(pbf[:].rearrange("p h k -> p (h k)"), paf[:].rearrange("p h k -> p (h k)"), -16.0,
                                               pcor[:].rearrange("p h k -> p (h k)"), op0=ALU.mult, op1=ALU.add), ["paf", "pcor"], ["pbf"])
            V(lambda e: e.tensor_scalar(pcor[:], pbf[:], 0.0, None, op0=ALU.is_lt), ["pbf"], ["pcor"])
            V(lambda e: e.tensor_tensor(paf[:], paf[:], pcor[:], op=ALU.subtract), ["paf", "pcor"], ["paf"])
            V(lambda e: e.scalar_tensor_tensor(pbf[:].rearrange("p h k -> p (h k)"), pcor[:].rearrange("p h k -> p (h k)"), 16.0,
                                               pbf[:].rearrange("p h k -> p (h k)"), op0=ALU.mult, op1=ALU.add), ["pcor", "pbf"], ["pbf"])
            for (pf, pfn, two, dst, dstn) in ((paf, "paf", 0, i1s, "i1s"), (pbf, "pbf", 1, i2s, "i2s")):
                for h in range(8):
                    ohv = oh[:, h, :].rearrange("p (k a) -> p k a", k=16)
                    V(lambda e, h=h, pf=pf, ohv=ohv: e.tensor_tensor(ohv, iota16[:, :].unsqueeze(1).to_broadcast([128, 16, 16]),
                                                                     pf[:, h, :].unsqueeze(2).to_broadcast([128, 16, 16]), op=ALU.is_equal),
                      ["iota16", pfn], ["oh"])
                    V(lambda e, h=h, two=two, ohv=ohv: e.tensor_tensor(ohv, ohv, i12v[:, h, two, :].unsqueeze(1).to_broadcast([128, 16, 16]), op=ALU.mult),
                      ["oh", "i12f"], ["oh"])
                V(lambda e, dst=dst: e.tensor_reduce(dst[:].rearrange("p h k -> p (h k)"), oh[:].rearrange("p h (k a) -> p (h k) a", a=16), axis=AX.X, op=ALU.add),
                  ["oh"], [dstn])
            V(lambda e: e.scalar_tensor_tensor(i1s[:], i1s[:], 128.0, i2s[:], op0=ALU.mult, op1=ALU.add), ["i1s", "i2s"], ["i1s"])
            V(lambda e: e.tensor_copy(eidx[:], i1s[:].rearrange("p h k -> p (h k)")), ["i1s"], ["eidx"])
            V(lambda e: e.tensor_tensor(gate[:], top[:], top[:, :, 0:1].to_broadcast([128, 8, 16]), op=ALU.subtract), ["top"], ["gate"])
            A(lambda e: e.activation(gate[:], gate[:], AF.Exp), ["gate"], ["gate"])
            V(lambda e: e.tensor_reduce(gsum[:], gate[:], axis=AX.X, op=ALU.add), ["gate"], ["gsum"])
            V(lambda e: e.reciprocal(gsum[:], gsum[:]), ["gsum"], ["gsum"])
            V(lambda e: e.tensor_tensor(gate[:], gate[:], gsum[:, :].unsqueeze(2).to_broadcast([128, 8, 16]), op=ALU.mult), ["gate", "gsum"], ["gate"])
            for hk in range(128):
                gt, gn = gnext()
                S.dma(lambda e, gt=gt, hk=hk: e.indirect_dma_start(out=gt[:], out_offset=None, in_=peer_u,
                                                                  in_offset=bass.IndirectOffsetOnAxis(ap=eidx[:, hk:hk + 1], axis=0)),
                      reads=["eidx"], writes=[gn], queue="pool")
                V(lambda e, gt=gt, hk=hk: e.scalar_tensor_tensor(junk[:], gt[:], 1.0, h2[:], op0=ALU.mult, op1=ALU.mult, accum_out=acol[:, hk:hk + 1]),
                  [gn, "h2"], ["junk", "acol"])
            A(lambda e: e.activation(wgt[:], acol[:], AF.Gelu), ["acol"], ["wgt"])
            V(lambda e: e.tensor_tensor(wgt[:], wgt[:], gate[:].rearrange("p h k -> p (h k)"), op=ALU.mult), ["wgt", "gate"], ["wgt"])
            for hk in range(128):
                gt, gn = gnext()
                S.dma(lambda e, gt=gt, hk=hk: e.indirect_dma_start(out=gt[:], out_offset=None, in_=peer_v,
                                                                  in_offset=bass.IndirectOffsetOnAxis(ap=eidx[:, hk:hk + 1], axis=0)),
                      reads=["eidx"], writes=[gn], queue="pool")
                if hk == 0:
                    V(lambda e, gt=gt: e.tensor_scalar(y[:], gt[:], wgt[:, 0:1], None, op0=ALU.mult), [gn, "wgt"], ["y"])
                else:
                    V(lambda e, gt=gt, hk=hk: e.scalar_tensor_tensor(y[:], gt[:], wgt[:, hk:hk + 1], y[:], op0=ALU.mult, op1=ALU.add), [gn, "wgt", "y"], ["y"])
            if dbg:
                ld(dbg_y[i], y[:], ["dbgy_%d" % i], r=["y"])
            ga2, ga2n = adaload(5 * D)
            V(lambda e, ga2=ga2: e.tensor_tensor(y[:], y[:], ga2[:], op=ALU.mult), ["y", ga2n], ["y"])
            V(lambda e: e.tensor_tensor(xt[:], xt[:], y[:], op=ALU.add), ["xt", "y"], ["xt"])
            A(lambda e: e.activation(junk[:], xt[:], AF.Square, accum_out=ss[:, 0:1]), ["xt"], ["junk", "ss"])
            V(lambda e: e.tensor_scalar(rstd[:, 0:1], ss[:, 0:1], 1.0 / D, EPS, op0=ALU.mult, op1=ALU.add), ["ss"], ["rstd"])
            A(lambda e: e.activation(rstd[:, 1:2], rstd[:, 0:1], AF.Sqrt), ["rstd"], ["rstd"])
            V(lambda e: e.reciprocal(rstd[:, 2:3], rstd[:, 1:2]), ["rstd"], ["rstd"])
            gf, gfn = bcload(norm_f_g)
            V(lambda e, gf=gf: e.scalar_tensor_tensor(y[:], xt[:], rstd[:, 2:3], gf[:], op0=ALU.mult, op1=ALU.mult), ["xt", "rstd", gfn], ["y"])
            ld(out_d[i], y[:], ["out%d" % i], r=["y"])
        fin = ["out%d" % i for i in range(NOWN)] + ([n % i for i in range(NOWN) for n in ("dbgx1_%d", "dbgy_%d")] if dbg else [])
        S.final_wait("sp", fin)
        S.emit()
    return nc


def _consts(seg):
    gam = np.array([1.0 - 2.0 ** (-5.0 - h) for h in range(H)], dtype=np.float64)
    scale = 128.0 ** -0.5
    i = np.arange(128)
    MT = np.zeros((128, H, 128), dtype=np.float64)
    ci, cj = i[None, :] // 64, i[:, None] // 64
    dist = np.abs(i[None, :] - i[:, None]).astype(np.float64)
    allowed = (ci >= cj)
    for h in range(H):
        MT[:, h, :] = np.where(allowed, gam[h] ** dist, 0.0) * scale
    qdecT = np.broadcast_to((gam[None, :, None] ** (i[None, None, :] + 1.0)), (128, H, 128))
    kdec = (gam[None, :] ** (127.0 - i[:, None])) * scale
    kdecP = np.zeros((128, NPRE, H), dtype=np.float64)
    P0 = seg * 1024
    for p in range(NPRE):
        gt = seg * 8 - NPRE + p
        if gt < 0:
            continue
        tok = gt * 128 + i
        kdecP[:, p, :] = (gam[None, :] ** (P0 - 1.0 - tok[:, None])) * scale
    f32 = lambda a: np.ascontiguousarray(a, dtype=np.float32)
    return f32(MT), f32(qdecT), f32(kdec), f32(kdecP)


def _in_maps(inputs):
    x = np.asarray(inputs["x"], dtype=np.float32)
    c = np.asarray(inputs["c"], dtype=np.float32)
    positions = np.asarray(inputs["positions"]).astype(np.int32)
    g = lambda k: np.asarray(inputs[k], dtype=np.float32)
    shared = {
        "w_ada": np.ascontiguousarray(g("w_ada")[0]),
        "b_ada": np.ascontiguousarray(g("b_ada")[0][None, :]),
        "norm1_g": np.ascontiguousarray(g("norm1_g")[0][None, :]),
        "w_in": np.ascontiguousarray(g("w_in")[0]),
        "w_bg": np.ascontiguousarray(g("w_branch_gate")[0]),
        "b_bg": np.ascontiguousarray(g("b_branch_gate")[0][None, :]),
        "gm_v_g": np.ascontiguousarray(g("gm_v_g")[0][None, :]),
        "wsT": np.ascontiguousarray(g("gm_ws")[0].transpose(2, 0, 1)),
        "gm_bT": np.ascontiguousarray(g("gm_b")[0].T),
        "ret_gn_g": np.ascontiguousarray(g("ret_gn_g")[0][None, :]),
        "w_a": np.ascontiguousarray(g("w_a_out")[0]),
        "w_b": np.ascontiguousarray(g("w_b_out")[0]),
        "w_o": np.ascontiguousarray(g("w_o")[0]),
        "norm2_g": np.ascontiguousarray(g("norm2_g")[0][None, :]),
        "wq": np.ascontiguousarray(g("peer_wq")[0]),
        "subkT": np.ascontiguousarray(g("peer_subkeys")[0].reshape(16, 128, 128).transpose(2, 0, 1)),
        "peer_u": np.ascontiguousarray(g("peer_u")[0]),
        "peer_v": np.ascontiguousarray(g("peer_v")[0]),
        "norm_f_g": np.ascontiguousarray(g("norm_f_g")[None, :]),
        "ident": np.eye(128, dtype=np.float32),
        "freq": np.ascontiguousarray(np.broadcast_to((10000.0 ** (-np.arange(64, dtype=np.float32) / 64)).astype(np.float32)[None, :], (128, 64))),
        "iota16": np.ascontiguousarray(np.broadcast_to(np.arange(16, dtype=np.float32)[None, :], (128, 16))),
    }
    maps = []
    for core in range(8):
        b, seg = core // 4, core % 4
        xsl = np.zeros((NPRE + NOWN, 128, D), dtype=np.float32)
        pos = np.zeros((128, NPRE + NOWN), dtype=np.int32)
        for p in range(NPRE + NOWN):
            gt = seg * 8 - NPRE + p
            if gt < 0:
                continue
            xsl[p] = x[b, gt * 128:(gt + 1) * 128]
            pos[:, p] = positions[b, gt * 128:(gt + 1) * 128]
        MT, qdecT, kdec, kdecP = _consts(seg)
        m = dict(shared)
        m.update({"xs": xsl, "posi": pos, "cT": np.ascontiguousarray(c[b].reshape(16, 128).T),
                  "MT": MT, "qdecT": qdecT, "kdec": kdec, "kdecP": kdecP})
        maps.append(m)
    return maps


_DBG = bool(int(os.environ.get("KDBG", "0")))
_last = {}


def kernel(**inputs):
    nc = build_program(dbg=_DBG)
    maps = _in_maps(inputs)
    res = run_bass_kernel_spmd(nc, maps, core_ids=list(range(8)))
    out = np.zeros((2, 4096, D), dtype=np.float32)
    for core in range(8):
        b, seg = core // 4, core % 4
        out[b, seg * 1024:(seg + 1) * 1024] = res.results[core]["out"].reshape(1024, D)
    if _DBG:
        _last["res"] = res.results
    return out
```

```python
import os
from contextlib import ExitStack
import numpy as np
import concourse.bass as bass
import concourse.mybir as mybir
from concourse.bass_utils import run_bass_kernel_spmd

F32 = mybir.dt.float32
BF16 = mybir.dt.bfloat16
I32 = mybir.dt.int32
U32 = mybir.dt.uint32
AF = mybir.ActivationFunctionType
ALU = mybir.AluOpType
AX = mybir.AxisListType

D = 2048
NPRE = 24
NOWN = 8
EPS = 1e-6
H = 8
ENGS = ("pe", "act", "dve", "pool", "sp")
NDMA_SEMS = 28
TWO_PI = float(2 * np.pi)


class Sched:
    def __init__(self, nc, stack):
        self.nc = nc
        self.stack = stack
        self.ops = {e: [] for e in ENGS}
        self.cnt = {e: 0 for e in ENGS}
        self.sem = {e: stack.enter_context(nc.semaphore("s_" + e)) for e in ENGS if e != "sp"}
        self.dsem = [stack.enter_context(nc.semaphore("d%d" % i)) for i in range(NDMA_SEMS)]
        self.dcnt = [0] * NDMA_SEMS
        half = NDMA_SEMS // 2
        self.dpool = {"sp": list(range(0, half)), "act": list(range(0, half)), "pool": list(range(half, NDMA_SEMS))}
        self.dnext = {"sp": 0, "act": 0, "pool": 0}
        self.waited = {e: {} for e in ENGS}
        self.lastw = {}
        self.readers = {}
        self.alias = {}

    def _x(self, names):
        out = []
        for n in names:
            out.extend(self.alias.get(n, [n]))
        return out

    def sb(self, name, shape, dtype=F32):
        return self.stack.enter_context(self.nc.sbuf_tensor("sb_" + name, list(shape), dtype))

    def ps(self, name, shape, dtype=F32):
        return self.stack.enter_context(self.nc.psum_tensor("ps_" + name, list(shape), dtype))

    def _semobj(self, key):
        return self.sem[key] if isinstance(key, str) else self.dsem[key]

    def _deps(self, eng, reads, writes):
        need = {}

        def add(k, v, same_ok):
            if k == eng and not same_ok and eng == "pe":
                return
            if need.get(k, 0) < v:
                need[k] = v

        for r in reads:
            if r in self.lastw:
                k, v = self.lastw[r]
                add(k, v, True)
        for w in writes:
            if w in self.lastw:
                k, v = self.lastw[w]
                add(k, v, False)
            for k, v in self.readers.get(w, {}).items():
                add(k, v, False)
        waits = []
        wd = self.waited[eng]
        for k, v in need.items():
            if wd.get(k, 0) >= v:
                continue
            wd[k] = v
            waits.append((k, v))
        return waits

    def _record(self, key, val, reads, writes):
        for r in reads:
            self.readers.setdefault(r, {})[key] = val
        for w in writes:
            self.lastw[w] = (key, val)
            self.readers[w] = {}

    def op(self, eng, fn, reads=(), writes=()):
        reads, writes = self._x(reads), self._x(writes)
        waits = self._deps(eng, reads, writes)
        self.cnt[eng] += 1
        self.ops[eng].append((waits, fn, (eng, 1)))
        self._record(eng, self.cnt[eng], reads, writes)

    def dma(self, fn, reads=(), writes=(), queue="sp"):
        reads, writes = self._x(reads), self._x(writes)
        qp = self.dpool[queue]
        i = qp[self.dnext[queue] % len(qp)]
        self.dnext[queue] += 1
        waits = self._deps(queue, reads, writes)
        if self.dcnt[i] > 0 and self.waited[queue].get(i, 0) < self.dcnt[i]:
            self.waited[queue][i] = self.dcnt[i]
            waits.append((i, self.dcnt[i]))
        self.dcnt[i] += 16
        self.ops[queue].append((waits, fn, (i, 16)))
        self._record(i, self.dcnt[i], reads, writes)

    def final_wait(self, eng, reslist):
        waits = self._deps(eng, self._x(reslist), ())
        self.ops[eng].append((waits, None, None))

    def emit(self):
        nc = self.nc
        with nc.Block() as block:
            def run(ename):
                def body(e):
                    for waits, fn, inc in self.ops[ename]:
                        for k, v in waits:
                            e.wait_ge(self._semobj(k), v)
                        if fn is None:
                            continue
                        ins = fn(e)
                        ins.then_inc(self._semobj(inc[0]), inc[1])
                return body
            block.tensor(run("pe"))
            block.scalar(run("act"))
            block.vector(run("dve"))
            block.gpsimd(run("pool"))
            block.sync(run("sp"))


def build_program(dbg=False):
    nc = bass.Bass("TRN2", target_bir_lowering=False)

    def din(name, shape, dt=F32):
        return nc.dram_tensor(name, list(shape), dt, kind="ExternalInput").ap()

    xs = din("xs", [NPRE + NOWN, 128, D])
    posi = din("posi", [128, NPRE + NOWN], I32)
    cT_d = din("cT", [128, 16])
    w_ada = din("w_ada", [D, 6 * D])
    b_ada = din("b_ada", [1, 6 * D])
    norm1_g = din("norm1_g", [1, D])
    w_in = din("w_in", [D, 8192])
    w_bg = din("w_bg", [D, 4096])
    b_bg = din("b_bg", [1, 4096])
    gm_v_g = din("gm_v_g", [1, 1024])
    wsT_d = din("wsT", [128, 8, 128])
    gm_bT_d = din("gm_bT", [128, 8])
    ret_gn_g = din("ret_gn_g", [1, D])
    w_a = din("w_a", [1024, D])
    w_b = din("w_b", [D, D])
    w_o = din("w_o", [D, D])
    norm2_g = din("norm2_g", [1, D])
    wq = din("wq", [D, D])
    subkT_d = din("subkT", [128, 16, 128])
    peer_u = din("peer_u", [16384, D])
    peer_v = din("peer_v", [16384, D])
    norm_f_g = din("norm_f_g", [1, D])
    ident_d = din("ident", [128, 128])
    freq_d = din("freq", [128, 64])
    iota16_d = din("iota16", [128, 16])
    MT_d = din("MT", [128, 8, 128])
    qdecT_d = din("qdecT", [128, 8, 128])
    kdec_d = din("kdec", [128, 8])
    kdecP_d = din("kdecP", [128, NPRE, 8])
    out_d = nc.dram_tensor("out", [NOWN, 128, D], F32, kind="ExternalOutput").ap()
    ada_d = nc.dram_tensor("ada_scr", [1, 6 * D], F32, kind="Internal").ap()
    WSPEC = {"w_in": (w_in, 16, 16), "w_bg": (w_bg, 16, 8), "w_a": (w_a, 8, 4), "w_b": (w_b, 16, 4), "w_o": (w_o, 16, 4), "wq": (wq, 16, 4)}
    wscr = {k: nc.dram_tensor("wb_" + k, [v[2], 128, v[1] * 512], BF16, kind="Internal").ap() for k, v in WSPEC.items()}
    if dbg:
        dbg_x1 = nc.dram_tensor("dbg_x1", [NOWN, 128, D], F32, kind="ExternalOutput").ap()
        dbg_y = nc.dram_tensor("dbg_y", [NOWN, 128, D], F32, kind="ExternalOutput").ap()

    GAM = [1.0 - 2.0 ** (-5.0 - h) for h in range(H)]

    with ExitStack() as st:
        S = Sched(nc, st)
        sb, ps = S.sb, S.ps

        ident = sb("ident", [128, 128]); identb = sb("identb", [128, 128], BF16)
        freq = sb("freq", [128, 64]); iota16 = sb("iota16", [128, 16])
        MT = sb("MT", [128, 8, 128]); qdecT = sb("qdecT", [128, 8, 128])
        kdec = sb("kdec", [128, 8]); kdecP = sb("kdecP", [128, NPRE, 8])
        wsT = sb("wsT", [128, 8, 128], BF16)
        gmbT = sb("gmbT", [128, 8]); subkT = sb("subkT", [128, 16, 128])
        posI = sb("posI", [128, NPRE + NOWN], I32); posF = sb("posF", [128, NPRE + NOWN])
        cT = sb("cT", [128, 16]); scT = sb("scT", [128, 16], BF16)

        NW = 3
        wring = [sb("w%d" % i, [128, 8192], BF16) for i in range(NW)]
        wr_i = [0]

        def wnext():
            i = wr_i[0]; wr_i[0] = (i + 1) % NW
            return wring[i], ["w%da" % i, "w%db" % i]

        NBC = 2
        bcring = [sb("bc%d" % i, [128, D]) for i in range(NBC)]
        bc_i = [0]

        def bcnext():
            i = bc_i[0]; bc_i[0] = (i + 1) % NBC
            return bcring[i], "bc%d" % i

        NP = 4
        pring = [ps("P%d" % i, [128, 1024]) for i in range(NP)]
        p_i = [0]

        def pnext():
            i = p_i[0]; p_i[0] = (i + 1) % NP
            return pring[i], "P%d" % i

        xt = sb("xt", [128, D])
        ss = sb("ss", [128, 8]); rstd = sb("rstd", [128, 8])
        hb = sb("hb", [128, D], BF16); hT = sb("hT", [128, 16, 128], BF16)
        junk = hb; S.alias["junk"] = ["hb"]
        cs = sb("cs", [128, 4, 64])
        ki = sb("ki", [128, 64], I32); kf = sb("kf", [128, 2, 64])
        pi32 = sb("pi32", [128, 8, 16], I32); pcor = sb("pcor", [128, 8, 16])
        Sf = sb("Sf", [128, 8, 256]); Sb = sb("Sb", [128, 8, 256], BF16)
        st8 = sb("st8", [128, 4, 8])
        wk = sb("wk", [128, 256])
        v12 = sb("v12", [128, 16, 16]); i12 = sb("i12", [128, 16, 16], U32); i12f = sb("i12f", [128, 16, 16])
        top = sb("top", [128, 8, 16]); pos = sb("pos", [128, 8, 16], U32)
        paf = sb("paf", [128, 8, 16]); pbf = sb("pbf", [128, 8, 16])
        i1s = sb("i1s", [128, 8, 16]); i2s = sb("i2s", [128, 8, 16])
        eidx = sb("eidx", [128, 128], I32)
        gate = sb("gate", [128, 8, 16]); gsum = sb("gsum", [128, 8])
        acol = sb("acol", [128, 128]); wgt = sb("wgt", [128, 128])

        ARENA = 80 * 1024
        GRAN = 2048
        arena = sb("arena", [128, ARENA // 4])
        DSZ = {F32: 4, BF16: 2, I32: 4, U32: 4}

        def carve(off, name, shape, dtype=F32, parts=128):
            nel = int(np.prod(shape[1:]))
            nbytes = nel * DSZ[dtype]
            assert off % 4 == 0 and off + nbytes <= ARENA, (name, off, nbytes)
            v = arena[0:parts, off // 4:(off + nbytes) // 4]
            if dtype != F32:
                v = v.bitcast(dtype)
            if len(shape) == 3:
                v = v.rearrange("p (a b) -> p a b", a=shape[1])
            S.alias[name] = ["ar%d" % g for g in range(off // GRAN, (off + nbytes + GRAN - 1) // GRAN)]
            return v, off + ((nbytes + GRAN - 1) // GRAN) * GRAN

        K = 1024
        sg, o = carve(0, "sg", [128, D])
        rotA, o = carve(o, "rotA", [128, 8, 128]); rotB, o2_off = carve(o, "rotB", [128, 8, 128])
        o_sb, _ = carve(o - 4 * K, "o_sb", [128, 8, 256])
        o = o2_off
        u_sb, o = carve(o, "u_sb", [128, 8, 128]); v_f, o = carve(o, "v_f", [128, 8, 128])
        o2, _ = carve(o - 8 * K, "o2", [128, 8, 256])
        qb, o = carve(o, "qb", [128, 8, 128], BF16); kb, o = carve(o, "kb", [128, 8, 128], BF16)
        kd, o = carve(o, "kd", [128, 8, 128], BF16); qT, o = carve(o, "qT", [128, 8, 128], BF16)
        qdT, o = carve(o, "qdT", [128, 8, 128], BF16); kT, o = carve(o, "kT", [128, 8, 128], BF16)
        vr, o = carve(o, "vr", [128, 8, 256], BF16)
        v_b, o = carve(o, "v_b", [128, 8, 128], BF16); preA, o = carve(o, "preA", [128, 8, 128], BF16)
        preAT, o = carve(o, "preAT", [128, 8, 128], BF16); attm, o = carve(o, "attm", [128, 8, 128], BF16)
        retb, o = carve(o, "retb", [128, D], BF16); retT, o = carve(o, "retT", [128, 16, 128], BF16)
        mb, o = carve(o, "mb", [128, D], BF16); mT, o = carve(o, "mT", [128, 16, 128], BF16)
        gA, o = carve(o, "gA", [128, 512]); gB, o = carve(o, "gB", [128, 512])
        xt2, _ = carve(40 * K, "xt2", [128, D]); hb2, _ = carve(48 * K, "hb2", [128, D], BF16)
        hT2, _ = carve(52 * K, "hT2", [128, 16, 128], BF16)
        A1p, _ = carve(56 * K, "A1p", [128, D]); sh1p, _ = carve(64 * K, "sh1p", [128, D])
        cvS = [carve(0, "cvS0", [128, 8, 512], BF16)[0], carve(16 * K, "cvS1", [128, 8, 512], BF16)[0]]
        brow, _ = carve(0, "brow", [1, 512], parts=1); arow, _ = carve(2 * K, "arow", [1, 512], parts=1)
        h2, o = carve(0, "h2", [128, D]); y, o = carve(o, "y", [128, D])
        qpT, o = carve(o, "qpT", [128, 16, 128]); sc_sb, o = carve(o, "sc_sb", [128, 16, 128])
        cand, o = carve(o, "cand", [128, 8, 256]); oh, o = carve(o, "oh", [128, 8, 256])
        NG = 4
        gring = []
        for gi in range(NG):
            gv, o = carve(o, "g%d" % gi, [128, D])
            gring.append(gv)
        g_i = [0]

        def gnext():
            i = g_i[0]; g_i[0] = (i + 1) % NG
            return gring[i], "g%d" % i

        def V(fn, r, w):
            S.op("dve", fn, r, w)

        def A(fn, r, w):
            S.op("act", fn, r, w)

        def P(fn, r, w):
            S.op("pe", fn, r, w)

        def G(fn, r, w):
            S.op("pool", fn, r, w)

        def ld(out_ap, in_ap, w, queue="sp", r=()):
            S.dma(lambda e: e.dma_start(out=out_ap, in_=in_ap), reads=r, writes=w, queue=queue)

        def bcload(src_row, r=()):
            t, n = bcnext()
            width = src_row.shape[1]
            ld(t[:, 0:width], src_row.partition_broadcast(128)[:, 0, :], [n], r=r, queue="act")
            return t, n

        def adaload(off):
            return bcload(ada_d[0:1, off:off + D], r=["ada%d" % k for k in range(off // 512, off // 512 + 4)])

        def wload_cast(src, K, N=512):
            t, n = wnext()
            view = t[:, 0:K * N].rearrange("p (k n) -> p k n", k=K)
            ld(view, src.rearrange("(k p) n -> p k n", p=128), n, queue="pool")
            return view, n

        def wload(name, j):
            K_ = WSPEC[name][1]
            t, n = wnext()
            ld(t[:, 0:K_ * 512], wscr[name][j], n, r=wb_res[(name, j)])
            return t[:, 0:K_ * 512].rearrange("p (k n) -> p k n", k=K_), n

        ld(ident[:], ident_d, ["ident"]); ld(freq[:], freq_d, ["freq"]); ld(iota16[:], iota16_d, ["iota16"])
        ld(MT[:], MT_d, ["MT"]); ld(qdecT[:], qdecT_d, ["qdecT"]); ld(kdec[:], kdec_d, ["kdec"])
        ld(kdecP[:], kdecP_d, ["kdecP"]); ld(wsT[:], wsT_d, ["wsT"], queue="pool"); ld(gmbT[:], gm_bT_d, ["gmbT"])
        ld(subkT[:], subkT_d, ["subkT"]); ld(posI[:], posi, ["posI"]); ld(cT[:], cT_d, ["cT"])
        V(lambda e: e.tensor_copy(identb[:], ident[:]), ["ident"], ["identb"])
        V(lambda e: e.tensor_copy(posF[:], posI[:]), ["posI"], ["posF"])
        V(lambda e: e.memset(wsT[64:128, :, 0:64], 0.0), ["wsT"], ["wsT"])
        V(lambda e: e.memset(Sf[:], 0.0), [], ["Sf"])

        wb_res = {}
        EARLY = [("w_in", j) for j in range(6, 12)]
        for name, j in EARLY:
            wsrc_, K_, nch = WSPEC[name]
            view, n = wload_cast(wsrc_[:, j * 512:(j + 1) * 512], K_)
            wb_res[(name, j)] = ["wb_%s_%d" % (name, j)]
            ld(wscr[name][j], view.rearrange("p k n -> p (k n)"), wb_res[(name, j)], r=n)
        late_jobs = []
        for name, (wsrc_, K_, nch) in WSPEC.items():
            for j in range(nch):
                if (name, j) in EARLY:
                    continue
                wb_res[(name, j)] = ["wb_%s_%d_%d" % (name, j, h) for h in range(K_ // 8)]
                for h in range(K_ // 8):
                    late_jobs.append((name, j, h))
        cv_i = [0]

        def emit_late(count):
            for _ in range(count):
                if not late_jobs:
                    return
                name, j, h = late_jobs.pop(0)
                wsrc_ = WSPEC[name][0]
                buf = cvS[cv_i[0] % 2]; bn = "cvS%d" % (cv_i[0] % 2); cv_i[0] += 1
                ld(buf[:], wsrc_[h * 1024:(h + 1) * 1024, j * 512:(j + 1) * 512].rearrange("(k p) n -> p k n", p=128), [bn], queue="pool")
                ld(wscr[name][j][:, h * 4096:(h + 1) * 4096], buf[:].rearrange("p k n -> p (k n)"), ["wb_%s_%d_%d" % (name, j, h)], r=[bn])

        A(lambda e: e.activation(scT[:], cT[:], AF.Silu), ["cT"], ["scT"])
        for n in range(24):
            pt, pn = pnext()
            wv, wn = wload_cast(w_ada[:, n * 512:(n + 1) * 512], 16)
            for kc in range(16):
                P(lambda e, wv=wv, kc=kc, pt=pt: e.matmul(pt[0:1, 0:512], scT[:, kc:kc + 1], wv[:, kc, :],
                                                          start=(kc == 0), stop=(kc == 15)), ["scT"] + wn, [pn])
            ld(brow[:], b_ada[0:1, n * 512:(n + 1) * 512], ["brow"])
            V(lambda e, pt=pt: e.tensor_tensor(arow[:], pt[0:1, 0:512], brow[:], op=ALU.add), [pn, "brow"], ["arow"])
            ld(ada_d[0:1, n * 512:(n + 1) * 512], arow[:], ["ada%d" % n], r=["arow"])

        def load_x_norm(tile_idx, g_row, sc_off, sh_off, out_f32=None):
            ld(xt[:], xs[tile_idx], ["xt"])
            norm_from(xt, "xt", g_row, sc_off, sh_off, out_f32)

        def prefix_norm(tile_idx, par):
            x_, xn_, hb_, hbn_, hT_, hTn_ = (xt, "xt", hb, "hb", hT, "hT") if par == 0 else (xt2, "xt2", hb2, "hb2", hT2, "hT2")
            ld(x_[:], xs[tile_idx], [xn_])
            c0 = 4 * par
            A(lambda e: e.activation(hb_[:], x_[:], AF.Square, accum_out=ss[:, c0:c0 + 1]), [xn_], [hbn_, "ss"])
            V(lambda e: e.tensor_scalar(rstd[:, c0:c0 + 1], ss[:, c0:c0 + 1], 1.0 / D, EPS, op0=ALU.mult, op1=ALU.add), ["ss"], ["rstd"])
            A(lambda e: e.activation(rstd[:, c0 + 1:c0 + 2], rstd[:, c0:c0 + 1], AF.Sqrt), ["rstd"], ["rstd"])
            V(lambda e: e.reciprocal(rstd[:, c0 + 2:c0 + 3], rstd[:, c0 + 1:c0 + 2]), ["rstd"], ["rstd"])
            tmp, tmpn = bcnext()
            V(lambda e: e.scalar_tensor_tensor(tmp[:], x_[:], rstd[:, c0 + 2:c0 + 3], A1p[:], op0=ALU.mult, op1=ALU.mult), [xn_, "rstd", "A1p"], [tmpn])
            V(lambda e: e.tensor_tensor(hb_[:], tmp[:], sh1p[:], op=ALU.add), [tmpn, "sh1p"], [hbn_])
            transpose16(hb_, hbn_, hT_, hTn_)
            return hT_, hTn_

        def norm_from(src, srcn, g_row, sc_off, sh_off, out_f32=None):
            A(lambda e: e.activation(junk[:], src[:], AF.Square, accum_out=ss[:, 0:1]), [srcn], ["junk", "ss"])
            V(lambda e: e.tensor_scalar(rstd[:, 0:1], ss[:, 0:1], 1.0 / D, EPS, op0=ALU.mult, op1=ALU.add), ["ss"], ["rstd"])
            A(lambda e: e.activation(rstd[:, 1:2], rstd[:, 0:1], AF.Sqrt), ["rstd"], ["rstd"])
            V(lambda e: e.reciprocal(rstd[:, 2:3], rstd[:, 1:2]), ["rstd"], ["rstd"])
            gt, gn = bcload(g_row)
            sct, scn = adaload(sc_off)
            V(lambda e: e.scalar_tensor_tensor(sct[:], sct[:], 1.0, gt[:], op0=ALU.add, op1=ALU.mult), [gn, scn], [scn])
            sht, shn = adaload(sh_off)
            V(lambda e: e.scalar_tensor_tensor(sct[:], src[:], rstd[:, 2:3], sct[:], op0=ALU.mult, op1=ALU.mult), [srcn, "rstd", scn], [scn])
            if out_f32 is not None:
                V(lambda e: e.tensor_tensor(out_f32[0][:], sct[:], sht[:], op=ALU.add), [scn, shn], [out_f32[1]])
                A(lambda e: e.copy(hb[:], out_f32[0][:]), [out_f32[1]], ["hb"])
            else:
                V(lambda e: e.tensor_tensor(hb[:], sct[:], sht[:], op=ALU.add), [scn, shn], ["hb"])
            transpose16(hb, "hb", hT, "hT")

        def transpose16(src, srcn, dst, dstn, nchunks=16):
            for half in range((nchunks + 7) // 8):
                pt, pn = pnext()
                pv = pt[:].bitcast(BF16)
                cnt = min(8, nchunks - half * 8)
                for j in range(cnt):
                    c = half * 8 + j
                    P(lambda e, pv=pv, j=j, c=c: e.transpose(pv[:, j * 128:(j + 1) * 128], src[:, c * 128:(c + 1) * 128], identb[:]),
                      [srcn, "identb"], [pn])
                A(lambda e, pv=pv, half=half, cnt=cnt: e.copy(dst[:, half * 8:half * 8 + cnt, :].rearrange("p a b -> p (a b)"), pv[:, 0:cnt * 128]),
                  [pn], [dstn])

        def rope_tables(tile_idx):
            pcol = posF[:, tile_idx:tile_idx + 1]
            PI = float(np.pi)
            for (shift, dsti) in ((0.0, 1), (PI / 2, 0)):
                V(lambda e, shift=shift: e.tensor_scalar(cs[:, 3, :], freq[:], pcol, shift, op0=ALU.mult, op1=ALU.add), ["freq", "posF", "cs"], ["cs3"])
                V(lambda e: e.tensor_scalar(ki[:], cs[:, 3, :], 1.0 / TWO_PI, None, op0=ALU.mult), ["cs3"], ["ki"])
                V(lambda e: e.tensor_copy(kf[:, 0, :], ki[:]), ["ki"], ["kf"])
                V(lambda e: e.scalar_tensor_tensor(cs[:, 3, :], kf[:, 0, :], -TWO_PI, cs[:, 3, :], op0=ALU.mult, op1=ALU.add), ["kf", "cs3"], ["cs3"])
                V(lambda e: e.tensor_scalar(kf[:, 1, :], cs[:, 3, :], PI, -TWO_PI, op0=ALU.is_gt, op1=ALU.mult), ["cs3"], ["kf"])
                V(lambda e: e.tensor_tensor(cs[:, 3, :], cs[:, 3, :], kf[:, 1, :], op=ALU.add), ["kf", "cs3"], ["cs3"])
                V(lambda e: e.tensor_scalar(cs[:, 3, :], cs[:, 3, :], -PI, PI, op0=ALU.max, op1=ALU.min), ["cs3"], ["cs3"])
                A(lambda e, dsti=dsti: e.activation(cs[:, dsti, :], cs[:, 3, :], AF.Sin), ["cs3"], ["cs"])
            V(lambda e: e.tensor_scalar(cs[:, 2, :], cs[:, 1, :], -1.0, None, op0=ALU.mult), ["cs"], ["cs"])

        def rope(pt, pn, nh, dst, dstn):
            pv = pt[:, 0:nh * 128].rearrange("p (h d) -> p h d", h=nh)
            cosb = cs[:, 0, :].unsqueeze(1).to_broadcast([128, nh, 64])
            sinb = cs[:, 1, :].unsqueeze(1).to_broadcast([128, nh, 64])
            nsinb = cs[:, 2, :].unsqueeze(1).to_broadcast([128, nh, 64])
            V(lambda e: e.tensor_tensor(rotA[:, 0:nh, 0:64], pv[:, :, 0:64], cosb, op=ALU.mult), [pn, "cs"], ["rotA"])
            V(lambda e: e.tensor_tensor(rotA[:, 0:nh, 64:128], pv[:, :, 64:128], cosb, op=ALU.mult), [pn, "cs"], ["rotA"])
            V(lambda e: e.tensor_tensor(rotB[:, 0:nh, 0:64], pv[:, :, 64:128], nsinb, op=ALU.mult), [pn, "cs"], ["rotB"])
            V(lambda e: e.tensor_tensor(rotB[:, 0:nh, 64:128], pv[:, :, 0:64], sinb, op=ALU.mult), [pn, "cs"], ["rotB"])
            V(lambda e: e.tensor_tensor(dst[:, 0:nh, :], rotA[:, 0:nh, :], rotB[:, 0:nh, :], op=ALU.add), ["rotA", "rotB"], [dstn])

        def proj(pt, pn, col, wname, c0, K=16, lhs=None, lhsn="hT"):
            lhs = hT if lhs is None else lhs
            wv, wn = wload(wname, c0 // 512)
            for kc in range(K):
                P(lambda e, wv=wv, kc=kc: e.matmul(pt[:, col * 512:(col + 1) * 512], lhs[:, kc, :], wv[:, kc, :],
                                                   start=(kc == 0), stop=(kc == K - 1)), [lhsn] + wn, [pn])

        gt_, gn_ = bcload(norm1_g)
        ld(A1p[:], ada_d[0:1, D:2 * D].partition_broadcast(128)[:, 0, :], ["A1p"], r=["ada%d" % k for k in range(4, 8)])
        V(lambda e: e.scalar_tensor_tensor(A1p[:], A1p[:], 1.0, gt_[:], op0=ALU.add, op1=ALU.mult), ["A1p", gn_], ["A1p"])
        ld(sh1p[:], ada_d[0:1, 0:D].partition_broadcast(128)[:, 0, :], ["sh1p"], r=["ada%d" % k for k in range(0, 4)])
        for hh in range(2):
            wk_v, wk_n = wload("w_in", 6 + hh)
            wv0, wv0n = wload("w_in", 8 + hh * 2)
            wv1, wv1n = wload("w_in", 9 + hh * 2)
            for p in range(NPRE):
                hT_, hTn_ = prefix_norm(p, p % 2)
                rope_tables(p)
                pk, pkn = pnext()
                for kc in range(16):
                    P(lambda e, kc=kc, pk=pk, hT_=hT_: e.matmul(pk[:, 0:512], hT_[:, kc, :], wk_v[:, kc, :], start=(kc == 0), stop=(kc == 15)),
                      [hTn_] + wk_n, [pkn])
                pv_, pvn = pnext()
                for j, (wv, wn) in enumerate(((wv0, wv0n), (wv1, wv1n))):
                    for kc in range(16):
                        P(lambda e, kc=kc, j=j, wv=wv, pv_=pv_, hT_=hT_: e.matmul(pv_[:, j * 512:(j + 1) * 512], hT_[:, kc, :], wv[:, kc, :],
                                                                         start=(kc == 0), stop=(kc == 15)), [hTn_] + wn, [pvn])
                rope(pk, pkn, 4, kb, "kb")
                V(lambda e, p=p, hh=hh: e.tensor_tensor(kd[:, 0:4, :], kb[:, 0:4, :],
                                                        kdecP[:, p, hh * 4:(hh + 1) * 4].unsqueeze(2).to_broadcast([128, 4, 128]), op=ALU.mult),
                  ["kb", "kdecP"], ["kd"])
                A(lambda e, pv_=pv_: e.copy(vr[:, 0:4, :].rearrange("p a b -> p (a b)"), pv_[:, :]), [pvn], ["vr"])
                pst, pstn = pnext()
                for h4 in range(4):
                    P(lambda e, h4=h4, pst=pst: e.matmul(pst[:, h4 * 256:(h4 + 1) * 256], kd[:, h4, :], vr[:, h4, :], start=True, stop=True),
                      ["kd", "vr"], [pstn])
                V(lambda e, hh=hh, pst=pst: e.tensor_tensor(Sf[:, hh * 4:(hh + 1) * 4, :].rearrange("p a b -> p (a b)"),
                                                            Sf[:, hh * 4:(hh + 1) * 4, :].rearrange("p a b -> p (a b)"), pst[:, :], op=ALU.add),
                  [pstn, "Sf"], ["Sf"])
                emit_late(2)
        emit_late(len(late_jobs))
        A(lambda e: e.copy(Sb[:], Sf[:]), ["Sf"], ["Sb"])

        for i in range(NOWN):
            ti = NPRE + i
            load_x_norm(ti, norm1_g, 1 * D, 0)
            rope_tables(ti)
            pu, pun = pnext(); proj(pu, pun, 0, "w_in", 0); proj(pu, pun, 1, "w_in", 512)
            A(lambda e, pu=pu: e.activation(u_sb[:].rearrange("p a b -> p (a b)"), pu[:, :], AF.Gelu), [pun], ["u_sb"])
            pvv, pvvn = pnext(); proj(pvv, pvvn, 0, "w_in", 1024); proj(pvv, pvvn, 1, "w_in", 1536)
            A(lambda e, pvv=pvv: e.activation(v_f[:].rearrange("p a b -> p (a b)"), pvv[:, :], AF.Gelu), [pvvn], ["v_f"])
            V(lambda e: e.tensor_tensor(rotA[:], v_f[:], v_f[:], op=ALU.mult), ["v_f"], ["rotA"])
            V(lambda e: e.tensor_reduce(ss[:, 0:8], rotA[:], axis=AX.X, op=ALU.add), ["rotA"], ["ss"])
            V(lambda e: e.tensor_scalar(rstd[:, 0:8], ss[:, 0:8], 1.0 / 128, EPS, op0=ALU.mult, op1=ALU.add), ["ss"], ["rstd"])
            A(lambda e: e.activation(ss[:, 0:8], rstd[:, 0:8], AF.Sqrt), ["rstd"], ["ss"])
            V(lambda e: e.reciprocal(rstd[:, 0:8], ss[:, 0:8]), ["ss"], ["rstd"])
            vg, vgn = bcload(gm_v_g)
            V(lambda e: e.tensor_tensor(rotA[:], v_f[:], rstd[:, 0:8].unsqueeze(2).to_broadcast([128, 8, 128]), op=ALU.mult), ["v_f", "rstd"], ["rotA"])
            V(lambda e, vg=vg: e.tensor_tensor(v_b[:].rearrange("p a b -> p (a b)"), rotA[:].rearrange("p a b -> p (a b)"), vg[:, 0:1024], op=ALU.mult),
              ["rotA", vgn], ["v_b"])
            psg, psgn = pnext()
            for g in range(8):
                P(lambda e, g=g, psg=psg: e.matmul(psg[:, g * 128:(g + 1) * 128], wsT[:, g, :], v_b[:, g, :], start=True, stop=True),
                  ["wsT", "v_b"], [psgn])
            V(lambda e, psg=psg: e.tensor_tensor(rotA[:], psg[:, :].rearrange("p (a b) -> p a b", a=8),
                                                 gmbT[:, :].unsqueeze(2).to_broadcast([128, 8, 128]), op=ALU.add), [psgn, "gmbT"], ["rotA"])
            V(lambda e: e.tensor_tensor(preA[:], rotA[:], u_sb[:], op=ALU.mult), ["rotA", "u_sb"], ["preA"])
            transpose16(preA[:].rearrange("p a b -> p (a b)"), "preA", preAT, "preAT", nchunks=8)
            pq, pqn = pnext(); proj(pq, pqn, 0, "w_in", 2048); proj(pq, pqn, 1, "w_in", 2560)
            rope(pq, pqn, 8, qb, "qb")
            pk, pkn = pnext(); proj(pk, pkn, 0, "w_in", 3072); proj(pk, pkn, 1, "w_in", 3584)
            rope(pk, pkn, 8, kb, "kb")
            V(lambda e: e.tensor_tensor(kd[:], kb[:], kdec[:, :].unsqueeze(2).to_broadcast([128, 8, 128]), op=ALU.mult), ["kb", "kdec"], ["kd"])
            pt, pn = pnext(); pv = pt[:].bitcast(BF16)
            for h in range(8):
                P(lambda e, h=h, pv=pv: e.transpose(pv[:, h * 128:(h + 1) * 128], qb[:, h, :], identb[:]), ["qb", "identb"], [pn])
            A(lambda e, pv=pv: e.copy(qT[:].rearrange("p a b -> p (a b)"), pv[:, 0:1024]), [pn], ["qT"])
            V(lambda e, pv=pv: e.tensor_tensor(qdT[:].rearrange("p a b -> p (a b)"), pv[:, 0:1024], qdecT[:].rearrange("p a b -> p (a b)"), op=ALU.mult),
              [pn, "qdecT"], ["qdT"])
            pt, pn = pnext(); pv = pt[:].bitcast(BF16)
            for h in range(8):
                P(lambda e, h=h, pv=pv: e.transpose(pv[:, h * 128:(h + 1) * 128], kb[:, h, :], identb[:]), ["kb", "identb"], [pn])
            A(lambda e, pv=pv: e.copy(kT[:].rearrange("p a b -> p (a b)"), pv[:, 0:1024]), [pn], ["kT"])
            for j in range(2):
                pvr, pvrn = pnext(); proj(pvr, pvrn, 0, "w_in", 4096 + j * 1024); proj(pvr, pvrn, 1, "w_in", 4096 + j * 1024 + 512)
                A(lambda e, j=j, pvr=pvr: e.copy(vr[:, j * 4:(j + 1) * 4, :].rearrange("p a b -> p (a b)"), pvr[:, :]), [pvrn], ["vr"])
            for j in range(2):
                pg, pgn = pnext(); proj(pg, pgn, 0, "w_in", 6144 + j * 1024); proj(pg, pgn, 1, "w_in", 6144 + j * 1024 + 512)
                A(lambda e, j=j, pg=pg: e.activation(sg[:, j * 1024:(j + 1) * 1024], pg[:, :], AF.Silu), [pgn], ["sg"])
            pat, patn = pnext()
            for h in range(8):
                P(lambda e, h=h, pat=pat: e.matmul(pat[:, h * 128:(h + 1) * 128], kT[:, h, :], qT[:, h, :], start=True, stop=True),
                  ["kT", "qT"], [patn])
            V(lambda e, pat=pat: e.tensor_tensor(attm[:].rearrange("p a b -> p (a b)"), pat[:, :], MT[:].rearrange("p a b -> p (a b)"), op=ALU.mult),
              [patn, "MT"], ["attm"])
            for j in range(2):
                po, pon = pnext()
                for h4 in range(4):
                    h = j * 4 + h4
                    P(lambda e, h=h, h4=h4, po=po: e.matmul(po[:, h4 * 256:(h4 + 1) * 256], attm[:, h, :], vr[:, h, :], start=True, stop=False),
                      ["attm", "vr"], [pon])
                    P(lambda e, h=h, h4=h4, po=po: e.matmul(po[:, h4 * 256:(h4 + 1) * 256], qdT[:, h, :], Sb[:, h, :], start=False, stop=True),
                      ["qdT", "Sb"], [pon])
                A(lambda e, j=j, po=po: e.copy(o_sb[:, j * 4:(j + 1) * 4, :].rearrange("p a b -> p (a b)"), po[:, :]), [pon], ["o_sb"])
            for j in range(2):
                pst, pstn = pnext()
                for h4 in range(4):
                    h = j * 4 + h4
                    P(lambda e, h=h, h4=h4, pst=pst: e.matmul(pst[:, h4 * 256:(h4 + 1) * 256], kd[:, h, :], vr[:, h, :], start=True, stop=True),
                      ["kd", "vr"], [pstn])
                for h4 in range(4):
                    h = j * 4 + h4
                    V(lambda e, h=h, h4=h4, pst=pst: e.scalar_tensor_tensor(Sf[:, h, :], Sf[:, h, :], float(GAM[h] ** 128), pst[:, h4 * 256:(h4 + 1) * 256],
                                                                            op0=ALU.mult, op1=ALU.add), [pstn, "Sf"], ["Sf"])
            A(lambda e: e.copy(Sb[:], Sf[:]), ["Sf"], ["Sb"])
            V(lambda e: e.tensor_reduce(st8[:, 0, :], o_sb[:], axis=AX.X, op=ALU.add), ["o_sb"], ["st8"])
            V(lambda e: e.tensor_tensor(o2[:], o_sb[:], o_sb[:], op=ALU.mult), ["o_sb"], ["o2"])
            V(lambda e: e.tensor_reduce(st8[:, 1, :], o2[:], axis=AX.X, op=ALU.add), ["o2"], ["st8"])
            V(lambda e: e.tensor_scalar(st8[:, 0, :], st8[:, 0, :], 1.0 / 256, None, op0=ALU.mult), ["st8"], ["st8"])
            V(lambda e: e.tensor_tensor(st8[:, 2, :], st8[:, 0, :], st8[:, 0, :], op=ALU.mult), ["st8"], ["st8"])
            V(lambda e: e.scalar_tensor_tensor(st8[:, 1, :], st8[:, 1, :], 1.0 / 256, st8[:, 2, :], op0=ALU.mult, op1=ALU.subtract), ["st8"], ["st8"])
            V(lambda e: e.tensor_scalar(st8[:, 1, :], st8[:, 1, :], EPS, None, op0=ALU.add), ["st8"], ["st8"])
            A(lambda e: e.activation(st8[:, 2, :], st8[:, 1, :], AF.Sqrt), ["st8"], ["st8"])
            V(lambda e: e.reciprocal(st8[:, 3, :], st8[:, 2, :]), ["st8"], ["st8"])
            V(lambda e: e.tensor_tensor(o2[:], o_sb[:], st8[:, 0, :].unsqueeze(2).to_broadcast([128, 8, 256]), op=ALU.subtract), ["o_sb", "st8"], ["o2"])
            V(lambda e: e.tensor_tensor(o2[:], o2[:], st8[:, 3, :].unsqueeze(2).to_broadcast([128, 8, 256]), op=ALU.mult), ["o2", "st8"], ["o2"])
            gnb, gnn = bcload(ret_gn_g)
            V(lambda e, gnb=gnb: e.tensor_tensor(o2[:].rearrange("p a b -> p (a b)"), o2[:].rearrange("p a b -> p (a b)"), gnb[:], op=ALU.mult), ["o2", gnn], ["o2"])
            V(lambda e: e.tensor_tensor(retb[:], o2[:].rearrange("p a b -> p (a b)"), sg[:], op=ALU.mult), ["o2", "sg"], ["retb"])
            transpose16(retb, "retb", retT, "retT")
            for n in range(4):
                pga, pgan = pnext()
                proj(pga, pgan, 0, "w_bg", n * 512); proj(pga, pgan, 1, "w_bg", 2048 + n * 512)
                bb, bbn = bcload(b_bg[0:1, n * 512:(n + 1) * 512])
                bb2, bb2n = bcload(b_bg[0:1, 2048 + n * 512:2048 + (n + 1) * 512])
                V(lambda e, pga=pga, bb=bb: e.tensor_tensor(gA[:], pga[:, 0:512], bb[:, 0:512], op=ALU.add), [pgan, bbn], ["gA"])
                V(lambda e, pga=pga, bb2=bb2: e.tensor_tensor(gB[:], pga[:, 512:1024], bb2[:, 0:512], op=ALU.add), [pgan, bb2n], ["gB"])
                A(lambda e: e.activation(gA[:], gA[:], AF.Sigmoid), ["gA"], ["gA"])
                A(lambda e: e.activation(gB[:], gB[:], AF.Sigmoid), ["gB"], ["gB"])
                pyy, pyyn = pnext()
                proj(pyy, pyyn, 0, "w_a", n * 512, K=8, lhs=preAT, lhsn="preAT")
                proj(pyy, pyyn, 1, "w_b", n * 512, K=16, lhs=retT, lhsn="retT")
                V(lambda e, pyy=pyy: e.tensor_tensor(gA[:], gA[:], pyy[:, 0:512], op=ALU.mult), ["gA", pyyn], ["gA"])
                V(lambda e, pyy=pyy: e.tensor_tensor(gB[:], gB[:], pyy[:, 512:1024], op=ALU.mult), ["gB", pyyn], ["gB"])
                V(lambda e, n=n: e.tensor_tensor(mb[:, n * 512:(n + 1) * 512], gA[:], gB[:], op=ALU.add), ["gA", "gB"], ["mb"])
            transpose16(mb, "mb", mT, "mT")
            ga1, ga1n = adaload(2 * D)
            for j in range(2):
                pm, pmn = pnext()
                proj(pm, pmn, 0, "w_o", j * 1024, lhs=mT, lhsn="mT"); proj(pm, pmn, 1, "w_o", j * 1024 + 512, lhs=mT, lhsn="mT")
                V(lambda e, j=j, pm=pm, ga1=ga1: e.tensor_tensor(ga1[:, j * 1024:(j + 1) * 1024], ga1[:, j * 1024:(j + 1) * 1024], pm[:, :], op=ALU.mult),
                  [pmn, ga1n], [ga1n])
            V(lambda e, ga1=ga1: e.tensor_tensor(xt[:], xt[:], ga1[:], op=ALU.add), ["xt", ga1n], ["xt"])
            if dbg:
                ld(dbg_x1[i], xt[:], ["dbgx1_%d" % i], r=["xt"])

            norm_from(xt, "xt", norm2_g, 4 * D, 3 * D, out_f32=(h2, "h2"))
            for half in range(2):
                pq2, pq2n = pnext()
                for cc in range(2):
                    wv, wn = wload("wq", half * 2 + cc)
                    for c4 in range(4):
                        j = cc * 4 + c4
                        for kc in range(16):
                            P(lambda e, wv=wv, kc=kc, c4=c4, j=j, pq2=pq2: e.matmul(pq2[:, j * 128:(j + 1) * 128], wv[:, kc, c4 * 128:(c4 + 1) * 128], hT[:, kc, :],
                                                                                   start=(kc == 0), stop=(kc == 15)), ["hT"] + wn, [pq2n])
                A(lambda e, half=half, pq2=pq2: e.copy(qpT[:, half * 8:(half + 1) * 8, :].rearrange("p a b -> p (a b)"), pq2[:, :]), [pq2n], ["qpT"])
            for half in range(2):
                psc, pscn = pnext()
                for j in range(8):
                    c = half * 8 + j
                    P(lambda e, c=c, j=j, psc=psc: e.matmul(psc[:, j * 128:(j + 1) * 128], qpT[:, c, :], subkT[:, c, :], start=True, stop=True),
                      ["qpT", "subkT"], [pscn])
                A(lambda e, half=half, psc=psc: e.copy(sc_sb[:, half * 8:(half + 1) * 8, :].rearrange("p a b -> p (a b)"), psc[:, :]), [pscn], ["sc_sb"])
            for c in range(16):
                V(lambda e, c=c: e.max(out=v12[:, c, 0:8], in_=sc_sb[:, c, :]), ["sc_sb"], ["v12"])
                V(lambda e, c=c: e.max_index(out=i12[:, c, 0:8], in_max=v12[:, c, 0:8], in_values=sc_sb[:, c, :]), ["sc_sb", "v12"], ["i12"])
                V(lambda e, c=c: e.match_replace(out=wk[:, 0:128], in_to_replace=v12[:, c, 0:8], in_values=sc_sb[:, c, :], imm_value=-1e30), ["sc_sb", "v12"], ["wk"])
                V(lambda e, c=c: e.max(out=v12[:, c, 8:16], in_=wk[:, 0:128]), ["wk"], ["v12"])
                V(lambda e, c=c: e.max_index(out=i12[:, c, 8:16], in_max=v12[:, c, 8:16], in_values=wk[:, 0:128]), ["wk", "v12"], ["i12"])
            V(lambda e: e.tensor_copy(i12f[:], i12[:]), ["i12"], ["i12f"])
            v12v = v12[:].rearrange("p (h two) k -> p h two k", two=2)
            i12v = i12f[:].rearrange("p (h two) k -> p h two k", two=2)
            for h in range(8):
                V(lambda e, h=h: e.tensor_tensor(cand[:, h, :].rearrange("p (a b) -> p a b", a=16),
                                                 v12v[:, h, 0, :].unsqueeze(2).to_broadcast([128, 16, 16]),
                                                 v12v[:, h, 1, :].unsqueeze(1).to_broadcast([128, 16, 16]), op=ALU.add), ["v12"], ["cand"])
            for h in range(8):
                V(lambda e, h=h: e.max(out=top[:, h, 0:8], in_=cand[:, h, :]), ["cand"], ["top"])
                V(lambda e, h=h: e.max_index(out=pos[:, h, 0:8], in_max=top[:, h, 0:8], in_values=cand[:, h, :]), ["cand", "top"], ["pos"])
                V(lambda e, h=h: e.match_replace(out=wk[:], in_to_replace=top[:, h, 0:8], in_values=cand[:, h, :], imm_value=-1e30), ["cand", "top"], ["wk"])
                V(lambda e, h=h: e.max(out=top[:, h, 8:16], in_=wk[:]), ["wk"], ["top"])
                V(lambda e, h=h: e.max_index(out=pos[:, h, 8:16], in_max=top[:, h, 8:16], in_values=wk[:]), ["wk", "top"], ["pos"])
            V(lambda e: e.tensor_copy(pcor[:], pos[:]), ["pos"], ["pcor"])
            V(lambda e: e.tensor_scalar(pi32[:], pcor[:], 0.0625, None, op0=ALU.mult), ["pcor"], ["pi32"])
            V(lambda e: e.tensor_copy(paf[:], pi32[:]), ["pi32"], ["paf"])
            V(lambda e: e.scalar_tensor_tensor(pbf[:].rearrange("p h k -> p (h k)"), paf[:].rearrange("p h k -> p (h k)"), -16.0,
                                               pcor[:].rearrange("p h k -> p (h k)"), op0=ALU.mult, op1=ALU.add), ["paf", "pcor"], ["pbf"])
            V(lambda e: e.tensor_scalar(pcor[:], pbf[:], 0.0, None, op0=ALU.is_lt), ["pbf"], ["pcor"])
            V(lambda e: e.tensor_tensor(paf[:], paf[:], pcor[:], op=ALU.subtract), ["paf", "pcor"], ["paf"])
            V(lambda e: e.scalar_tensor_tensor(pbf[:].rearrange("p h k -> p (h k)"), pcor[:].rearrange("p h k -> p (h k)"), 16.0,
                                               pbf[:].rearrange("p h k -> p (h k)"), op0=ALU.mult, op1=ALU.add), ["pcor", "pbf"], ["pbf"])
            for (pf, pfn, two, dst, dstn) in ((paf, "paf", 0, i1s, "i1s"), (pbf, "pbf", 1, i2s, "i2s")):
                for h in range(8):
                    ohv = oh[:, h, :].rearrange("p (k a) -> p k a", k=16)
                    V(lambda e, h=h, pf=pf, ohv=ohv: e.tensor_tensor(ohv, iota16[:, :].unsqueeze(1).to_broadcast([128, 16, 16]),
                                                                     pf[:, h, :].unsqueeze(2).to_broadcast([128, 16, 16]), op=ALU.is_equal),
                      ["iota16", pfn], ["oh"])
                    V(lambda e, h=h, two=two, ohv=ohv: e.tensor_tensor(ohv, ohv, i12v[:, h, two, :].unsqueeze(1).to_broadcast([128, 16, 16]), op=ALU.mult),
                      ["oh", "i12f"], ["oh"])
                V(lambda e, dst=dst: e.tensor_reduce(dst[:].rearrange("p h k -> p (h k)"), oh[:].rearrange("p h (k a) -> p (h k) a", a=16), axis=AX.X, op=ALU.add),
                  ["oh"], [dstn])
            V(lambda e: e.scalar_tensor_tensor(i1s[:], i1s[:], 128.0, i2s[:], op0=ALU.mult, op1=ALU.add), ["i1s", "i2s"], ["i1s"])
            V(lambda e: e.tensor_copy(eidx[:], i1s[:].rearrange("p h k -> p (h k)")), ["i1s"], ["eidx"])
            V(lambda e: e.tensor_tensor(gate[:], top[:], top[:, :, 0:1].to_broadcast([128, 8, 16]), op=ALU.subtract), ["top"], ["gate"])
            A(lambda e: e.activation(gate[:], gate[:], AF.Exp), ["gate"], ["gate"])
            V(lambda e: e.tensor_reduce(gsum[:], gate[:], axis=AX.X, op=ALU.add), ["gate"], ["gsum"])
            V(lambda e: e.reciprocal(gsum[:], gsum[:]), ["gsum"], ["gsum"])
            V(lambda e: e.tensor_tensor(gate[:], gate[:], gsum[:, :].unsqueeze(2).to_broadcast([128, 8, 16]), op=ALU.mult), ["gate", "gsum"], ["gate"])
            for hk in range(128):
                gt, gn = gnext()
                S.dma(lambda e, gt=gt, hk=hk: e.indirect_dma_start(out=gt[:], out_offset=None, in_=peer_u,
                                                                  in_offset=bass.IndirectOffsetOnAxis(ap=eidx[:, hk:hk + 1], axis=0)),
                      reads=["eidx"], writes=[gn], queue="pool")
                V(lambda e, gt=gt, hk=hk: e.scalar_tensor_tensor(junk[:], gt[:], 1.0, h2[:], op0=ALU.mult, op1=ALU.mult, accum_out=acol[:, hk:hk + 1]),
                  [gn, "h2"], ["junk", "acol"])
            A(lambda e: e.activation(wgt[:], acol[:], AF.Gelu), ["acol"], ["wgt"])
            V(lambda e: e.tensor_tensor(wgt[:], wgt[:], gate[:].rearrange("p h k -> p (h k)"), op=ALU.mult), ["wgt", "gate"], ["wgt"])
            for hk in range(128):
                gt, gn = gnext()
                S.dma(lambda e, gt=gt, hk=hk: e.indirect_dma_start(out=gt[:], out_offset=None, in_=peer_v,
                                                                  in_offset=bass.IndirectOffsetOnAxis(ap=eidx[:, hk:hk + 1], axis=0)),
                      reads=["eidx"], writes=[gn], queue="pool")
                if hk == 0:
                    V(lambda e, gt=gt: e.tensor_scalar(y[:], gt[:], wgt[:, 0:1], None, op0=ALU.mult), [gn, "wgt"], ["y"])
                else:
                    V(lambda e, gt=gt, hk=hk: e.scalar_tensor_tensor(y[:], gt[:], wgt[:, hk:hk + 1], y[:], op0=ALU.mult, op1=ALU.add), [gn, "wgt", "y"], ["y"])
            if dbg:
                ld(dbg_y[i], y[:], ["dbgy_%d" % i], r=["y"])
            ga2, ga2n = adaload(5 * D)
            V(lambda e, ga2=ga2: e.tensor_tensor(y[:], y[:], ga2[:], op=ALU.mult), ["y", ga2n], ["y"])
            V(lambda e: e.tensor_tensor(xt[:], xt[:], y[:], op=ALU.add), ["xt", "y"], ["xt"])
            A(lambda e: e.activation(junk[:], xt[:], AF.Square, accum_out=ss[:, 0:1]), ["xt"], ["junk", "ss"])
            V(lambda e: e.tensor_scalar(rstd[:, 0:1], ss[:, 0:1], 1.0 / D, EPS, op0=ALU.mult, op1=ALU.add), ["ss"], ["rstd"])
            A(lambda e: e.activation(rstd[:, 1:2], rstd[:, 0:1], AF.Sqrt), ["rstd"], ["rstd"])
            V(lambda e: e.reciprocal(rstd[:, 2:3], rstd[:, 1:2]), ["rstd"], ["rstd"])
            gf, gfn = bcload(norm_f_g)
            V(lambda e, gf=gf: e.scalar_tensor_tensor(y[:], xt[:], rstd[:, 2:3], gf[:], op0=ALU.mult, op1=ALU.mult), ["xt", "rstd", gfn], ["y"])
            ld(out_d[i], y[:], ["out%d" % i], r=["y"])
        fin = ["out%d" % i for i in range(NOWN)] + ([n % i for i in range(NOWN) for n in ("dbgx1_%d", "dbgy_%d")] if dbg else [])
        S.final_wait("sp", fin)
        S.emit()
    return nc


def _consts(seg):
    gam = np.array([1.0 - 2.0 ** (-5.0 - h) for h in range(H)], dtype=np.float64)
    scale = 128.0 ** -0.5
    i = np.arange(128)
    MT = np.zeros((128, H, 128), dtype=np.float64)
    ci, cj = i[None, :] // 64, i[:, None] // 64
    dist = np.abs(i[None, :] - i[:, None]).astype(np.float64)
    allowed = (ci >= cj)
    for h in range(H):
        MT[:, h, :] = np.where(allowed, gam[h] ** dist, 0.0) * scale
    qdecT = np.broadcast_to((gam[None, :, None] ** (i[None, None, :] + 1.0)), (128, H, 128))
    kdec = (gam[None, :] ** (127.0 - i[:, None])) * scale
    kdecP = np.zeros((128, NPRE, H), dtype=np.float64)
    P0 = seg * 1024
    for p in range(NPRE):
        gt = seg * 8 - NPRE + p
        if gt < 0:
            continue
        tok = gt * 128 + i
        kdecP[:, p, :] = (gam[None, :] ** (P0 - 1.0 - tok[:, None])) * scale
    f32 = lambda a: np.ascontiguousarray(a, dtype=np.float32)
    return f32(MT), f32(qdecT), f32(kdec), f32(kdecP)


def _in_maps(inputs):
    x = np.asarray(inputs["x"], dtype=np.float32)
    c = np.asarray(inputs["c"], dtype=np.float32)
    positions = np.asarray(inputs["positions"]).astype(np.int32)
    g = lambda k: np.asarray(inputs[k], dtype=np.float32)
    shared = {
        "w_ada": np.ascontiguousarray(g("w_ada")[0]),
        "b_ada": np.ascontiguousarray(g("b_ada")[0][None, :]),
        "norm1_g": np.ascontiguousarray(g("norm1_g")[0][None, :]),
        "w_in": np.ascontiguousarray(g("w_in")[0]),
        "w_bg": np.ascontiguousarray(g("w_branch_gate")[0]),
        "b_bg": np.ascontiguousarray(g("b_branch_gate")[0][None, :]),
        "gm_v_g": np.ascontiguousarray(g("gm_v_g")[0][None, :]),
        "wsT": np.ascontiguousarray(g("gm_ws")[0].transpose(2, 0, 1)),
        "gm_bT": np.ascontiguousarray(g("gm_b")[0].T),
        "ret_gn_g": np.ascontiguousarray(g("ret_gn_g")[0][None, :]),
        "w_a": np.ascontiguousarray(g("w_a_out")[0]),
        "w_b": np.ascontiguousarray(g("w_b_out")[0]),
        "w_o": np.ascontiguousarray(g("w_o")[0]),
        "norm2_g": np.ascontiguousarray(g("norm2_g")[0][None, :]),
        "wq": np.ascontiguousarray(g("peer_wq")[0]),
        "subkT": np.ascontiguousarray(g("peer_subkeys")[0].reshape(16, 128, 128).transpose(2, 0, 1)),
        "peer_u": np.ascontiguousarray(g("peer_u")[0]),
        "peer_v": np.ascontiguousarray(g("peer_v")[0]),
        "norm_f_g": np.ascontiguousarray(g("norm_f_g")[None, :]),
        "ident": np.eye(128, dtype=np.float32),
        "freq": np.ascontiguousarray(np.broadcast_to((10000.0 ** (-np.arange(64, dtype=np.float32) / 64)).astype(np.float32)[None, :], (128, 64))),
        "iota16": np.ascontiguousarray(np.broadcast_to(np.arange(16, dtype=np.float32)[None, :], (128, 16))),
    }
    maps = []
    for core in range(8):
        b, seg = core // 4, core % 4
        xsl = np.zeros((NPRE + NOWN, 128, D), dtype=np.float32)
        pos = np.zeros((128, NPRE + NOWN), dtype=np.int32)
        for p in range(NPRE + NOWN):
            gt = seg * 8 - NPRE + p
            if gt < 0:
                continue
            xsl[p] = x[b, gt * 128:(gt + 1) * 128]
            pos[:, p] = positions[b, gt * 128:(gt + 1) * 128]
        MT, qdecT, kdec, kdecP = _consts(seg)
        m = dict(shared)
        m.update({"xs": xsl, "posi": pos, "cT": np.ascontiguousarray(c[b].reshape(16, 128).T),
                  "MT": MT, "qdecT": qdecT, "kdec": kdec, "kdecP": kdecP})
        maps.append(m)
    return maps


_DBG = bool(int(os.environ.get("KDBG", "0")))
_last = {}


def kernel(**inputs):
    nc = build_program(dbg=_DBG)
    maps = _in_maps(inputs)
    res = run_bass_kernel_spmd(nc, maps, core_ids=list(range(8)))
    out = np.zeros((2, 4096, D), dtype=np.float32)
    for core in range(8):
        b, seg = core // 4, core % 4
        out[b, seg * 1024:(seg + 1) * 1024] = res.results[core]["out"].reshape(1024, D)
    if _DBG:
        _last["res"] = res.results
    return out
```

```python
import os
from contextlib import ExitStack
import numpy as np
import concourse.bass as bass
import concourse.mybir as mybir
from concourse.bass_utils import run_bass_kernel_spmd

F32 = mybir.dt.float32
BF16 = mybir.dt.bfloat16
I32 = mybir.dt.int32
U32 = mybir.dt.uint32
AF = mybir.ActivationFunctionType
ALU = mybir.AluOpType
AX = mybir.AxisListType

D = 2048
NPRE = 24
NOWN = 8
EPS = 1e-6
H = 8
ENGS = ("pe", "act", "dve", "pool", "sp")
NDMA_SEMS = 28
TWO_PI = float(2 * np.pi)


class Sched:
    def __init__(self, nc, stack):
        self.nc = nc
        self.stack = stack
        self.ops = {e: [] for e in ENGS}
        self.cnt = {e: 0 for e in ENGS}
        self.sem = {e: stack.enter_context(nc.semaphore("s_" + e)) for e in ENGS if e != "sp"}
        self.dsem = [stack.enter_context(nc.semaphore("d%d" % i)) for i in range(NDMA_SEMS)]
        self.dcnt = [0] * NDMA_SEMS
        half = NDMA_SEMS // 2
        self.dpool = {"sp": list(range(0, half)), "act": list(range(0, half)), "pool": list(range(half, NDMA_SEMS))}
        self.dnext = {"sp": 0, "act": 0, "pool": 0}
        self.waited = {e: {} for e in ENGS}
        self.lastw = {}
        self.readers = {}
        self.alias = {}

    def _x(self, names):
        out = []
        for n in names:
            out.extend(self.alias.get(n, [n]))
        return out

    def sb(self, name, shape, dtype=F32):
        return self.stack.enter_context(self.nc.sbuf_tensor("sb_" + name, list(shape), dtype))

    def ps(self, name, shape, dtype=F32):
        return self.stack.enter_context(self.nc.psum_tensor("ps_" + name, list(shape), dtype))

    def _semobj(self, key):
        return self.sem[key] if isinstance(key, str) else self.dsem[key]

    def _deps(self, eng, reads, writes):
        need = {}

        def add(k, v, same_ok):
            if k == eng and not same_ok and eng == "pe":
                return
            if need.get(k, 0) < v:
                need[k] = v

        for r in reads:
            if r in self.lastw:
                k, v = self.lastw[r]
                add(k, v, True)
        for w in writes:
            if w in self.lastw:
                k, v = self.lastw[w]
                add(k, v, False)
            for k, v in self.readers.get(w, {}).items():
                add(k, v, False)
        waits = []
        wd = self.waited[eng]
        for k, v in need.items():
            if wd.get(k, 0) >= v:
                continue
            wd[k] = v
            waits.append((k, v))
        return waits

    def _record(self, key, val, reads, writes):
        for r in reads:
            self.readers.setdefault(r, {})[key] = val
        for w in writes:
            self.lastw[w] = (key, val)
            self.readers[w] = {}

    def op(self, eng, fn, reads=(), writes=()):
        reads, writes = self._x(reads), self._x(writes)
        waits = self._deps(eng, reads, writes)
        self.cnt[eng] += 1
        self.ops[eng].append((waits, fn, (eng, 1)))
        self._record(eng, self.cnt[eng], reads, writes)

    def dma(self, fn, reads=(), writes=(), queue="sp"):
        reads, writes = self._x(reads), self._x(writes)
        qp = self.dpool[queue]
        i = qp[self.dnext[queue] % len(qp)]
        self.dnext[queue] += 1
        waits = self._deps(queue, reads, writes)
        if self.dcnt[i] > 0 and self.waited[queue].get(i, 0) < self.dcnt[i]:
            self.waited[queue][i] = self.dcnt[i]
            waits.append((i, self.dcnt[i]))
        self.dcnt[i] += 16
        self.ops[queue].append((waits, fn, (i, 16)))
        self._record(i, self.dcnt[i], reads, writes)

    def final_wait(self, eng, reslist):
        waits = self._deps(eng, self._x(reslist), ())
        self.ops[eng].append((waits, None, None))

    def emit(self):
        nc = self.nc
        with nc.Block() as block:
            def run(ename):
                def body(e):
                    for waits, fn, inc in self.ops[ename]:
                        for k, v in waits:
                            e.wait_ge(self._semobj(k), v)
                        if fn is None:
                            continue
                        ins = fn(e)
                        ins.then_inc(self._semobj(inc[0]), inc[1])
                return body
            block.tensor(run("pe"))
            block.scalar(run("act"))
            block.vector(run("dve"))
            block.gpsimd(run("pool"))
            block.sync(run("sp"))


def build_program(dbg=False):
    nc = bass.Bass("TRN2", target_bir_lowering=False)

    def din(name, shape, dt=F32):
        return nc.dram_tensor(name, list(shape), dt, kind="ExternalInput").ap()

    xs = din("xs", [NPRE + NOWN, 128, D])
    posi = din("posi", [128, NPRE + NOWN], I32)
    cT_d = din("cT", [128, 16])
    w_ada = din("w_ada", [D, 6 * D])
    b_ada = din("b_ada", [1, 6 * D])
    norm1_g = din("norm1_g", [1, D])
    w_in = din("w_in", [D, 8192])
    w_bg = din("w_bg", [D, 4096])
    b_bg = din("b_bg", [1, 4096])
    gm_v_g = din("gm_v_g", [1, 1024])
    wsT_d = din("wsT", [128, 8, 128])
    gm_bT_d = din("gm_bT", [128, 8])
    ret_gn_g = din("ret_gn_g", [1, D])
    w_a = din("w_a", [1024, D])
    w_b = din("w_b", [D, D])
    w_o = din("w_o", [D, D])
    norm2_g = din("norm2_g", [1, D])
    wq = din("wq", [D, D])
    subkT_d = din("subkT", [128, 16, 128])
    peer_u = din("peer_u", [16384, D])
    peer_v = din("peer_v", [16384, D])
    norm_f_g = din("norm_f_g", [1, D])
    ident_d = din("ident", [128, 128])
    freq_d = din("freq", [128, 64])
    iota16_d = din("iota16", [128, 16])
    MT_d = din("MT", [128, 8, 128])
    qdecT_d = din("qdecT", [128, 8, 128])
    kdec_d = din("kdec", [128, 8])
    kdecP_d = din("kdecP", [128, NPRE, 8])
    out_d = nc.dram_tensor("out", [NOWN, 128, D], F32, kind="ExternalOutput").ap()
    ada_d = nc.dram_tensor("ada_scr", [1, 6 * D], F32, kind="Internal").ap()
    WSPEC = {"w_in": (w_in, 16, 16), "w_bg": (w_bg, 16, 8), "w_a": (w_a, 8, 4), "w_b": (w_b, 16, 4), "w_o": (w_o, 16, 4), "wq": (wq, 16, 4)}
    wscr = {k: nc.dram_tensor("wb_" + k, [v[2], 128, v[1] * 512], BF16, kind="Internal").ap() for k, v in WSPEC.items()}
    if dbg:
        dbg_x1 = nc.dram_tensor("dbg_x1", [NOWN, 128, D], F32, kind="ExternalOutput").ap()
        dbg_y = nc.dram_tensor("dbg_y", [NOWN, 128, D], F32, kind="ExternalOutput").ap()

    GAM = [1.0 - 2.0 ** (-5.0 - h) for h in range(H)]

    with ExitStack() as st:
        S = Sched(nc, st)
        sb, ps = S.sb, S.ps

        ident = sb("ident", [128, 128]); identb = sb("identb", [128, 128], BF16)
        freq = sb("freq", [128, 64]); iota16 = sb("iota16", [128, 16])
        MT = sb("MT", [128, 8, 128]); qdecT = sb("qdecT", [128, 8, 128])
        kdec = sb("kdec", [128, 8]); kdecP = sb("kdecP", [128, NPRE, 8])
        wsT = sb("wsT", [128, 8, 128], BF16)
        gmbT = sb("gmbT", [128, 8]); subkT = sb("subkT", [128, 16, 128])
        posI = sb("posI", [128, NPRE + NOWN], I32); posF = sb("posF", [128, NPRE + NOWN])
        cT = sb("cT", [128, 16]); scT = sb("scT", [128, 16], BF16)

        NW = 3
        wring = [sb("w%d" % i, [128, 8192], BF16) for i in range(NW)]
        wr_i = [0]

        def wnext():
            i = wr_i[0]; wr_i[0] = (i + 1) % NW
            return wring[i], ["w%da" % i, "w%db" % i]

        NBC = 2
        bcring = [sb("bc%d" % i, [128, D]) for i in range(NBC)]
        bc_i = [0]

        def bcnext():
            i = bc_i[0]; bc_i[0] = (i + 1) % NBC
            return bcring[i], "bc%d" % i

        NP = 4
        pring = [ps("P%d" % i, [128, 1024]) for i in range(NP)]
        p_i = [0]

        def pnext():
            i = p_i[0]; p_i[0] = (i + 1) % NP
            return pring[i], "P%d" % i

        xt = sb("xt", [128, D])
        ss = sb("ss", [128, 8]); rstd = sb("rstd", [128, 8])
        hb = sb("hb", [128, D], BF16); hT = sb("hT", [128, 16, 128], BF16)
        junk = hb; S.alias["junk"] = ["hb"]
        cs = sb("cs", [128, 4, 64])
        ki = sb("ki", [128, 64], I32); kf = sb("kf", [128, 2, 64])
        pi32 = sb("pi32", [128, 8, 16], I32); pcor = sb("pcor", [128, 8, 16])
        Sf = sb("Sf", [128, 8, 256]); Sb = sb("Sb", [128, 8, 256], BF16)
        st8 = sb("st8", [128, 4, 8])
        wk = sb("wk", [128, 256])
        v12 = sb("v12", [128, 16, 16]); i12 = sb("i12", [128, 16, 16], U32); i12f = sb("i12f", [128, 16, 16])
        top = sb("top", [128, 8, 16]); pos = sb("pos", [128, 8, 16], U32)
        paf = sb("paf", [128, 8, 16]); pbf = sb("pbf", [128, 8, 16])
        i1s = sb("i1s", [128, 8, 16]); i2s = sb("i2s", [128, 8, 16])
        eidx = sb("eidx", [128, 128], I32)
        gate = sb("gate", [128, 8, 16]); gsum = sb("gsum", [128, 8])
        acol = sb("acol", [128, 128]); wgt = sb("wgt", [128, 128])

        ARENA = 80 * 1024
        GRAN = 2048
        arena = sb("arena", [128, ARENA // 4])
        DSZ = {F32: 4, BF16: 2, I32: 4, U32: 4}

        def carve(off, name, shape, dtype=F32, parts=128):
            nel = int(np.prod(shape[1:]))
            nbytes = nel * DSZ[dtype]
            assert off % 4 == 0 and off + nbytes <= ARENA, (name, off, nbytes)
            v = arena[0:parts, off // 4:(off + nbytes) // 4]
            if dtype != F32:
                v = v.bitcast(dtype)
            if len(shape) == 3:
                v = v.rearrange("p (a b) -> p a b", a=shape[1])
            S.alias[name] = ["ar%d" % g for g in range(off // GRAN, (off + nbytes + GRAN - 1) // GRAN)]
            return v, off + ((nbytes + GRAN - 1) // GRAN) * GRAN

        K = 1024
        sg, o = carve(0, "sg", [128, D])
        rotA, o = carve(o, "rotA", [128, 8, 128]); rotB, o2_off = carve(o, "rotB", [128, 8, 128])
        o_sb, _ = carve(o - 4 * K, "o_sb", [128, 8, 256])
        o = o2_off
        u_sb, o = carve(o, "u_sb", [128, 8, 128]); v_f, o = carve(o, "v_f", [128, 8, 128])
        o2, _ = carve(o - 8 * K, "o2", [128, 8, 256])
        qb, o = carve(o, "qb", [128, 8, 128], BF16); kb, o = carve(o, "kb", [128, 8, 128], BF16)
        kd, o = carve(o, "kd", [128, 8, 128], BF16); qT, o = carve(o, "qT", [128, 8, 128], BF16)
        qdT, o = carve(o, "qdT", [128, 8, 128], BF16); kT, o = carve(o, "kT", [128, 8, 128], BF16)
        vr, o = carve(o, "vr", [128, 8, 256], BF16)
        v_b, o = carve(o, "v_b", [128, 8, 128], BF16); preA, o = carve(o, "preA", [128, 8, 128], BF16)
        preAT, o = carve(o, "preAT", [128, 8, 128], BF16); attm, o = carve(o, "attm", [128, 8, 128], BF16)
        retb, o = carve(o, "retb", [128, D], BF16); retT, o = carve(o, "retT", [128, 16, 128], BF16)
        mb, o = carve(o, "mb", [128, D], BF16); mT, o = carve(o, "mT", [128, 16, 128], BF16)
        gA, o = carve(o, "gA", [128, 512]); gB, o = carve(o, "gB", [128, 512])
        xt2, _ = carve(40 * K, "xt2", [128, D]); hb2, _ = carve(48 * K, "hb2", [128, D], BF16)
        hT2, _ = carve(52 * K, "hT2", [128, 16, 128], BF16)
        A1p, _ = carve(56 * K, "A1p", [128, D]); sh1p, _ = carve(64 * K, "sh1p", [128, D])
        cvS = [carve(0, "cvS0", [128, 8, 512], BF16)[0], carve(16 * K, "cvS1", [128, 8, 512], BF16)[0]]
        brow, _ = carve(0, "brow", [1, 512], parts=1); arow, _ = carve(2 * K, "arow", [1, 512], parts=1)
        h2, o = carve(0, "h2", [128, D]); y, o = carve(o, "y", [128, D])
        qpT, o = carve(o, "qpT", [128, 16, 128]); sc_sb, o = carve(o, "sc_sb", [128, 16, 128])
        cand, o = carve(o, "cand", [128, 8, 256]); oh, o = carve(o, "oh", [128, 8, 256])
        NG = 4
        gring = []
        for gi in range(NG):
            gv, o = carve(o, "g%d" % gi, [128, D])
            gring.append(gv)
        for gi in range(4):
            gv, _ = carve(16 * K + gi * 8 * K, "g%d" % (NG + gi), [128, D])
            gring.append(gv)
        NG = 8
        g_i = [0]

        def gnext():
            i = g_i[0]; g_i[0] = (i + 1) % NG
            return gring[i], "g%d" % i

        def V(fn, r, w):
            S.op("dve", fn, r, w)

        def A(fn, r, w):
            S.op("act", fn, r, w)

        def P(fn, r, w):
            S.op("pe", fn, r, w)

        def G(fn, r, w):
            S.op("pool", fn, r, w)

        def ld(out_ap, in_ap, w, queue="sp", r=()):
            S.dma(lambda e: e.dma_start(out=out_ap, in_=in_ap), reads=r, writes=w, queue=queue)

        def bcload(src_row, r=()):
            t, n = bcnext()
            width = src_row.shape[1]
            ld(t[:, 0:width], src_row.partition_broadcast(128)[:, 0, :], [n], r=r)
            return t, n

        def adaload(off):
            return bcload(ada_d[0:1, off:off + D], r=["ada%d" % k for k in range(off // 512, off // 512 + 4)])

        def wload_cast(src, K, N=512):
            t, n = wnext()
            view = t[:, 0:K * N].rearrange("p (k n) -> p k n", k=K)
            ld(view, src.rearrange("(k p) n -> p k n", p=128), n, queue="pool")
            return view, n

        def wload(name, j):
            K_ = WSPEC[name][1]
            t, n = wnext()
            ld(t[:, 0:K_ * 512], wscr[name][j], n, r=wb_res[(name, j)])
            return t[:, 0:K_ * 512].rearrange("p (k n) -> p k n", k=K_), n

        ld(ident[:], ident_d, ["ident"]); ld(freq[:], freq_d, ["freq"]); ld(iota16[:], iota16_d, ["iota16"])
        ld(MT[:], MT_d, ["MT"]); ld(qdecT[:], qdecT_d, ["qdecT"]); ld(kdec[:], kdec_d, ["kdec"])
        ld(kdecP[:], kdecP_d, ["kdecP"]); ld(wsT[:], wsT_d, ["wsT"], queue="pool"); ld(gmbT[:], gm_bT_d, ["gmbT"])
        ld(subkT[:], subkT_d, ["subkT"]); ld(posI[:], posi, ["posI"]); ld(cT[:], cT_d, ["cT"])
        V(lambda e: e.tensor_copy(identb[:], ident[:]), ["ident"], ["identb"])
        V(lambda e: e.tensor_copy(posF[:], posI[:]), ["posI"], ["posF"])
        V(lambda e: e.memset(wsT[64:128, :, 0:64], 0.0), ["wsT"], ["wsT"])
        V(lambda e: e.memset(Sf[:], 0.0), [], ["Sf"])

        wb_res = {}
        EARLY = [("w_in", j) for j in range(6, 12)]
        for name, j in EARLY:
            wsrc_, K_, nch = WSPEC[name]
            view, n = wload_cast(wsrc_[:, j * 512:(j + 1) * 512], K_)
            wb_res[(name, j)] = ["wb_%s_%d" % (name, j)]
            ld(wscr[name][j], view.rearrange("p k n -> p (k n)"), wb_res[(name, j)], r=n)
        late_jobs = []
        for name, (wsrc_, K_, nch) in WSPEC.items():
            for j in range(nch):
                if (name, j) in EARLY:
                    continue
                wb_res[(name, j)] = ["wb_%s_%d_%d" % (name, j, h) for h in range(K_ // 8)]
                for h in range(K_ // 8):
                    late_jobs.append((name, j, h))
        cv_i = [0]

        def emit_late(count):
            for _ in range(count):
                if not late_jobs:
                    return
                name, j, h = late_jobs.pop(0)
                wsrc_ = WSPEC[name][0]
                buf = cvS[cv_i[0] % 2]; bn = "cvS%d" % (cv_i[0] % 2); cv_i[0] += 1
                ld(buf[:], wsrc_[h * 1024:(h + 1) * 1024, j * 512:(j + 1) * 512].rearrange("(k p) n -> p k n", p=128), [bn], queue="pool")
                ld(wscr[name][j][:, h * 4096:(h + 1) * 4096], buf[:].rearrange("p k n -> p (k n)"), ["wb_%s_%d_%d" % (name, j, h)], r=[bn])

        A(lambda e: e.activation(scT[:], cT[:], AF.Silu), ["cT"], ["scT"])
        for n in range(24):
            pt, pn = pnext()
            wv, wn = wload_cast(w_ada[:, n * 512:(n + 1) * 512], 16)
            for kc in range(16):
                P(lambda e, wv=wv, kc=kc, pt=pt: e.matmul(pt[0:1, 0:512], scT[:, kc:kc + 1], wv[:, kc, :],
                                                          start=(kc == 0), stop=(kc == 15)), ["scT"] + wn, [pn])
            ld(brow[:], b_ada[0:1, n * 512:(n + 1) * 512], ["brow"])
            V(lambda e, pt=pt: e.tensor_tensor(arow[:], pt[0:1, 0:512], brow[:], op=ALU.add), [pn, "brow"], ["arow"])
            ld(ada_d[0:1, n * 512:(n + 1) * 512], arow[:], ["ada%d" % n], r=["arow"])

        def load_x_norm(tile_idx, g_row, sc_off, sh_off, out_f32=None):
            ld(xt[:], xs[tile_idx], ["xt"])
            norm_from(xt, "xt", g_row, sc_off, sh_off, out_f32)

        def prefix_norm(tile_idx, par):
            x_, xn_, hb_, hbn_, hT_, hTn_ = (xt, "xt", hb, "hb", hT, "hT") if par == 0 else (xt2, "xt2", hb2, "hb2", hT2, "hT2")
            ld(x_[:], xs[tile_idx], [xn_])
            c0 = 4 * par
            A(lambda e: e.activation(hb_[:], x_[:], AF.Square, accum_out=ss[:, c0:c0 + 1]), [xn_], [hbn_, "ss"])
            V(lambda e: e.tensor_scalar(rstd[:, c0:c0 + 1], ss[:, c0:c0 + 1], 1.0 / D, EPS, op0=ALU.mult, op1=ALU.add), ["ss"], ["rstd"])
            A(lambda e: e.activation(rstd[:, c0 + 1:c0 + 2], rstd[:, c0:c0 + 1], AF.Sqrt), ["rstd"], ["rstd"])
            V(lambda e: e.reciprocal(rstd[:, c0 + 2:c0 + 3], rstd[:, c0 + 1:c0 + 2]), ["rstd"], ["rstd"])
            tmp, tmpn = bcnext()
            V(lambda e: e.scalar_tensor_tensor(tmp[:], x_[:], rstd[:, c0 + 2:c0 + 3], A1p[:], op0=ALU.mult, op1=ALU.mult), [xn_, "rstd", "A1p"], [tmpn])
            V(lambda e: e.tensor_tensor(hb_[:], tmp[:], sh1p[:], op=ALU.add), [tmpn, "sh1p"], [hbn_])
            transpose16(hb_, hbn_, hT_, hTn_)
            return hT_, hTn_

        def norm_from(src, srcn, g_row, sc_off, sh_off, out_f32=None):
            A(lambda e: e.activation(junk[:], src[:], AF.Square, accum_out=ss[:, 0:1]), [srcn], ["junk", "ss"])
            V(lambda e: e.tensor_scalar(rstd[:, 0:1], ss[:, 0:1], 1.0 / D, EPS, op0=ALU.mult, op1=ALU.add), ["ss"], ["rstd"])
            A(lambda e: e.activation(rstd[:, 1:2], rstd[:, 0:1], AF.Sqrt), ["rstd"], ["rstd"])
            V(lambda e: e.reciprocal(rstd[:, 2:3], rstd[:, 1:2]), ["rstd"], ["rstd"])
            gt, gn = bcload(g_row)
            sct, scn = adaload(sc_off)
            V(lambda e: e.scalar_tensor_tensor(sct[:], sct[:], 1.0, gt[:], op0=ALU.add, op1=ALU.mult), [gn, scn], [scn])
            sht, shn = adaload(sh_off)
            V(lambda e: e.scalar_tensor_tensor(sct[:], src[:], rstd[:, 2:3], sct[:], op0=ALU.mult, op1=ALU.mult), [srcn, "rstd", scn], [scn])
            if out_f32 is not None:
                V(lambda e: e.tensor_tensor(out_f32[0][:], sct[:], sht[:], op=ALU.add), [scn, shn], [out_f32[1]])
                A(lambda e: e.copy(hb[:], out_f32[0][:]), [out_f32[1]], ["hb"])
            else:
                V(lambda e: e.tensor_tensor(hb[:], sct[:], sht[:], op=ALU.add), [scn, shn], ["hb"])
            transpose16(hb, "hb", hT, "hT")

        def transpose16(src, srcn, dst, dstn, nchunks=16):
            for half in range((nchunks + 7) // 8):
                pt, pn = pnext()
                pv = pt[:].bitcast(BF16)
                cnt = min(8, nchunks - half * 8)
                for j in range(cnt):
                    c = half * 8 + j
                    P(lambda e, pv=pv, j=j, c=c: e.transpose(pv[:, j * 128:(j + 1) * 128], src[:, c * 128:(c + 1) * 128], identb[:]),
                      [srcn, "identb"], [pn])
                A(lambda e, pv=pv, half=half, cnt=cnt: e.copy(dst[:, half * 8:half * 8 + cnt, :].rearrange("p a b -> p (a b)"), pv[:, 0:cnt * 128]),
                  [pn], [dstn])

        def rope_tables(tile_idx):
            pcol = posF[:, tile_idx:tile_idx + 1]
            PI = float(np.pi)
            for (shift, dsti) in ((0.0, 1), (PI / 2, 0)):
                V(lambda e, shift=shift: e.tensor_scalar(cs[:, 3, :], freq[:], pcol, shift, op0=ALU.mult, op1=ALU.add), ["freq", "posF", "cs"], ["cs3"])
                V(lambda e: e.tensor_scalar(ki[:], cs[:, 3, :], 1.0 / TWO_PI, None, op0=ALU.mult), ["cs3"], ["ki"])
                V(lambda e: e.tensor_copy(kf[:, 0, :], ki[:]), ["ki"], ["kf"])
                V(lambda e: e.scalar_tensor_tensor(cs[:, 3, :], kf[:, 0, :], -TWO_PI, cs[:, 3, :], op0=ALU.mult, op1=ALU.add), ["kf", "cs3"], ["cs3"])
                V(lambda e: e.tensor_scalar(kf[:, 1, :], cs[:, 3, :], PI, -TWO_PI, op0=ALU.is_gt, op1=ALU.mult), ["cs3"], ["kf"])
                V(lambda e: e.tensor_tensor(cs[:, 3, :], cs[:, 3, :], kf[:, 1, :], op=ALU.add), ["kf", "cs3"], ["cs3"])
                V(lambda e: e.tensor_scalar(cs[:, 3, :], cs[:, 3, :], -PI, PI, op0=ALU.max, op1=ALU.min), ["cs3"], ["cs3"])
                A(lambda e, dsti=dsti: e.activation(cs[:, dsti, :], cs[:, 3, :], AF.Sin), ["cs3"], ["cs"])
            V(lambda e: e.tensor_scalar(cs[:, 2, :], cs[:, 1, :], -1.0, None, op0=ALU.mult), ["cs"], ["cs"])

        def rope(pt, pn, nh, dst, dstn):
            pv = pt[:, 0:nh * 128].rearrange("p (h d) -> p h d", h=nh)
            cosb = cs[:, 0, :].unsqueeze(1).to_broadcast([128, nh, 64])
            sinb = cs[:, 1, :].unsqueeze(1).to_broadcast([128, nh, 64])
            nsinb = cs[:, 2, :].unsqueeze(1).to_broadcast([128, nh, 64])
            V(lambda e: e.tensor_tensor(rotA[:, 0:nh, 0:64], pv[:, :, 0:64], cosb, op=ALU.mult), [pn, "cs"], ["rotA"])
            V(lambda e: e.tensor_tensor(rotA[:, 0:nh, 64:128], pv[:, :, 64:128], cosb, op=ALU.mult), [pn, "cs"], ["rotA"])
            V(lambda e: e.tensor_tensor(rotB[:, 0:nh, 0:64], pv[:, :, 64:128], nsinb, op=ALU.mult), [pn, "cs"], ["rotB"])
            V(lambda e: e.tensor_tensor(rotB[:, 0:nh, 64:128], pv[:, :, 0:64], sinb, op=ALU.mult), [pn, "cs"], ["rotB"])
            V(lambda e: e.tensor_tensor(dst[:, 0:nh, :], rotA[:, 0:nh, :], rotB[:, 0:nh, :], op=ALU.add), ["rotA", "rotB"], [dstn])

        def proj(pt, pn, col, wname, c0, K=16, lhs=None, lhsn="hT"):
            lhs = hT if lhs is None else lhs
            wv, wn = wload(wname, c0 // 512)
            for kc in range(K):
                P(lambda e, wv=wv, kc=kc: e.matmul(pt[:, col * 512:(col + 1) * 512], lhs[:, kc, :], wv[:, kc, :],
                                                   start=(kc == 0), stop=(kc == K - 1)), [lhsn] + wn, [pn])

        gt_, gn_ = bcload(norm1_g)
        ld(A1p[:], ada_d[0:1, D:2 * D].partition_broadcast(128)[:, 0, :], ["A1p"], r=["ada%d" % k for k in range(4, 8)])
        V(lambda e: e.scalar_tensor_tensor(A1p[:], A1p[:], 1.0, gt_[:], op0=ALU.add, op1=ALU.mult), ["A1p", gn_], ["A1p"])
        ld(sh1p[:], ada_d[0:1, 0:D].partition_broadcast(128)[:, 0, :], ["sh1p"], r=["ada%d" % k for k in range(0, 4)])
        for hh in range(2):
            wk_v, wk_n = wload("w_in", 6 + hh)
            wv0, wv0n = wload("w_in", 8 + hh * 2)
            wv1, wv1n = wload("w_in", 9 + hh * 2)
            for p in range(NPRE):
                hT_, hTn_ = prefix_norm(p, p % 2)
                rope_tables(p)
                pk, pkn = pnext()
                for kc in range(16):
                    P(lambda e, kc=kc, pk=pk, hT_=hT_: e.matmul(pk[:, 0:512], hT_[:, kc, :], wk_v[:, kc, :], start=(kc == 0), stop=(kc == 15)),
                      [hTn_] + wk_n, [pkn])
                pv_, pvn = pnext()
                for j, (wv, wn) in enumerate(((wv0, wv0n), (wv1, wv1n))):
                    for kc in range(16):
                        P(lambda e, kc=kc, j=j, wv=wv, pv_=pv_, hT_=hT_: e.matmul(pv_[:, j * 512:(j + 1) * 512], hT_[:, kc, :], wv[:, kc, :],
                                                                         start=(kc == 0), stop=(kc == 15)), [hTn_] + wn, [pvn])
                rope(pk, pkn, 4, kb, "kb")
                V(lambda e, p=p, hh=hh: e.tensor_tensor(kd[:, 0:4, :], kb[:, 0:4, :],
                                                        kdecP[:, p, hh * 4:(hh + 1) * 4].unsqueeze(2).to_broadcast([128, 4, 128]), op=ALU.mult),
                  ["kb", "kdecP"], ["kd"])
                A(lambda e, pv_=pv_: e.copy(vr[:, 0:4, :].rearrange("p a b -> p (a b)"), pv_[:, :]), [pvn], ["vr"])
                pst, pstn = pnext()
                for h4 in range(4):
                    P(lambda e, h4=h4, pst=pst: e.matmul(pst[:, h4 * 256:(h4 + 1) * 256], kd[:, h4, :], vr[:, h4, :], start=True, stop=True),
                      ["kd", "vr"], [pstn])
                V(lambda e, hh=hh, pst=pst: e.tensor_tensor(Sf[:, hh * 4:(hh + 1) * 4, :].rearrange("p a b -> p (a b)"),
                                                            Sf[:, hh * 4:(hh + 1) * 4, :].rearrange("p a b -> p (a b)"), pst[:, :], op=ALU.add),
                  [pstn, "Sf"], ["Sf"])
                emit_late(2)
        emit_late(len(late_jobs))
        A(lambda e: e.copy(Sb[:], Sf[:]), ["Sf"], ["Sb"])

        for i in range(NOWN):
            ti = NPRE + i
            load_x_norm(ti, norm1_g, 1 * D, 0)
            rope_tables(ti)
            pu, pun = pnext(); proj(pu, pun, 0, "w_in", 0); proj(pu, pun, 1, "w_in", 512)
            A(lambda e, pu=pu: e.activation(u_sb[:].rearrange("p a b -> p (a b)"), pu[:, :], AF.Gelu), [pun], ["u_sb"])
            pvv, pvvn = pnext(); proj(pvv, pvvn, 0, "w_in", 1024); proj(pvv, pvvn, 1, "w_in", 1536)
            A(lambda e, pvv=pvv: e.activation(v_f[:].rearrange("p a b -> p (a b)"), pvv[:, :], AF.Gelu), [pvvn], ["v_f"])
            V(lambda e: e.tensor_tensor(rotA[:], v_f[:], v_f[:], op=ALU.mult), ["v_f"], ["rotA"])
            V(lambda e: e.tensor_reduce(ss[:, 0:8], rotA[:], axis=AX.X, op=ALU.add), ["rotA"], ["ss"])
            V(lambda e: e.tensor_scalar(rstd[:, 0:8], ss[:, 0:8], 1.0 / 128, EPS, op0=ALU.mult, op1=ALU.add), ["ss"], ["rstd"])
            A(lambda e: e.activation(ss[:, 0:8], rstd[:, 0:8], AF.Sqrt), ["rstd"], ["ss"])
            V(lambda e: e.reciprocal(rstd[:, 0:8], ss[:, 0:8]), ["ss"], ["rstd"])
            vg, vgn = bcload(gm_v_g)
            V(lambda e: e.tensor_tensor(rotA[:], v_f[:], rstd[:, 0:8].unsqueeze(2).to_broadcast([128, 8, 128]), op=ALU.mult), ["v_f", "rstd"], ["rotA"])
            V(lambda e, vg=vg: e.tensor_tensor(v_b[:].rearrange("p a b -> p (a b)"), rotA[:].rearrange("p a b -> p (a b)"), vg[:, 0:1024], op=ALU.mult),
              ["rotA", vgn], ["v_b"])
            psg, psgn = pnext()
            for g in range(8):
                P(lambda e, g=g, psg=psg: e.matmul(psg[:, g * 128:(g + 1) * 128], wsT[:, g, :], v_b[:, g, :], start=True, stop=True),
                  ["wsT", "v_b"], [psgn])
            V(lambda e, psg=psg: e.tensor_tensor(rotA[:], psg[:, :].rearrange("p (a b) -> p a b", a=8),
                                                 gmbT[:, :].unsqueeze(2).to_broadcast([128, 8, 128]), op=ALU.add), [psgn, "gmbT"], ["rotA"])
            V(lambda e: e.tensor_tensor(preA[:], rotA[:], u_sb[:], op=ALU.mult), ["rotA", "u_sb"], ["preA"])
            transpose16(preA[:].rearrange("p a b -> p (a b)"), "preA", preAT, "preAT", nchunks=8)
            pq, pqn = pnext(); proj(pq, pqn, 0, "w_in", 2048); proj(pq, pqn, 1, "w_in", 2560)
            rope(pq, pqn, 8, qb, "qb")
            pk, pkn = pnext(); proj(pk, pkn, 0, "w_in", 3072); proj(pk, pkn, 1, "w_in", 3584)
            rope(pk, pkn, 8, kb, "kb")
            V(lambda e: e.tensor_tensor(kd[:], kb[:], kdec[:, :].unsqueeze(2).to_broadcast([128, 8, 128]), op=ALU.mult), ["kb", "kdec"], ["kd"])
            pt, pn = pnext(); pv = pt[:].bitcast(BF16)
            for h in range(8):
                P(lambda e, h=h, pv=pv: e.transpose(pv[:, h * 128:(h + 1) * 128], qb[:, h, :], identb[:]), ["qb", "identb"], [pn])
            A(lambda e, pv=pv: e.copy(qT[:].rearrange("p a b -> p (a b)"), pv[:, 0:1024]), [pn], ["qT"])
            V(lambda e, pv=pv: e.tensor_tensor(qdT[:].rearrange("p a b -> p (a b)"), pv[:, 0:1024], qdecT[:].rearrange("p a b -> p (a b)"), op=ALU.mult),
              [pn, "qdecT"], ["qdT"])
            pt, pn = pnext(); pv = pt[:].bitcast(BF16)
            for h in range(8):
                P(lambda e, h=h, pv=pv: e.transpose(pv[:, h * 128:(h + 1) * 128], kb[:, h, :], identb[:]), ["kb", "identb"], [pn])
            A(lambda e, pv=pv: e.copy(kT[:].rearrange("p a b -> p (a b)"), pv[:, 0:1024]), [pn], ["kT"])
            for j in range(2):
                pvr, pvrn = pnext(); proj(pvr, pvrn, 0, "w_in", 4096 + j * 1024); proj(pvr, pvrn, 1, "w_in", 4096 + j * 1024 + 512)
                A(lambda e, j=j, pvr=pvr: e.copy(vr[:, j * 4:(j + 1) * 4, :].rearrange("p a b -> p (a b)"), pvr[:, :]), [pvrn], ["vr"])
            for j in range(2):
                pg, pgn = pnext(); proj(pg, pgn, 0, "w_in", 6144 + j * 1024); proj(pg, pgn, 1, "w_in", 6144 + j * 1024 + 512)
                A(lambda e, j=j, pg=pg: e.activation(sg[:, j * 1024:(j + 1) * 1024], pg[:, :], AF.Silu), [pgn], ["sg"])
            pat, patn = pnext()
            for h in range(8):
                P(lambda e, h=h, pat=pat: e.matmul(pat[:, h * 128:(h + 1) * 128], kT[:, h, :], qT[:, h, :], start=True, stop=True),
                  ["kT", "qT"], [patn])
            V(lambda e, pat=pat: e.tensor_tensor(attm[:].rearrange("p a b -> p (a b)"), pat[:, :], MT[:].rearrange("p a b -> p (a b)"), op=ALU.mult),
              [patn, "MT"], ["attm"])
            for j in range(2):
                po, pon = pnext()
                for h4 in range(4):
                    h = j * 4 + h4
                    P(lambda e, h=h, h4=h4, po=po: e.matmul(po[:, h4 * 256:(h4 + 1) * 256], attm[:, h, :], vr[:, h, :], start=True, stop=False),
                      ["attm", "vr"], [pon])
                    P(lambda e, h=h, h4=h4, po=po: e.matmul(po[:, h4 * 256:(h4 + 1) * 256], qdT[:, h, :], Sb[:, h, :], start=False, stop=True),
                      ["qdT", "Sb"], [pon])
                A(lambda e, j=j, po=po: e.copy(o_sb[:, j * 4:(j + 1) * 4, :].rearrange("p a b -> p (a b)"), po[:, :]), [pon], ["o_sb"])
            for j in range(2):
                pst, pstn = pnext()
                for h4 in range(4):
                    h = j * 4 + h4
                    P(lambda e, h=h, h4=h4, pst=pst: e.matmul(pst[:, h4 * 256:(h4 + 1) * 256], kd[:, h, :], vr[:, h, :], start=True, stop=True),
                      ["kd", "vr"], [pstn])
                for h4 in range(4):
                    h = j * 4 + h4
                    V(lambda e, h=h, h4=h4, pst=pst: e.scalar_tensor_tensor(Sf[:, h, :], Sf[:, h, :], float(GAM[h] ** 128), pst[:, h4 * 256:(h4 + 1) * 256],
                                                                            op0=ALU.mult, op1=ALU.add), [pstn, "Sf"], ["Sf"])
            A(lambda e: e.copy(Sb[:], Sf[:]), ["Sf"], ["Sb"])
            V(lambda e: e.tensor_reduce(st8[:, 0, :], o_sb[:], axis=AX.X, op=ALU.add), ["o_sb"], ["st8"])
            V(lambda e: e.tensor_tensor(o2[:], o_sb[:], o_sb[:], op=ALU.mult), ["o_sb"], ["o2"])
            V(lambda e: e.tensor_reduce(st8[:, 1, :], o2[:], axis=AX.X, op=ALU.add), ["o2"], ["st8"])
            V(lambda e: e.tensor_scalar(st8[:, 0, :], st8[:, 0, :], 1.0 / 256, None, op0=ALU.mult), ["st8"], ["st8"])
            V(lambda e: e.tensor_tensor(st8[:, 2, :], st8[:, 0, :], st8[:, 0, :], op=ALU.mult), ["st8"], ["st8"])
            V(lambda e: e.scalar_tensor_tensor(st8[:, 1, :], st8[:, 1, :], 1.0 / 256, st8[:, 2, :], op0=ALU.mult, op1=ALU.subtract), ["st8"], ["st8"])
            V(lambda e: e.tensor_scalar(st8[:, 1, :], st8[:, 1, :], EPS, None, op0=ALU.add), ["st8"], ["st8"])
            A(lambda e: e.activation(st8[:, 2, :], st8[:, 1, :], AF.Sqrt), ["st8"], ["st8"])
            V(lambda e: e.reciprocal(st8[:, 3, :], st8[:, 2, :]), ["st8"], ["st8"])
            V(lambda e: e.tensor_tensor(o2[:], o_sb[:], st8[:, 0, :].unsqueeze(2).to_broadcast([128, 8, 256]), op=ALU.subtract), ["o_sb", "st8"], ["o2"])
            V(lambda e: e.tensor_tensor(o2[:], o2[:], st8[:, 3, :].unsqueeze(2).to_broadcast([128, 8, 256]), op=ALU.mult), ["o2", "st8"], ["o2"])
            gnb, gnn = bcload(ret_gn_g)
            V(lambda e, gnb=gnb: e.tensor_tensor(o2[:].rearrange("p a b -> p (a b)"), o2[:].rearrange("p a b -> p (a b)"), gnb[:], op=ALU.mult), ["o2", gnn], ["o2"])
            V(lambda e: e.tensor_tensor(retb[:], o2[:].rearrange("p a b -> p (a b)"), sg[:], op=ALU.mult), ["o2", "sg"], ["retb"])
            transpose16(retb, "retb", retT, "retT")
            for n in range(4):
                pga, pgan = pnext()
                proj(pga, pgan, 0, "w_bg", n * 512); proj(pga, pgan, 1, "w_bg", 2048 + n * 512)
                bb, bbn = bcload(b_bg[0:1, n * 512:(n + 1) * 512])
                bb2, bb2n = bcload(b_bg[0:1, 2048 + n * 512:2048 + (n + 1) * 512])
                V(lambda e, pga=pga, bb=bb: e.tensor_tensor(gA[:], pga[:, 0:512], bb[:, 0:512], op=ALU.add), [pgan, bbn], ["gA"])
                V(lambda e, pga=pga, bb2=bb2: e.tensor_tensor(gB[:], pga[:, 512:1024], bb2[:, 0:512], op=ALU.add), [pgan, bb2n], ["gB"])
                A(lambda e: e.activation(gA[:], gA[:], AF.Sigmoid), ["gA"], ["gA"])
                A(lambda e: e.activation(gB[:], gB[:], AF.Sigmoid), ["gB"], ["gB"])
                pyy, pyyn = pnext()
                proj(pyy, pyyn, 0, "w_a", n * 512, K=8, lhs=preAT, lhsn="preAT")
                proj(pyy, pyyn, 1, "w_b", n * 512, K=16, lhs=retT, lhsn="retT")
                V(lambda e, pyy=pyy: e.tensor_tensor(gA[:], gA[:], pyy[:, 0:512], op=ALU.mult), ["gA", pyyn], ["gA"])
                V(lambda e, pyy=pyy: e.tensor_tensor(gB[:], gB[:], pyy[:, 512:1024], op=ALU.mult), ["gB", pyyn], ["gB"])
                V(lambda e, n=n: e.tensor_tensor(mb[:, n * 512:(n + 1) * 512], gA[:], gB[:], op=ALU.add), ["gA", "gB"], ["mb"])
            transpose16(mb, "mb", mT, "mT")
            ga1, ga1n = adaload(2 * D)
            for j in range(2):
                pm, pmn = pnext()
                proj(pm, pmn, 0, "w_o", j * 1024, lhs=mT, lhsn="mT"); proj(pm, pmn, 1, "w_o", j * 1024 + 512, lhs=mT, lhsn="mT")
                V(lambda e, j=j, pm=pm, ga1=ga1: e.tensor_tensor(ga1[:, j * 1024:(j + 1) * 1024], ga1[:, j * 1024:(j + 1) * 1024], pm[:, :], op=ALU.mult),
                  [pmn, ga1n], [ga1n])
            V(lambda e, ga1=ga1: e.tensor_tensor(xt[:], xt[:], ga1[:], op=ALU.add), ["xt", ga1n], ["xt"])
            if dbg:
                ld(dbg_x1[i], xt[:], ["dbgx1_%d" % i], r=["xt"])

            norm_from(xt, "xt", norm2_g, 4 * D, 3 * D, out_f32=(h2, "h2"))
            for half in range(2):
                pq2, pq2n = pnext()
                for cc in range(2):
                    wv, wn = wload("wq", half * 2 + cc)
                    for c4 in range(4):
                        j = cc * 4 + c4
                        for kc in range(16):
                            P(lambda e, wv=wv, kc=kc, c4=c4, j=j, pq2=pq2: e.matmul(pq2[:, j * 128:(j + 1) * 128], wv[:, kc, c4 * 128:(c4 + 1) * 128], hT[:, kc, :],
                                                                                   start=(kc == 0), stop=(kc == 15)), ["hT"] + wn, [pq2n])
                A(lambda e, half=half, pq2=pq2: e.copy(qpT[:, half * 8:(half + 1) * 8, :].rearrange("p a b -> p (a b)"), pq2[:, :]), [pq2n], ["qpT"])
            for half in range(2):
                psc, pscn = pnext()
                for j in range(8):
                    c = half * 8 + j
                    P(lambda e, c=c, j=j, psc=psc: e.matmul(psc[:, j * 128:(j + 1) * 128], qpT[:, c, :], subkT[:, c, :], start=True, stop=True),
                      ["qpT", "subkT"], [pscn])
                A(lambda e, half=half, psc=psc: e.copy(sc_sb[:, half * 8:(half + 1) * 8, :].rearrange("p a b -> p (a b)"), psc[:, :]), [pscn], ["sc_sb"])
            for c in range(16):
                V(lambda e, c=c: e.max(out=v12[:, c, 0:8], in_=sc_sb[:, c, :]), ["sc_sb"], ["v12"])
                V(lambda e, c=c: e.max_index(out=i12[:, c, 0:8], in_max=v12[:, c, 0:8], in_values=sc_sb[:, c, :]), ["sc_sb", "v12"], ["i12"])
                V(lambda e, c=c: e.match_replace(out=wk[:, 0:128], in_to_replace=v12[:, c, 0:8], in_values=sc_sb[:, c, :], imm_value=-1e30), ["sc_sb", "v12"], ["wk"])
                V(lambda e, c=c: e.max(out=v12[:, c, 8:16], in_=wk[:, 0:128]), ["wk"], ["v12"])
                V(lambda e, c=c: e.max_index(out=i12[:, c, 8:16], in_max=v12[:, c, 8:16], in_values=wk[:, 0:128]), ["wk", "v12"], ["i12"])
            V(lambda e: e.tensor_copy(i12f[:], i12[:]), ["i12"], ["i12f"])
            v12v = v12[:].rearrange("p (h two) k -> p h two k", two=2)
            i12v = i12f[:].rearrange("p (h two) k -> p h two k", two=2)
            for h in range(8):
                V(lambda e, h=h: e.tensor_tensor(cand[:, h, :].rearrange("p (a b) -> p a b", a=16),
                                                 v12v[:, h, 0, :].unsqueeze(2).to_broadcast([128, 16, 16]),
                                                 v12v[:, h, 1, :].unsqueeze(1).to_broadcast([128, 16, 16]), op=ALU.add), ["v12"], ["cand"])
            for h in range(8):
                V(lambda e, h=h: e.max(out=top[:, h, 0:8], in_=cand[:, h, :]), ["cand"], ["top"])
                V(lambda e, h=h: e.max_index(out=pos[:, h, 0:8], in_max=top[:, h, 0:8], in_values=cand[:, h, :]), ["cand", "top"], ["pos"])
                V(lambda e, h=h: e.match_replace(out=wk[:], in_to_replace=top[:, h, 0:8], in_values=cand[:, h, :], imm_value=-1e30), ["cand", "top"], ["wk"])
                V(lambda e, h=h: e.max(out=top[:, h, 8:16], in_=wk[:]), ["wk"], ["top"])
                V(lambda e, h=h: e.max_index(out=pos[:, h, 8:16], in_max=top[:, h, 8:16], in_values=wk[:]), ["wk", "top"], ["pos"])
            V(lambda e: e.tensor_copy(pcor[:], pos[:]), ["pos"], ["pcor"])
            V(lambda e: e.tensor_scalar(pi32[:], pcor[:], 0.0625, None, op0=ALU.mult), ["pcor"], ["pi32"])
            V(lambda e: e.tensor_copy(paf[:], pi32[:]), ["pi32"], ["paf"])
            V(lambda e: e.scalar_tensor_tensor(pbf[:].rearrange("p h k -> p (h k)"), paf[:].rearrange("p h k -> p (h k)"), -16.0,
                                               pcor[:].rearrange("p h k -> p (h k)"), op0=ALU.mult, op1=ALU.add), ["paf", "pcor"], ["pbf"])
            V(lambda e: e.tensor_scalar(pcor[:], pbf[:], 0.0, None, op0=ALU.is_lt), ["pbf"], ["pcor"])
            V(lambda e: e.tensor_tensor(paf[:], paf[:], pcor[:], op=ALU.subtract), ["paf", "pcor"], ["paf"])
            V(lambda e: e.scalar_tensor_tensor(pbf[:].rearrange("p h k -> p (h k)"), pcor[:].rearrange("p h k -> p (h k)"), 16.0,
                                               pbf[:].rearrange("p h k -> p (h k)"), op0=ALU.mult, op1=ALU.add), ["pcor", "pbf"], ["pbf"])
            for (pf, pfn, two, dst, dstn) in ((paf, "paf", 0, i1s, "i1s"), (pbf, "pbf", 1, i2s, "i2s")):
                for h in range(8):
                    ohv = oh[:, h, :].rearrange("p (k a) -> p k a", k=16)
                    V(lambda e, h=h, pf=pf, ohv=ohv: e.tensor_tensor(ohv, iota16[:, :].unsqueeze(1).to_broadcast([128, 16, 16]),
                                                                     pf[:, h, :].unsqueeze(2).to_broadcast([128, 16, 16]), op=ALU.is_equal),
                      ["iota16", pfn], ["oh"])
                    V(lambda e, h=h, two=two, ohv=ohv: e.tensor_tensor(ohv, ohv, i12v[:, h, two, :].unsqueeze(1).to_broadcast([128, 16, 16]), op=ALU.mult),
                      ["oh", "i12f"], ["oh"])
                V(lambda e, dst=dst: e.tensor_reduce(dst[:].rearrange("p h k -> p (h k)"), oh[:].rearrange("p h (k a) -> p (h k) a", a=16), axis=AX.X, op=ALU.add),
                  ["oh"], [dstn])
            V(lambda e: e.scalar_tensor_tensor(i1s[:], i1s[:], 128.0, i2s[:], op0=ALU.mult, op1=ALU.add), ["i1s", "i2s"], ["i1s"])
            V(lambda e: e.tensor_copy(eidx[:], i1s[:].rearrange("p h k -> p (h k)")), ["i1s"], ["eidx"])
            V(lambda e: e.tensor_tensor(gate[:], top[:], top[:, :, 0:1].to_broadcast([128, 8, 16]), op=ALU.subtract), ["top"], ["gate"])
            A(lambda e: e.activation(gate[:], gate[:], AF.Exp), ["gate"], ["gate"])
            V(lambda e: e.tensor_reduce(gsum[:], gate[:], axis=AX.X, op=ALU.add), ["gate"], ["gsum"])
            V(lambda e: e.reciprocal(gsum[:], gsum[:]), ["gsum"], ["gsum"])
            V(lambda e: e.tensor_tensor(gate[:], gate[:], gsum[:, :].unsqueeze(2).to_broadcast([128, 8, 16]), op=ALU.mult), ["gate", "gsum"], ["gate"])
            for hk in range(128):
                gt, gn = gnext()
                S.dma(lambda e, gt=gt, hk=hk: e.indirect_dma_start(out=gt[:], out_offset=None, in_=peer_u,
                                                                  in_offset=bass.IndirectOffsetOnAxis(ap=eidx[:, hk:hk + 1], axis=0)),
                      reads=["eidx"], writes=[gn], queue="pool")
                V(lambda e, gt=gt, hk=hk: e.scalar_tensor_tensor(junk[:], gt[:], 1.0, h2[:], op0=ALU.mult, op1=ALU.mult, accum_out=acol[:, hk:hk + 1]),
                  [gn, "h2"], ["junk", "acol"])
            A(lambda e: e.activation(wgt[:], acol[:], AF.Gelu), ["acol"], ["wgt"])
            V(lambda e: e.tensor_tensor(wgt[:], wgt[:], gate[:].rearrange("p h k -> p (h k)"), op=ALU.mult), ["wgt", "gate"], ["wgt"])
            for hk in range(128):
                gt, gn = gnext()
                S.dma(lambda e, gt=gt, hk=hk: e.indirect_dma_start(out=gt[:], out_offset=None, in_=peer_v,
                                                                  in_offset=bass.IndirectOffsetOnAxis(ap=eidx[:, hk:hk + 1], axis=0)),
                      reads=["eidx"], writes=[gn], queue="pool")
                if hk == 0:
                    V(lambda e, gt=gt: e.tensor_scalar(y[:], gt[:], wgt[:, 0:1], None, op0=ALU.mult), [gn, "wgt"], ["y"])
                else:
                    V(lambda e, gt=gt, hk=hk: e.scalar_tensor_tensor(y[:], gt[:], wgt[:, hk:hk + 1], y[:], op0=ALU.mult, op1=ALU.add), [gn, "wgt", "y"], ["y"])
            if dbg:
                ld(dbg_y[i], y[:], ["dbgy_%d" % i], r=["y"])
            ga2, ga2n = adaload(5 * D)
            V(lambda e, ga2=ga2: e.tensor_tensor(y[:], y[:], ga2[:], op=ALU.mult), ["y", ga2n], ["y"])
            V(lambda e: e.tensor_tensor(xt[:], xt[:], y[:], op=ALU.add), ["xt", "y"], ["xt"])
            A(lambda e: e.activation(junk[:], xt[:], AF.Square, accum_out=ss[:, 0:1]), ["xt"], ["junk", "ss"])
            V(lambda e: e.tensor_scalar(rstd[:, 0:1], ss[:, 0:1], 1.0 / D, EPS, op0=ALU.mult, op1=ALU.add), ["ss"], ["rstd"])
            A(lambda e: e.activation(rstd[:, 1:2], rstd[:, 0:1], AF.Sqrt), ["rstd"], ["rstd"])
            V(lambda e: e.reciprocal(rstd[:, 2:3], rstd[:, 1:2]), ["rstd"], ["rstd"])
            gf, gfn = bcload(norm_f_g)
            V(lambda e, gf=gf: e.scalar_tensor_tensor(y[:], xt[:], rstd[:, 2:3], gf[:], op0=ALU.mult, op1=ALU.mult), ["xt", "rstd", gfn], ["y"])
            ld(out_d[i], y[:], ["out%d" % i], r=["y"])
        fin = ["out%d" % i for i in range(NOWN)] + ([n % i for i in range(NOWN) for n in ("dbgx1_%d", "dbgy_%d")] if dbg else [])
        S.final_wait("sp", fin)
        S.emit()
    return nc


def _consts(seg):
    gam = np.array([1.0 - 2.0 ** (-5.0 - h) for h in range(H)], dtype=np.float64)
    scale = 128.0 ** -0.5
    i = np.arange(128)
    MT = np.zeros((128, H, 128), dtype=np.float64)
    ci, cj = i[None, :] // 64, i[:, None] // 64
    dist = np.abs(i[None, :] - i[:, None]).astype(np.float64)
    allowed = (ci >= cj)
    for h in range(H):
        MT[:, h, :] = np.where(allowed, gam[h] ** dist, 0.0) * scale
    qdecT = np.broadcast_to((gam[None, :, None] ** (i[None, None, :] + 1.0)), (128, H, 128))
    kdec = (gam[None, :] ** (127.0 - i[:, None])) * scale
    kdecP = np.zeros((128, NPRE, H), dtype=np.float64)
    P0 = seg * 1024
    for p in range(NPRE):
        gt = seg * 8 - NPRE + p
        if gt < 0:
            continue
        tok = gt * 128 + i
        kdecP[:, p, :] = (gam[None, :] ** (P0 - 1.0 - tok[:, None])) * scale
    f32 = lambda a: np.ascontiguousarray(a, dtype=np.float32)
    return f32(MT), f32(qdecT), f32(kdec), f32(kdecP)


def _in_maps(inputs):
    x = np.asarray(inputs["x"], dtype=np.float32)
    c = np.asarray(inputs["c"], dtype=np.float32)
    positions = np.asarray(inputs["positions"]).astype(np.int32)
    g = lambda k: np.asarray(inputs[k], dtype=np.float32)
    shared = {
        "w_ada": np.ascontiguousarray(g("w_ada")[0]),
        "b_ada": np.ascontiguousarray(g("b_ada")[0][None, :]),
        "norm1_g": np.ascontiguousarray(g("norm1_g")[0][None, :]),
        "w_in": np.ascontiguousarray(g("w_in")[0]),
        "w_bg": np.ascontiguousarray(g("w_branch_gate")[0]),
        "b_bg": np.ascontiguousarray(g("b_branch_gate")[0][None, :]),
        "gm_v_g": np.ascontiguousarray(g("gm_v_g")[0][None, :]),
        "wsT": np.ascontiguousarray(g("gm_ws")[0].transpose(2, 0, 1)),
        "gm_bT": np.ascontiguousarray(g("gm_b")[0].T),
        "ret_gn_g": np.ascontiguousarray(g("ret_gn_g")[0][None, :]),
        "w_a": np.ascontiguousarray(g("w_a_out")[0]),
        "w_b": np.ascontiguousarray(g("w_b_out")[0]),
        "w_o": np.ascontiguousarray(g("w_o")[0]),
        "norm2_g": np.ascontiguousarray(g("norm2_g")[0][None, :]),
        "wq": np.ascontiguousarray(g("peer_wq")[0]),
        "subkT": np.ascontiguousarray(g("peer_subkeys")[0].reshape(16, 128, 128).transpose(2, 0, 1)),
        "peer_u": np.ascontiguousarray(g("peer_u")[0]),
        "peer_v": np.ascontiguousarray(g("peer_v")[0]),
        "norm_f_g": np.ascontiguousarray(g("norm_f_g")[None, :]),
        "ident": np.eye(128, dtype=np.float32),
        "freq": np.ascontiguousarray(np.broadcast_to((10000.0 ** (-np.arange(64, dtype=np.float32) / 64)).astype(np.float32)[None, :], (128, 64))),
        "iota16": np.ascontiguousarray(np.broadcast_to(np.arange(16, dtype=np.float32)[None, :], (128, 16))),
    }
    maps = []
    for core in range(8):
        b, seg = core // 4, core % 4
        xsl = np.zeros((NPRE + NOWN, 128, D), dtype=np.float32)
        pos = np.zeros((128, NPRE + NOWN), dtype=np.int32)
        for p in range(NPRE + NOWN):
            gt = seg * 8 - NPRE + p
            if gt < 0:
                continue
            xsl[p] = x[b, gt * 128:(gt + 1) * 128]
            pos[:, p] = positions[b, gt * 128:(gt + 1) * 128]
        MT, qdecT, kdec, kdecP = _consts(seg)
        m = dict(shared)
        m.update({"xs": xsl, "posi": pos, "cT": np.ascontiguousarray(c[b].reshape(16, 128).T),
                  "MT": MT, "qdecT": qdecT, "kdec": kdec, "kdecP": kdecP})
        maps.append(m)
    return maps


_DBG = bool(int(os.environ.get("KDBG", "0")))
_last = {}


def kernel(**inputs):
    nc = build_program(dbg=_DBG)
    maps = _in_maps(inputs)
    res = run_bass_kernel_spmd(nc, maps, core_ids=list(range(8)))
    out = np.zeros((2, 4096, D), dtype=np.float32)
    for core in range(8):
        b, seg = core // 4, core % 4
        out[b, seg * 1024:(seg + 1) * 1024] = res.results[core]["out"].reshape(1024, D)
    if _DBG:
        _last["res"] = res.results
    return out
```

```python
import os
from contextlib import ExitStack
import numpy as np
import concourse.bass as bass
import concourse.mybir as mybir
from concourse.bass_utils import run_bass_kernel_spmd

F32 = mybir.dt.float32
BF16 = mybir.dt.bfloat16
I32 = mybir.dt.int32
U32 = mybir.dt.uint32
AF = mybir.ActivationFunctionType
ALU = mybir.AluOpType
AX = mybir.AxisListType

D = 2048
NPRE = 24
NOWN = 8
EPS = 1e-6
H = 8
ENGS = ("pe", "act", "dve", "pool", "sp")
NDMA_SEMS = 28
TWO_PI = float(2 * np.pi)


class Sched:
    def __init__(self, nc, stack):
        self.nc = nc
        self.stack = stack
        self.ops = {e: [] for e in ENGS}
        self.cnt = {e: 0 for e in ENGS}
        self.sem = {e: stack.enter_context(nc.semaphore("s_" + e)) for e in ENGS if e != "sp"}
        self.dsem = [stack.enter_context(nc.semaphore("d%d" % i)) for i in range(NDMA_SEMS)]
        self.dcnt = [0] * NDMA_SEMS
        half = NDMA_SEMS // 2
        self.dpool = {"sp": list(range(0, half)), "act": list(range(0, half)), "pool": list(range(half, NDMA_SEMS))}
        self.dnext = {"sp": 0, "act": 0, "pool": 0}
        self.waited = {e: {} for e in ENGS}
        self.lastw = {}
        self.readers = {}
        self.alias = {}

    def _x(self, names):
        out = []
        for n in names:
            out.extend(self.alias.get(n, [n]))
        return out

    def sb(self, name, shape, dtype=F32):
        return self.stack.enter_context(self.nc.sbuf_tensor("sb_" + name, list(shape), dtype))

    def ps(self, name, shape, dtype=F32):
        return self.stack.enter_context(self.nc.psum_tensor("ps_" + name, list(shape), dtype))

    def _semobj(self, key):
        return self.sem[key] if isinstance(key, str) else self.dsem[key]

    def _deps(self, eng, reads, writes):
        need = {}

        def add(k, v, same_ok):
            if k == eng and not same_ok and eng == "pe":
                return
            if need.get(k, 0) < v:
                need[k] = v

        for r in reads:
            if r in self.lastw:
                k, v = self.lastw[r]
                add(k, v, True)
        for w in writes:
            if w in self.lastw:
                k, v = self.lastw[w]
                add(k, v, False)
            for k, v in self.readers.get(w, {}).items():
                add(k, v, False)
        waits = []
        wd = self.waited[eng]
        for k, v in need.items():
            if wd.get(k, 0) >= v:
                continue
            wd[k] = v
            waits.append((k, v))
        return waits

    def _record(self, key, val, reads, writes):
        for r in reads:
            self.readers.setdefault(r, {})[key] = val
        for w in writes:
            self.lastw[w] = (key, val)
            self.readers[w] = {}

    def op(self, eng, fn, reads=(), writes=()):
        reads, writes = self._x(reads), self._x(writes)
        waits = self._deps(eng, reads, writes)
        self.cnt[eng] += 1
        self.ops[eng].append((waits, fn, (eng, 1)))
        self._record(eng, self.cnt[eng], reads, writes)

    def dma(self, fn, reads=(), writes=(), queue="sp"):
        reads, writes = self._x(reads), self._x(writes)
        qp = self.dpool[queue]
        i = qp[self.dnext[queue] % len(qp)]
        self.dnext[queue] += 1
        waits = self._deps(queue, reads, writes)
        if self.dcnt[i] > 0 and self.waited[queue].get(i, 0) < self.dcnt[i]:
            self.waited[queue][i] = self.dcnt[i]
            waits.append((i, self.dcnt[i]))
        self.dcnt[i] += 16
        self.ops[queue].append((waits, fn, (i, 16)))
        self._record(i, self.dcnt[i], reads, writes)

    def final_wait(self, eng, reslist):
        waits = self._deps(eng, self._x(reslist), ())
        self.ops[eng].append((waits, None, None))

    def emit(self):
        nc = self.nc
        with nc.Block() as block:
            def run(ename):
                def body(e):
                    for waits, fn, inc in self.ops[ename]:
                        for k, v in waits:
                            e.wait_ge(self._semobj(k), v)
                        if fn is None:
                            continue
                        ins = fn(e)
                        ins.then_inc(self._semobj(inc[0]), inc[1])
                return body
            block.tensor(run("pe"))
            block.scalar(run("act"))
            block.vector(run("dve"))
            block.gpsimd(run("pool"))
            block.sync(run("sp"))


def build_program(dbg=False):
    nc = bass.Bass("TRN2", target_bir_lowering=False)

    def din(name, shape, dt=F32):
        return nc.dram_tensor(name, list(shape), dt, kind="ExternalInput").ap()

    xs = din("xs", [NPRE + NOWN, 128, D])
    posi = din("posi", [128, NPRE + NOWN], I32)
    cT_d = din("cT", [128, 16])
    w_ada = din("w_ada", [D, 6 * D])
    b_ada = din("b_ada", [1, 6 * D])
    norm1_g = din("norm1_g", [1, D])
    w_in = din("w_in", [D, 8192])
    w_bg = din("w_bg", [D, 4096])
    b_bg = din("b_bg", [1, 4096])
    gm_v_g = din("gm_v_g", [1, 1024])
    wsT_d = din("wsT", [128, 8, 128])
    gm_bT_d = din("gm_bT", [128, 8])
    ret_gn_g = din("ret_gn_g", [1, D])
    w_a = din("w_a", [1024, D])
    w_b = din("w_b", [D, D])
    w_o = din("w_o", [D, D])
    norm2_g = din("norm2_g", [1, D])
    wq = din("wq", [D, D])
    subkT_d = din("subkT", [128, 16, 128])
    peer_u = din("peer_u", [16384, D])
    peer_v = din("peer_v", [16384, D])
    norm_f_g = din("norm_f_g", [1, D])
    ident_d = din("ident", [128, 128])
    freq_d = din("freq", [128, 64])
    iota16_d = din("iota16", [128, 16])
    MT_d = din("MT", [128, 8, 128])
    qdecT_d = din("qdecT", [128, 8, 128])
    kdec_d = din("kdec", [128, 8])
    kdecP_d = din("kdecP", [128, NPRE, 8])
    out_d = nc.dram_tensor("out", [NOWN, 128, D], F32, kind="ExternalOutput").ap()
    ada_d = nc.dram_tensor("ada_scr", [1, 6 * D], F32, kind="Internal").ap()
    WSPEC = {"w_in": (w_in, 16, 16), "w_bg": (w_bg, 16, 8), "w_a": (w_a, 8, 4), "w_b": (w_b, 16, 4), "w_o": (w_o, 16, 4), "wq": (wq, 16, 4)}
    wscr = {k: nc.dram_tensor("wb_" + k, [v[2], 128, v[1] * 512], BF16, kind="Internal").ap() for k, v in WSPEC.items()}
    if dbg:
        dbg_x1 = nc.dram_tensor("dbg_x1", [NOWN, 128, D], F32, kind="ExternalOutput").ap()
        dbg_y = nc.dram_tensor("dbg_y", [NOWN, 128, D], F32, kind="ExternalOutput").ap()

    GAM = [1.0 - 2.0 ** (-5.0 - h) for h in range(H)]

    with ExitStack() as st:
        S = Sched(nc, st)
        sb, ps = S.sb, S.ps

        ident = sb("ident", [128, 128]); identb = sb("identb", [128, 128], BF16)
        freq = sb("freq", [128, 64]); iota16 = sb("iota16", [128, 16])
        MT = sb("MT", [128, 8, 128]); qdecT = sb("qdecT", [128, 8, 128])
        kdec = sb("kdec", [128, 8]); kdecP = sb("kdecP", [128, NPRE, 8])
        wsT = sb("wsT", [128, 8, 128], BF16)
        gmbT = sb("gmbT", [128, 8]); subkT = sb("subkT", [128, 16, 128])
        posI = sb("posI", [128, NPRE + NOWN], I32); posF = sb("posF", [128, NPRE + NOWN])
        cT = sb("cT", [128, 16]); scT = sb("scT", [128, 16], BF16)

        NW = 3
        wring = [sb("w%d" % i, [128, 8192], BF16) for i in range(NW)]
        wr_i = [0]

        def wnext():
            i = wr_i[0]; wr_i[0] = (i + 1) % NW
            return wring[i], ["w%da" % i, "w%db" % i]

        NBC = 2
        bcring = [sb("bc%d" % i, [128, D]) for i in range(NBC)]
        bc_i = [0]

        def bcnext():
            i = bc_i[0]; bc_i[0] = (i + 1) % NBC
            return bcring[i], "bc%d" % i

        NP = 4
        pring = [ps("P%d" % i, [128, 1024]) for i in range(NP)]
        p_i = [0]

        def pnext():
            i = p_i[0]; p_i[0] = (i + 1) % NP
            return pring[i], "P%d" % i

        xt = sb("xt", [128, D])
        ss = sb("ss", [128, 8]); rstd = sb("rstd", [128, 8])
        hb = sb("hb", [128, D], BF16); hT = sb("hT", [128, 16, 128], BF16)
        junk = hb; S.alias["junk"] = ["hb"]
        cs = sb("cs", [128, 4, 64])
        ki = sb("ki", [128, 64], I32); kf = sb("kf", [128, 2, 64])
        pi32 = sb("pi32", [128, 8, 16], I32); pcor = sb("pcor", [128, 8, 16])
        Sf = sb("Sf", [128, 8, 256]); Sb = sb("Sb", [128, 8, 256], BF16)
        st8 = sb("st8", [128, 4, 8])
        wk = sb("wk", [128, 256])
        v12 = sb("v12", [128, 16, 16]); i12 = sb("i12", [128, 16, 16], U32); i12f = sb("i12f", [128, 16, 16])
        top = sb("top", [128, 8, 16]); pos = sb("pos", [128, 8, 16], U32)
        paf = sb("paf", [128, 8, 16]); pbf = sb("pbf", [128, 8, 16])
        i1s = sb("i1s", [128, 8, 16]); i2s = sb("i2s", [128, 8, 16])
        eidx = sb("eidx", [128, 128], I32)
        gate = sb("gate", [128, 8, 16]); gsum = sb("gsum", [128, 8])
        acol = sb("acol", [128, 128]); wgt = sb("wgt", [128, 128])

        ARENA = 80 * 1024
        GRAN = 2048
        arena = sb("arena", [128, ARENA // 4])
        DSZ = {F32: 4, BF16: 2, I32: 4, U32: 4}

        def carve(off, name, shape, dtype=F32, parts=128):
            nel = int(np.prod(shape[1:]))
            nbytes = nel * DSZ[dtype]
            assert off % 4 == 0 and off + nbytes <= ARENA, (name, off, nbytes)
            v = arena[0:parts, off // 4:(off + nbytes) // 4]
            if dtype != F32:
                v = v.bitcast(dtype)
            if len(shape) == 3:
                v = v.rearrange("p (a b) -> p a b", a=shape[1])
            S.alias[name] = ["ar%d" % g for g in range(off // GRAN, (off + nbytes + GRAN - 1) // GRAN)]
            return v, off + ((nbytes + GRAN - 1) // GRAN) * GRAN

        K = 1024
        sg, o = carve(0, "sg", [128, D])
        rotA, o = carve(o, "rotA", [128, 8, 128]); rotB, o2_off = carve(o, "rotB", [128, 8, 128])
        o_sb, _ = carve(o - 4 * K, "o_sb", [128, 8, 256])
        o = o2_off
        u_sb, o = carve(o, "u_sb", [128, 8, 128]); v_f, o = carve(o, "v_f", [128, 8, 128])
        o2, _ = carve(o - 8 * K, "o2", [128, 8, 256])
        qb, o = carve(o, "qb", [128, 8, 128], BF16); kb, o = carve(o, "kb", [128, 8, 128], BF16)
        kd, o = carve(o, "kd", [128, 8, 128], BF16); qT, o = carve(o, "qT", [128, 8, 128], BF16)
        qdT, o = carve(o, "qdT", [128, 8, 128], BF16); kT, o = carve(o, "kT", [128, 8, 128], BF16)
        vr, o = carve(o, "vr", [128, 8, 256], BF16)
        v_b, o = carve(o, "v_b", [128, 8, 128], BF16); preA, o = carve(o, "preA", [128, 8, 128], BF16)
        preAT, o = carve(o, "preAT", [128, 8, 128], BF16); attm, o = carve(o, "attm", [128, 8, 128], BF16)
        retb, o = carve(o, "retb", [128, D], BF16); retT, o = carve(o, "retT", [128, 16, 128], BF16)
        mb, o = carve(o, "mb", [128, D], BF16); mT, o = carve(o, "mT", [128, 16, 128], BF16)
        gA, o = carve(o, "gA", [128, 512]); gB, o = carve(o, "gB", [128, 512])
        xt2, _ = carve(40 * K, "xt2", [128, D]); hb2, _ = carve(48 * K, "hb2", [128, D], BF16)
        hT2, _ = carve(52 * K, "hT2", [128, 16, 128], BF16)
        A1p, _ = carve(56 * K, "A1p", [128, D]); sh1p, _ = carve(64 * K, "sh1p", [128, D])
        cvS = [carve(0, "cvS0", [128, 8, 512], BF16)[0], carve(16 * K, "cvS1", [128, 8, 512], BF16)[0]]
        brow, _ = carve(0, "brow", [1, 512], parts=1); arow, _ = carve(2 * K, "arow", [1, 512], parts=1)
        h2, o = carve(0, "h2", [128, D]); y, o = carve(o, "y", [128, D])
        qpT, o = carve(o, "qpT", [128, 16, 128]); sc_sb, o = carve(o, "sc_sb", [128, 16, 128])
        cand, o = carve(o, "cand", [128, 8, 256]); oh, o = carve(o, "oh", [128, 8, 256])
        NG = 4
        gring = []
        for gi in range(NG):
            gv, o = carve(o, "g%d" % gi, [128, D])
            gring.append(gv)
        for gi in range(4):
            gv, _ = carve(16 * K + gi * 8 * K, "g%d" % (NG + gi), [128, D])
            gring.append(gv)
        NG = 8
        gnames = ["g%d" % i for i in range(NG)]
        for wi in range(NW):
            for hf, sfx in ((0, "a"), (1, "b")):
                gring.append(wring[wi][:, hf * 4096:(hf + 1) * 4096].bitcast(F32))
                gnames.append("w%d%s" % (wi, sfx))
        NG = len(gring)
        g_i = [0]

        def gnext():
            i = g_i[0]; g_i[0] = (i + 1) % NG
            return gring[i], gnames[i]

        def V(fn, r, w):
            S.op("dve", fn, r, w)

        def A(fn, r, w):
            S.op("act", fn, r, w)

        def P(fn, r, w):
            S.op("pe", fn, r, w)

        def G(fn, r, w):
            S.op("pool", fn, r, w)

        def ld(out_ap, in_ap, w, queue="sp", r=()):
            S.dma(lambda e: e.dma_start(out=out_ap, in_=in_ap), reads=r, writes=w, queue=queue)

        def bcload(src_row, r=()):
            t, n = bcnext()
            width = src_row.shape[1]
            ld(t[:, 0:width], src_row.partition_broadcast(128)[:, 0, :], [n], r=r)
            return t, n

        def adaload(off):
            return bcload(ada_d[0:1, off:off + D], r=["ada%d" % k for k in range(off // 512, off // 512 + 4)])

        def wload_cast(src, K, N=512):
            t, n = wnext()
            view = t[:, 0:K * N].rearrange("p (k n) -> p k n", k=K)
            ld(view, src.rearrange("(k p) n -> p k n", p=128), n, queue="pool")
            return view, n

        def wload(name, j):
            K_ = WSPEC[name][1]
            t, n = wnext()
            ld(t[:, 0:K_ * 512], wscr[name][j], n, r=wb_res[(name, j)])
            return t[:, 0:K_ * 512].rearrange("p (k n) -> p k n", k=K_), n

        ld(ident[:], ident_d, ["ident"]); ld(freq[:], freq_d, ["freq"]); ld(iota16[:], iota16_d, ["iota16"])
        ld(MT[:], MT_d, ["MT"]); ld(qdecT[:], qdecT_d, ["qdecT"]); ld(kdec[:], kdec_d, ["kdec"])
        ld(kdecP[:], kdecP_d, ["kdecP"]); ld(wsT[:], wsT_d, ["wsT"], queue="pool"); ld(gmbT[:], gm_bT_d, ["gmbT"])
        ld(subkT[:], subkT_d, ["subkT"]); ld(posI[:], posi, ["posI"]); ld(cT[:], cT_d, ["cT"])
        V(lambda e: e.tensor_copy(identb[:], ident[:]), ["ident"], ["identb"])
        V(lambda e: e.tensor_copy(posF[:], posI[:]), ["posI"], ["posF"])
        V(lambda e: e.memset(wsT[64:128, :, 0:64], 0.0), ["wsT"], ["wsT"])
        V(lambda e: e.memset(Sf[:], 0.0), [], ["Sf"])

        wb_res = {}
        EARLY = [("w_in", j) for j in range(6, 12)]
        for name, j in EARLY:
            wsrc_, K_, nch = WSPEC[name]
            view, n = wload_cast(wsrc_[:, j * 512:(j + 1) * 512], K_)
            wb_res[(name, j)] = ["wb_%s_%d" % (name, j)]
            ld(wscr[name][j], view.rearrange("p k n -> p (k n)"), wb_res[(name, j)], r=n)
        late_jobs = []
        for name, (wsrc_, K_, nch) in WSPEC.items():
            for j in range(nch):
                if (name, j) in EARLY:
                    continue
                wb_res[(name, j)] = ["wb_%s_%d_%d" % (name, j, h) for h in range(K_ // 8)]
                for h in range(K_ // 8):
                    late_jobs.append((name, j, h))
        cv_i = [0]

        def emit_late(count):
            for _ in range(count):
                if not late_jobs:
                    return
                name, j, h = late_jobs.pop(0)
                wsrc_ = WSPEC[name][0]
                buf = cvS[cv_i[0] % 2]; bn = "cvS%d" % (cv_i[0] % 2); cv_i[0] += 1
                ld(buf[:], wsrc_[h * 1024:(h + 1) * 1024, j * 512:(j + 1) * 512].rearrange("(k p) n -> p k n", p=128), [bn], queue="pool")
                ld(wscr[name][j][:, h * 4096:(h + 1) * 4096], buf[:].rearrange("p k n -> p (k n)"), ["wb_%s_%d_%d" % (name, j, h)], r=[bn])

        A(lambda e: e.activation(scT[:], cT[:], AF.Silu), ["cT"], ["scT"])
        for n in range(24):
            pt, pn = pnext()
            wv, wn = wload_cast(w_ada[:, n * 512:(n + 1) * 512], 16)
            for kc in range(16):
                P(lambda e, wv=wv, kc=kc, pt=pt: e.matmul(pt[0:1, 0:512], scT[:, kc:kc + 1], wv[:, kc, :],
                                                          start=(kc == 0), stop=(kc == 15)), ["scT"] + wn, [pn])
            ld(brow[:], b_ada[0:1, n * 512:(n + 1) * 512], ["brow"])
            V(lambda e, pt=pt: e.tensor_tensor(arow[:], pt[0:1, 0:512], brow[:], op=ALU.add), [pn, "brow"], ["arow"])
            ld(ada_d[0:1, n * 512:(n + 1) * 512], arow[:], ["ada%d" % n], r=["arow"])

        def load_x_norm(tile_idx, g_row, sc_off, sh_off, out_f32=None):
            ld(xt[:], xs[tile_idx], ["xt"])
            norm_from(xt, "xt", g_row, sc_off, sh_off, out_f32)

        def prefix_norm(tile_idx, par):
            x_, xn_, hb_, hbn_, hT_, hTn_ = (xt, "xt", hb, "hb", hT, "hT") if par == 0 else (xt2, "xt2", hb2, "hb2", hT2, "hT2")
            ld(x_[:], xs[tile_idx], [xn_])
            c0 = 4 * par
            A(lambda e: e.activation(hb_[:], x_[:], AF.Square, accum_out=ss[:, c0:c0 + 1]), [xn_], [hbn_, "ss"])
            V(lambda e: e.tensor_scalar(rstd[:, c0:c0 + 1], ss[:, c0:c0 + 1], 1.0 / D, EPS, op0=ALU.mult, op1=ALU.add), ["ss"], ["rstd"])
            A(lambda e: e.activation(rstd[:, c0 + 1:c0 + 2], rstd[:, c0:c0 + 1], AF.Sqrt), ["rstd"], ["rstd"])
            V(lambda e: e.reciprocal(rstd[:, c0 + 2:c0 + 3], rstd[:, c0 + 1:c0 + 2]), ["rstd"], ["rstd"])
            tmp, tmpn = bcnext()
            V(lambda e: e.scalar_tensor_tensor(tmp[:], x_[:], rstd[:, c0 + 2:c0 + 3], A1p[:], op0=ALU.mult, op1=ALU.mult), [xn_, "rstd", "A1p"], [tmpn])
            V(lambda e: e.tensor_tensor(hb_[:], tmp[:], sh1p[:], op=ALU.add), [tmpn, "sh1p"], [hbn_])
            transpose16(hb_, hbn_, hT_, hTn_)
            return hT_, hTn_

        def norm_from(src, srcn, g_row, sc_off, sh_off, out_f32=None):
            A(lambda e: e.activation(junk[:], src[:], AF.Square, accum_out=ss[:, 0:1]), [srcn], ["junk", "ss"])
            V(lambda e: e.tensor_scalar(rstd[:, 0:1], ss[:, 0:1], 1.0 / D, EPS, op0=ALU.mult, op1=ALU.add), ["ss"], ["rstd"])
            A(lambda e: e.activation(rstd[:, 1:2], rstd[:, 0:1], AF.Sqrt), ["rstd"], ["rstd"])
            V(lambda e: e.reciprocal(rstd[:, 2:3], rstd[:, 1:2]), ["rstd"], ["rstd"])
            gt, gn = bcload(g_row)
            sct, scn = adaload(sc_off)
            V(lambda e: e.scalar_tensor_tensor(sct[:], sct[:], 1.0, gt[:], op0=ALU.add, op1=ALU.mult), [gn, scn], [scn])
            sht, shn = adaload(sh_off)
            V(lambda e: e.scalar_tensor_tensor(sct[:], src[:], rstd[:, 2:3], sct[:], op0=ALU.mult, op1=ALU.mult), [srcn, "rstd", scn], [scn])
            if out_f32 is not None:
                V(lambda e: e.tensor_tensor(out_f32[0][:], sct[:], sht[:], op=ALU.add), [scn, shn], [out_f32[1]])
                A(lambda e: e.copy(hb[:], out_f32[0][:]), [out_f32[1]], ["hb"])
            else:
                V(lambda e: e.tensor_tensor(hb[:], sct[:], sht[:], op=ALU.add), [scn, shn], ["hb"])
            transpose16(hb, "hb", hT, "hT")

        def transpose16(src, srcn, dst, dstn, nchunks=16):
            for half in range((nchunks + 7) // 8):
                pt, pn = pnext()
                pv = pt[:].bitcast(BF16)
                cnt = min(8, nchunks - half * 8)
                for j in range(cnt):
                    c = half * 8 + j
                    P(lambda e, pv=pv, j=j, c=c: e.transpose(pv[:, j * 128:(j + 1) * 128], src[:, c * 128:(c + 1) * 128], identb[:]),
                      [srcn, "identb"], [pn])
                A(lambda e, pv=pv, half=half, cnt=cnt: e.copy(dst[:, half * 8:half * 8 + cnt, :].rearrange("p a b -> p (a b)"), pv[:, 0:cnt * 128]),
                  [pn], [dstn])

        def rope_tables(tile_idx):
            pcol = posF[:, tile_idx:tile_idx + 1]
            PI = float(np.pi)
            for (shift, dsti) in ((0.0, 1), (PI / 2, 0)):
                V(lambda e, shift=shift: e.tensor_scalar(cs[:, 3, :], freq[:], pcol, shift, op0=ALU.mult, op1=ALU.add), ["freq", "posF", "cs"], ["cs3"])
                V(lambda e: e.tensor_scalar(ki[:], cs[:, 3, :], 1.0 / TWO_PI, None, op0=ALU.mult), ["cs3"], ["ki"])
                V(lambda e: e.tensor_copy(kf[:, 0, :], ki[:]), ["ki"], ["kf"])
                V(lambda e: e.scalar_tensor_tensor(cs[:, 3, :], kf[:, 0, :], -TWO_PI, cs[:, 3, :], op0=ALU.mult, op1=ALU.add), ["kf", "cs3"], ["cs3"])
                V(lambda e: e.tensor_scalar(kf[:, 1, :], cs[:, 3, :], PI, -TWO_PI, op0=ALU.is_gt, op1=ALU.mult), ["cs3"], ["kf"])
                V(lambda e: e.tensor_tensor(cs[:, 3, :], cs[:, 3, :], kf[:, 1, :], op=ALU.add), ["kf", "cs3"], ["cs3"])
                V(lambda e: e.tensor_scalar(cs[:, 3, :], cs[:, 3, :], -PI, PI, op0=ALU.max, op1=ALU.min), ["cs3"], ["cs3"])
                A(lambda e, dsti=dsti: e.activation(cs[:, dsti, :], cs[:, 3, :], AF.Sin), ["cs3"], ["cs"])
            V(lambda e: e.tensor_scalar(cs[:, 2, :], cs[:, 1, :], -1.0, None, op0=ALU.mult), ["cs"], ["cs"])

        def rope(pt, pn, nh, dst, dstn):
            pv = pt[:, 0:nh * 128].rearrange("p (h d) -> p h d", h=nh)
            cosb = cs[:, 0, :].unsqueeze(1).to_broadcast([128, nh, 64])
            sinb = cs[:, 1, :].unsqueeze(1).to_broadcast([128, nh, 64])
            nsinb = cs[:, 2, :].unsqueeze(1).to_broadcast([128, nh, 64])
            V(lambda e: e.tensor_tensor(rotA[:, 0:nh, 0:64], pv[:, :, 0:64], cosb, op=ALU.mult), [pn, "cs"], ["rotA"])
            V(lambda e: e.tensor_tensor(rotA[:, 0:nh, 64:128], pv[:, :, 64:128], cosb, op=ALU.mult), [pn, "cs"], ["rotA"])
            V(lambda e: e.tensor_tensor(rotB[:, 0:nh, 0:64], pv[:, :, 64:128], nsinb, op=ALU.mult), [pn, "cs"], ["rotB"])
            V(lambda e: e.tensor_tensor(rotB[:, 0:nh, 64:128], pv[:, :, 0:64], sinb, op=ALU.mult), [pn, "cs"], ["rotB"])
            V(lambda e: e.tensor_tensor(dst[:, 0:nh, :], rotA[:, 0:nh, :], rotB[:, 0:nh, :], op=ALU.add), ["rotA", "rotB"], [dstn])

        def proj(pt, pn, col, wname, c0, K=16, lhs=None, lhsn="hT"):
            lhs = hT if lhs is None else lhs
            wv, wn = wload(wname, c0 // 512)
            for kc in range(K):
                P(lambda e, wv=wv, kc=kc: e.matmul(pt[:, col * 512:(col + 1) * 512], lhs[:, kc, :], wv[:, kc, :],
                                                   start=(kc == 0), stop=(kc == K - 1)), [lhsn] + wn, [pn])

        gt_, gn_ = bcload(norm1_g)
        ld(A1p[:], ada_d[0:1, D:2 * D].partition_broadcast(128)[:, 0, :], ["A1p"], r=["ada%d" % k for k in range(4, 8)])
        V(lambda e: e.scalar_tensor_tensor(A1p[:], A1p[:], 1.0, gt_[:], op0=ALU.add, op1=ALU.mult), ["A1p", gn_], ["A1p"])
        ld(sh1p[:], ada_d[0:1, 0:D].partition_broadcast(128)[:, 0, :], ["sh1p"], r=["ada%d" % k for k in range(0, 4)])
        for hh in range(2):
            wk_v, wk_n = wload("w_in", 6 + hh)
            wv0, wv0n = wload("w_in", 8 + hh * 2)
            wv1, wv1n = wload("w_in", 9 + hh * 2)
            for p in range(NPRE):
                hT_, hTn_ = prefix_norm(p, p % 2)
                rope_tables(p)
                pk, pkn = pnext()
                for kc in range(16):
                    P(lambda e, kc=kc, pk=pk, hT_=hT_: e.matmul(pk[:, 0:512], hT_[:, kc, :], wk_v[:, kc, :], start=(kc == 0), stop=(kc == 15)),
                      [hTn_] + wk_n, [pkn])
                pv_, pvn = pnext()
                for j, (wv, wn) in enumerate(((wv0, wv0n), (wv1, wv1n))):
                    for kc in range(16):
                        P(lambda e, kc=kc, j=j, wv=wv, pv_=pv_, hT_=hT_: e.matmul(pv_[:, j * 512:(j + 1) * 512], hT_[:, kc, :], wv[:, kc, :],
                                                                         start=(kc == 0), stop=(kc == 15)), [hTn_] + wn, [pvn])
                rope(pk, pkn, 4, kb, "kb")
                V(lambda e, p=p, hh=hh: e.tensor_tensor(kd[:, 0:4, :], kb[:, 0:4, :],
                                                        kdecP[:, p, hh * 4:(hh + 1) * 4].unsqueeze(2).to_broadcast([128, 4, 128]), op=ALU.mult),
                  ["kb", "kdecP"], ["kd"])
                A(lambda e, pv_=pv_: e.copy(vr[:, 0:4, :].rearrange("p a b -> p (a b)"), pv_[:, :]), [pvn], ["vr"])
                pst, pstn = pnext()
                for h4 in range(4):
                    P(lambda e, h4=h4, pst=pst: e.matmul(pst[:, h4 * 256:(h4 + 1) * 256], kd[:, h4, :], vr[:, h4, :], start=True, stop=True),
                      ["kd", "vr"], [pstn])
                V(lambda e, hh=hh, pst=pst: e.tensor_tensor(Sf[:, hh * 4:(hh + 1) * 4, :].rearrange("p a b -> p (a b)"),
                                                            Sf[:, hh * 4:(hh + 1) * 4, :].rearrange("p a b -> p (a b)"), pst[:, :], op=ALU.add),
                  [pstn, "Sf"], ["Sf"])
                emit_late(2)
        emit_late(len(late_jobs))
        A(lambda e: e.copy(Sb[:], Sf[:]), ["Sf"], ["Sb"])

        for i in range(NOWN):
            ti = NPRE + i
            load_x_norm(ti, norm1_g, 1 * D, 0)
            rope_tables(ti)
            pu, pun = pnext(); proj(pu, pun, 0, "w_in", 0); proj(pu, pun, 1, "w_in", 512)
            A(lambda e, pu=pu: e.activation(u_sb[:].rearrange("p a b -> p (a b)"), pu[:, :], AF.Gelu), [pun], ["u_sb"])
            pvv, pvvn = pnext(); proj(pvv, pvvn, 0, "w_in", 1024); proj(pvv, pvvn, 1, "w_in", 1536)
            A(lambda e, pvv=pvv: e.activation(v_f[:].rearrange("p a b -> p (a b)"), pvv[:, :], AF.Gelu), [pvvn], ["v_f"])
            V(lambda e: e.tensor_tensor(rotA[:], v_f[:], v_f[:], op=ALU.mult), ["v_f"], ["rotA"])
            V(lambda e: e.tensor_reduce(ss[:, 0:8], rotA[:], axis=AX.X, op=ALU.add), ["rotA"], ["ss"])
            V(lambda e: e.tensor_scalar(rstd[:, 0:8], ss[:, 0:8], 1.0 / 128, EPS, op0=ALU.mult, op1=ALU.add), ["ss"], ["rstd"])
            A(lambda e: e.activation(ss[:, 0:8], rstd[:, 0:8], AF.Sqrt), ["rstd"], ["ss"])
            V(lambda e: e.reciprocal(rstd[:, 0:8], ss[:, 0:8]), ["ss"], ["rstd"])
            vg, vgn = bcload(gm_v_g)
            V(lambda e: e.tensor_tensor(rotA[:], v_f[:], rstd[:, 0:8].unsqueeze(2).to_broadcast([128, 8, 128]), op=ALU.mult), ["v_f", "rstd"], ["rotA"])
            V(lambda e, vg=vg: e.tensor_tensor(v_b[:].rearrange("p a b -> p (a b)"), rotA[:].rearrange("p a b -> p (a b)"), vg[:, 0:1024], op=ALU.mult),
              ["rotA", vgn], ["v_b"])
            psg, psgn = pnext()
            for g in range(8):
                P(lambda e, g=g, psg=psg: e.matmul(psg[:, g * 128:(g + 1) * 128], wsT[:, g, :], v_b[:, g, :], start=True, stop=True),
                  ["wsT", "v_b"], [psgn])
            V(lambda e, psg=psg: e.tensor_tensor(rotA[:], psg[:, :].rearrange("p (a b) -> p a b", a=8),
                                                 gmbT[:, :].unsqueeze(2).to_broadcast([128, 8, 128]), op=ALU.add), [psgn, "gmbT"], ["rotA"])
            V(lambda e: e.tensor_tensor(preA[:], rotA[:], u_sb[:], op=ALU.mult), ["rotA", "u_sb"], ["preA"])
            transpose16(preA[:].rearrange("p a b -> p (a b)"), "preA", preAT, "preAT", nchunks=8)
            pq, pqn = pnext(); proj(pq, pqn, 0, "w_in", 2048); proj(pq, pqn, 1, "w_in", 2560)
            rope(pq, pqn, 8, qb, "qb")
            pk, pkn = pnext(); proj(pk, pkn, 0, "w_in", 3072); proj(pk, pkn, 1, "w_in", 3584)
            rope(pk, pkn, 8, kb, "kb")
            V(lambda e: e.tensor_tensor(kd[:], kb[:], kdec[:, :].unsqueeze(2).to_broadcast([128, 8, 128]), op=ALU.mult), ["kb", "kdec"], ["kd"])
            pt, pn = pnext(); pv = pt[:].bitcast(BF16)
            for h in range(8):
                P(lambda e, h=h, pv=pv: e.transpose(pv[:, h * 128:(h + 1) * 128], qb[:, h, :], identb[:]), ["qb", "identb"], [pn])
            A(lambda e, pv=pv: e.copy(qT[:].rearrange("p a b -> p (a b)"), pv[:, 0:1024]), [pn], ["qT"])
            V(lambda e, pv=pv: e.tensor_tensor(qdT[:].rearrange("p a b -> p (a b)"), pv[:, 0:1024], qdecT[:].rearrange("p a b -> p (a b)"), op=ALU.mult),
              [pn, "qdecT"], ["qdT"])
            pt, pn = pnext(); pv = pt[:].bitcast(BF16)
            for h in range(8):
                P(lambda e, h=h, pv=pv: e.transpose(pv[:, h * 128:(h + 1) * 128], kb[:, h, :], identb[:]), ["kb", "identb"], [pn])
            A(lambda e, pv=pv: e.copy(kT[:].rearrange("p a b -> p (a b)"), pv[:, 0:1024]), [pn], ["kT"])
            for j in range(2):
                pvr, pvrn = pnext(); proj(pvr, pvrn, 0, "w_in", 4096 + j * 1024); proj(pvr, pvrn, 1, "w_in", 4096 + j * 1024 + 512)
                A(lambda e, j=j, pvr=pvr: e.copy(vr[:, j * 4:(j + 1) * 4, :].rearrange("p a b -> p (a b)"), pvr[:, :]), [pvrn], ["vr"])
            for j in range(2):
                pg, pgn = pnext(); proj(pg, pgn, 0, "w_in", 6144 + j * 1024); proj(pg, pgn, 1, "w_in", 6144 + j * 1024 + 512)
                A(lambda e, j=j, pg=pg: e.activation(sg[:, j * 1024:(j + 1) * 1024], pg[:, :], AF.Silu), [pgn], ["sg"])
            pat, patn = pnext()
            for h in range(8):
                P(lambda e, h=h, pat=pat: e.matmul(pat[:, h * 128:(h + 1) * 128], kT[:, h, :], qT[:, h, :], start=True, stop=True),
                  ["kT", "qT"], [patn])
            V(lambda e, pat=pat: e.tensor_tensor(attm[:].rearrange("p a b -> p (a b)"), pat[:, :], MT[:].rearrange("p a b -> p (a b)"), op=ALU.mult),
              [patn, "MT"], ["attm"])
            for j in range(2):
                po, pon = pnext()
                for h4 in range(4):
                    h = j * 4 + h4
                    P(lambda e, h=h, h4=h4, po=po: e.matmul(po[:, h4 * 256:(h4 + 1) * 256], attm[:, h, :], vr[:, h, :], start=True, stop=False),
                      ["attm", "vr"], [pon])
                    P(lambda e, h=h, h4=h4, po=po: e.matmul(po[:, h4 * 256:(h4 + 1) * 256], qdT[:, h, :], Sb[:, h, :], start=False, stop=True),
                      ["qdT", "Sb"], [pon])
                A(lambda e, j=j, po=po: e.copy(o_sb[:, j * 4:(j + 1) * 4, :].rearrange("p a b -> p (a b)"), po[:, :]), [pon], ["o_sb"])
            for j in range(2):
                pst, pstn = pnext()
                for h4 in range(4):
                    h = j * 4 + h4
                    P(lambda e, h=h, h4=h4, pst=pst: e.matmul(pst[:, h4 * 256:(h4 + 1) * 256], kd[:, h, :], vr[:, h, :], start=True, stop=True),
                      ["kd", "vr"], [pstn])
                for h4 in range(4):
                    h = j * 4 + h4
                    V(lambda e, h=h, h4=h4, pst=pst: e.scalar_tensor_tensor(Sf[:, h, :], Sf[:, h, :], float(GAM[h] ** 128), pst[:, h4 * 256:(h4 + 1) * 256],
                                                                            op0=ALU.mult, op1=ALU.add), [pstn, "Sf"], ["Sf"])
            A(lambda e: e.copy(Sb[:], Sf[:]), ["Sf"], ["Sb"])
            V(lambda e: e.tensor_reduce(st8[:, 0, :], o_sb[:], axis=AX.X, op=ALU.add), ["o_sb"], ["st8"])
            V(lambda e: e.tensor_tensor(o2[:], o_sb[:], o_sb[:], op=ALU.mult), ["o_sb"], ["o2"])
            V(lambda e: e.tensor_reduce(st8[:, 1, :], o2[:], axis=AX.X, op=ALU.add), ["o2"], ["st8"])
            V(lambda e: e.tensor_scalar(st8[:, 0, :], st8[:, 0, :], 1.0 / 256, None, op0=ALU.mult), ["st8"], ["st8"])
            V(lambda e: e.tensor_tensor(st8[:, 2, :], st8[:, 0, :], st8[:, 0, :], op=ALU.mult), ["st8"], ["st8"])
            V(lambda e: e.scalar_tensor_tensor(st8[:, 1, :], st8[:, 1, :], 1.0 / 256, st8[:, 2, :], op0=ALU.mult, op1=ALU.subtract), ["st8"], ["st8"])
            V(lambda e: e.tensor_scalar(st8[:, 1, :], st8[:, 1, :], EPS, None, op0=ALU.add), ["st8"], ["st8"])
            A(lambda e: e.activation(st8[:, 2, :], st8[:, 1, :], AF.Sqrt), ["st8"], ["st8"])
            V(lambda e: e.reciprocal(st8[:, 3, :], st8[:, 2, :]), ["st8"], ["st8"])
            V(lambda e: e.tensor_tensor(o2[:], o_sb[:], st8[:, 0, :].unsqueeze(2).to_broadcast([128, 8, 256]), op=ALU.subtract), ["o_sb", "st8"], ["o2"])
            V(lambda e: e.tensor_tensor(o2[:], o2[:], st8[:, 3, :].unsqueeze(2).to_broadcast([128, 8, 256]), op=ALU.mult), ["o2", "st8"], ["o2"])
            gnb, gnn = bcload(ret_gn_g)
            V(lambda e, gnb=gnb: e.tensor_tensor(o2[:].rearrange("p a b -> p (a b)"), o2[:].rearrange("p a b -> p (a b)"), gnb[:], op=ALU.mult), ["o2", gnn], ["o2"])
            V(lambda e: e.tensor_tensor(retb[:], o2[:].rearrange("p a b -> p (a b)"), sg[:], op=ALU.mult), ["o2", "sg"], ["retb"])
            transpose16(retb, "retb", retT, "retT")
            for n in range(4):
                pga, pgan = pnext()
                proj(pga, pgan, 0, "w_bg", n * 512); proj(pga, pgan, 1, "w_bg", 2048 + n * 512)
                bb, bbn = bcload(b_bg[0:1, n * 512:(n + 1) * 512])
                bb2, bb2n = bcload(b_bg[0:1, 2048 + n * 512:2048 + (n + 1) * 512])
                V(lambda e, pga=pga, bb=bb: e.tensor_tensor(gA[:], pga[:, 0:512], bb[:, 0:512], op=ALU.add), [pgan, bbn], ["gA"])
                V(lambda e, pga=pga, bb2=bb2: e.tensor_tensor(gB[:], pga[:, 512:1024], bb2[:, 0:512], op=ALU.add), [pgan, bb2n], ["gB"])
                A(lambda e: e.activation(gA[:], gA[:], AF.Sigmoid), ["gA"], ["gA"])
                A(lambda e: e.activation(gB[:], gB[:], AF.Sigmoid), ["gB"], ["gB"])
                pyy, pyyn = pnext()
                proj(pyy, pyyn, 0, "w_a", n * 512, K=8, lhs=preAT, lhsn="preAT")
                proj(pyy, pyyn, 1, "w_b", n * 512, K=16, lhs=retT, lhsn="retT")
                V(lambda e, pyy=pyy: e.tensor_tensor(gA[:], gA[:], pyy[:, 0:512], op=ALU.mult), ["gA", pyyn], ["gA"])
                V(lambda e, pyy=pyy: e.tensor_tensor(gB[:], gB[:], pyy[:, 512:1024], op=ALU.mult), ["gB", pyyn], ["gB"])
                V(lambda e, n=n: e.tensor_tensor(mb[:, n * 512:(n + 1) * 512], gA[:], gB[:], op=ALU.add), ["gA", "gB"], ["mb"])
            transpose16(mb, "mb", mT, "mT")
            ga1, ga1n = adaload(2 * D)
            for j in range(2):
                pm, pmn = pnext()
                proj(pm, pmn, 0, "w_o", j * 1024, lhs=mT, lhsn="mT"); proj(pm, pmn, 1, "w_o", j * 1024 + 512, lhs=mT, lhsn="mT")
                V(lambda e, j=j, pm=pm, ga1=ga1: e.tensor_tensor(ga1[:, j * 1024:(j + 1) * 1024], ga1[:, j * 1024:(j + 1) * 1024], pm[:, :], op=ALU.mult),
                  [pmn, ga1n], [ga1n])
            V(lambda e, ga1=ga1: e.tensor_tensor(xt[:], xt[:], ga1[:], op=ALU.add), ["xt", ga1n], ["xt"])
            if dbg:
                ld(dbg_x1[i], xt[:], ["dbgx1_%d" % i], r=["xt"])

            norm_from(xt, "xt", norm2_g, 4 * D, 3 * D, out_f32=(h2, "h2"))
            for half in range(2):
                pq2, pq2n = pnext()
                for cc in range(2):
                    wv, wn = wload("wq", half * 2 + cc)
                    for c4 in range(4):
                        j = cc * 4 + c4
                        for kc in range(16):
                            P(lambda e, wv=wv, kc=kc, c4=c4, j=j, pq2=pq2: e.matmul(pq2[:, j * 128:(j + 1) * 128], wv[:, kc, c4 * 128:(c4 + 1) * 128], hT[:, kc, :],
                                                                                   start=(kc == 0), stop=(kc == 15)), ["hT"] + wn, [pq2n])
                A(lambda e, half=half, pq2=pq2: e.copy(qpT[:, half * 8:(half + 1) * 8, :].rearrange("p a b -> p (a b)"), pq2[:, :]), [pq2n], ["qpT"])
            for half in range(2):
                psc, pscn = pnext()
                for j in range(8):
                    c = half * 8 + j
                    P(lambda e, c=c, j=j, psc=psc: e.matmul(psc[:, j * 128:(j + 1) * 128], qpT[:, c, :], subkT[:, c, :], start=True, stop=True),
                      ["qpT", "subkT"], [pscn])
                A(lambda e, half=half, psc=psc: e.copy(sc_sb[:, half * 8:(half + 1) * 8, :].rearrange("p a b -> p (a b)"), psc[:, :]), [pscn], ["sc_sb"])
            for c in range(16):
                V(lambda e, c=c: e.max(out=v12[:, c, 0:8], in_=sc_sb[:, c, :]), ["sc_sb"], ["v12"])
                V(lambda e, c=c: e.max_index(out=i12[:, c, 0:8], in_max=v12[:, c, 0:8], in_values=sc_sb[:, c, :]), ["sc_sb", "v12"], ["i12"])
                V(lambda e, c=c: e.match_replace(out=wk[:, 0:128], in_to_replace=v12[:, c, 0:8], in_values=sc_sb[:, c, :], imm_value=-1e30), ["sc_sb", "v12"], ["wk"])
                V(lambda e, c=c: e.max(out=v12[:, c, 8:16], in_=wk[:, 0:128]), ["wk"], ["v12"])
                V(lambda e, c=c: e.max_index(out=i12[:, c, 8:16], in_max=v12[:, c, 8:16], in_values=wk[:, 0:128]), ["wk", "v12"], ["i12"])
            V(lambda e: e.tensor_copy(i12f[:], i12[:]), ["i12"], ["i12f"])
            v12v = v12[:].rearrange("p (h two) k -> p h two k", two=2)
            i12v = i12f[:].rearrange("p (h two) k -> p h two k", two=2)
            for h in range(8):
                V(lambda e, h=h: e.tensor_tensor(cand[:, h, :].rearrange("p (a b) -> p a b", a=16),
                                                 v12v[:, h, 0, :].unsqueeze(2).to_broadcast([128, 16, 16]),
                                                 v12v[:, h, 1, :].unsqueeze(1).to_broadcast([128, 16, 16]), op=ALU.add), ["v12"], ["cand"])
            for h in range(8):
                V(lambda e, h=h: e.max(out=top[:, h, 0:8], in_=cand[:, h, :]), ["cand"], ["top"])
                V(lambda e, h=h: e.max_index(out=pos[:, h, 0:8], in_max=top[:, h, 0:8], in_values=cand[:, h, :]), ["cand", "top"], ["pos"])
                V(lambda e, h=h: e.match_replace(out=wk[:], in_to_replace=top[:, h, 0:8], in_values=cand[:, h, :], imm_value=-1e30), ["cand", "top"], ["wk"])
                V(lambda e, h=h: e.max(out=top[:, h, 8:16], in_=wk[:]), ["wk"], ["top"])
                V(lambda e, h=h: e.max_index(out=pos[:, h, 8:16], in_max=top[:, h, 8:16], in_values=wk[:]), ["wk", "top"], ["pos"])
            V(lambda e: e.tensor_copy(pcor[:], pos[:]), ["pos"], ["pcor"])
            V(lambda e: e.tensor_scalar(pi32[:], pcor[:], 0.0625, None, op0=ALU.mult), ["pcor"], ["pi32"])
            V(lambda e: e.tensor_copy(paf[:], pi32[:]), ["pi32"], ["paf"])
            V(lambda e: e.scalar_tensor_tensor(pbf[:].rearrange("p h k -> p (h k)"), paf[:].rearrange("p h k -> p (h k)"), -16.0,
                                               pcor[:].rearrange("p h k -> p (h k)"), op0=ALU.mult, op1=ALU.add), ["paf", "pcor"], ["pbf"])
            V(lambda e: e.tensor_scalar(pcor[:], pbf[:], 0.0, None, op0=ALU.is_lt), ["pbf"], ["pcor"])
            V(lambda e: e.tensor_tensor(paf[:], paf[:], pcor[:], op=ALU.subtract), ["paf", "pcor"], ["paf"])
            V(lambda e: e.scalar_tensor_tensor(pbf[:].rearrange("p h k -> p (h k)"), pcor[:].rearrange("p h k -> p (h k)"), 16.0,
                                               pbf[:].rearrange("p h k -> p (h k)"), op0=ALU.mult, op1=ALU.add), ["pcor", "pbf"], ["pbf"])
            for (pf, pfn, two, dst, dstn) in ((paf, "paf", 0, i1s, "i1s"), (pbf, "pbf", 1, i2s, "i2s")):
                for h in range(8):
                    ohv = oh[:, h, :].rearrange("p (k a) -> p k a", k=16)
                    V(lambda e, h=h, pf=pf, ohv=ohv: e.tensor_tensor(ohv, iota16[:, :].unsqueeze(1).to_broadcast([128, 16, 16]),
                                                                     pf[:, h, :].unsqueeze(2).to_broadcast([128, 16, 16]), op=ALU.is_equal),
                      ["iota16", pfn], ["oh"])
                    V(lambda e, h=h, two=two, ohv=ohv: e.tensor_tensor(ohv, ohv, i12v[:, h, two, :].unsqueeze(1).to_broadcast([128, 16, 16]), op=ALU.mult),
                      ["oh", "i12f"], ["oh"])
                V(lambda e, dst=dst: e.tensor_reduce(dst[:].rearrange("p h k -> p (h k)"), oh[:].rearrange("p h (k a) -> p (h k) a", a=16), axis=AX.X, op=ALU.add),
                  ["oh"], [dstn])
            V(lambda e: e.scalar_tensor_tensor(i1s[:], i1s[:], 128.0, i2s[:], op0=ALU.mult, op1=ALU.add), ["i1s", "i2s"], ["i1s"])
            V(lambda e: e.tensor_copy(eidx[:], i1s[:].rearrange("p h k -> p (h k)")), ["i1s"], ["eidx"])
            V(lambda e: e.tensor_tensor(gate[:], top[:], top[:, :, 0:1].to_broadcast([128, 8, 16]), op=ALU.subtract), ["top"], ["gate"])
            A(lambda e: e.activation(gate[:], gate[:], AF.Exp), ["gate"], ["gate"])
            V(lambda e: e.tensor_reduce(gsum[:], gate[:], axis=AX.X, op=ALU.add), ["gate"], ["gsum"])
            V(lambda e: e.reciprocal(gsum[:], gsum[:]), ["gsum"], ["gsum"])
            V(lambda e: e.tensor_tensor(gate[:], gate[:], gsum[:, :].unsqueeze(2).to_broadcast([128, 8, 16]), op=ALU.mult), ["gate", "gsum"], ["gate"])
            for hk in range(128):
                gt, gn = gnext()
                S.dma(lambda e, gt=gt, hk=hk: e.indirect_dma_start(out=gt[:], out_offset=None, in_=peer_u,
                                                                  in_offset=bass.IndirectOffsetOnAxis(ap=eidx[:, hk:hk + 1], axis=0)),
                      reads=["eidx"], writes=[gn], queue="pool")
                V(lambda e, gt=gt, hk=hk: e.scalar_tensor_tensor(junk[:], gt[:], 1.0, h2[:], op0=ALU.mult, op1=ALU.mult, accum_out=acol[:, hk:hk + 1]),
                  [gn, "h2"], ["junk", "acol"])
            A(lambda e: e.activation(wgt[:], acol[:], AF.Gelu), ["acol"], ["wgt"])
            V(lambda e: e.tensor_tensor(wgt[:], wgt[:], gate[:].rearrange("p h k -> p (h k)"), op=ALU.mult), ["wgt", "gate"], ["wgt"])
            for hk in range(128):
                gt, gn = gnext()
                S.dma(lambda e, gt=gt, hk=hk: e.indirect_dma_start(out=gt[:], out_offset=None, in_=peer_v,
                                                                  in_offset=bass.IndirectOffsetOnAxis(ap=eidx[:, hk:hk + 1], axis=0)),
                      reads=["eidx"], writes=[gn], queue="pool")
                if hk == 0:
                    V(lambda e, gt=gt: e.tensor_scalar(y[:], gt[:], wgt[:, 0:1], None, op0=ALU.mult), [gn, "wgt"], ["y"])
                else:
                    V(lambda e, gt=gt, hk=hk: e.scalar_tensor_tensor(y[:], gt[:], wgt[:, hk:hk + 1], y[:], op0=ALU.mult, op1=ALU.add), [gn, "wgt", "y"], ["y"])
            if dbg:
                ld(dbg_y[i], y[:], ["dbgy_%d" % i], r=["y"])
            ga2, ga2n = adaload(5 * D)
            V(lambda e, ga2=ga2: e.tensor_tensor(y[:], y[:], ga2[:], op=ALU.mult), ["y", ga2n], ["y"])
            V(lambda e: e.tensor_tensor(xt[:], xt[:], y[:], op=ALU.add), ["xt", "y"], ["xt"])
            A(lambda e: e.activation(junk[:], xt[:], AF.Square, accum_out=ss[:, 0:1]), ["xt"], ["junk", "ss"])
            V(lambda e: e.tensor_scalar(rstd[:, 0:1], ss[:, 0:1], 1.0 / D, EPS, op0=ALU.mult, op1=ALU.add), ["ss"], ["rstd"])
            A(lambda e: e.activation(rstd[:, 1:2], rstd[:, 0:1], AF.Sqrt), ["rstd"], ["rstd"])
            V(lambda e: e.reciprocal(rstd[:, 2:3], rstd[:, 1:2]), ["rstd"], ["rstd"])
            gf, gfn = bcload(norm_f_g)
            V(lambda e, gf=gf: e.scalar_tensor_tensor(y[:], xt[:], rstd[:, 2:3], gf[:], op0=ALU.mult, op1=ALU.mult), ["xt", "rstd", gfn], ["y"])
            ld(out_d[i], y[:], ["out%d" % i], r=["y"])
        fin = ["out%d" % i for i in range(NOWN)] + ([n % i for i in range(NOWN) for n in ("dbgx1_%d", "dbgy_%d")] if dbg else [])
        S.final_wait("sp", fin)
        S.emit()
    return nc


def _consts(seg):
    gam = np.array([1.0 - 2.0 ** (-5.0 - h) for h in range(H)], dtype=np.float64)
    scale = 128.0 ** -0.5
    i = np.arange(128)
    MT = np.zeros((128, H, 128), dtype=np.float64)
    ci, cj = i[None, :] // 64, i[:, None] // 64
    dist = np.abs(i[None, :] - i[:, None]).astype(np.float64)
    allowed = (ci >= cj)
    for h in range(H):
        MT[:, h, :] = np.where(allowed, gam[h] ** dist, 0.0) * scale
    qdecT = np.broadcast_to((gam[None, :, None] ** (i[None, None, :] + 1.0)), (128, H, 128))
    kdec = (gam[None, :] ** (127.0 - i[:, None])) * scale
    kdecP = np.zeros((128, NPRE, H), dtype=np.float64)
    P0 = seg * 1024
    for p in range(NPRE):
        gt = seg * 8 - NPRE + p
        if gt < 0:
            continue
        tok = gt * 128 + i
        kdecP[:, p, :] = (gam[None, :] ** (P0 - 1.0 - tok[:, None])) * scale
    f32 = lambda a: np.ascontiguousarray(a, dtype=np.float32)
    return f32(MT), f32(qdecT), f32(kdec), f32(kdecP)


def _in_maps(inputs):
    x = np.asarray(inputs["x"], dtype=np.float32)
    c = np.asarray(inputs["c"], dtype=np.float32)
    positions = np.asarray(inputs["positions"]).astype(np.int32)
    g = lambda k: np.asarray(inputs[k], dtype=np.float32)
    shared = {
        "w_ada": np.ascontiguousarray(g("w_ada")[0]),
        "b_ada": np.ascontiguousarray(g("b_ada")[0][None, :]),
        "norm1_g": np.ascontiguousarray(g("norm1_g")[0][None, :]),
        "w_in": np.ascontiguousarray(g("w_in")[0]),
        "w_bg": np.ascontiguousarray(g("w_branch_gate")[0]),
        "b_bg": np.ascontiguousarray(g("b_branch_gate")[0][None, :]),
        "gm_v_g": np.ascontiguousarray(g("gm_v_g")[0][None, :]),
        "wsT": np.ascontiguousarray(g("gm_ws")[0].transpose(2, 0, 1)),
        "gm_bT": np.ascontiguousarray(g("gm_b")[0].T),
        "ret_gn_g": np.ascontiguousarray(g("ret_gn_g")[0][None, :]),
        "w_a": np.ascontiguousarray(g("w_a_out")[0]),
        "w_b": np.ascontiguousarray(g("w_b_out")[0]),
        "w_o": np.ascontiguousarray(g("w_o")[0]),
        "norm2_g": np.ascontiguousarray(g("norm2_g")[0][None, :]),
        "wq": np.ascontiguousarray(g("peer_wq")[0]),
        "subkT": np.ascontiguousarray(g("peer_subkeys")[0].reshape(16, 128, 128).transpose(2, 0, 1)),
        "peer_u": np.ascontiguousarray(g("peer_u")[0]),
        "peer_v": np.ascontiguousarray(g("peer_v")[0]),
        "norm_f_g": np.ascontiguousarray(g("norm_f_g")[None, :]),
        "ident": np.eye(128, dtype=np.float32),
        "freq": np.ascontiguousarray(np.broadcast_to((10000.0 ** (-np.arange(64, dtype=np.float32) / 64)).astype(np.float32)[None, :], (128, 64))),
        "iota16": np.ascontiguousarray(np.broadcast_to(np.arange(16, dtype=np.float32)[None, :], (128, 16))),
    }
    maps = []
    for core in range(8):
        b, seg = core // 4, core % 4
        xsl = np.zeros((NPRE + NOWN, 128, D), dtype=np.float32)
        pos = np.zeros((128, NPRE + NOWN), dtype=np.int32)
        for p in range(NPRE + NOWN):
            gt = seg * 8 - NPRE + p
            if gt < 0:
                continue
            xsl[p] = x[b, gt * 128:(gt + 1) * 128]
            pos[:, p] = positions[b, gt * 128:(gt + 1) * 128]
        MT, qdecT, kdec, kdecP = _consts(seg)
        m = dict(shared)
        m.update({"xs": xsl, "posi": pos, "cT": np.ascontiguousarray(c[b].reshape(16, 128).T),
                  "MT": MT, "qdecT": qdecT, "kdec": kdec, "kdecP": kdecP})
        maps.append(m)
    return maps


_DBG = bool(int(os.environ.get("KDBG", "0")))
_last = {}


def kernel(**inputs):
    nc = build_program(dbg=_DBG)
    maps = _in_maps(inputs)
    res = run_bass_kernel_spmd(nc, maps, core_ids=list(range(8)))
    out = np.zeros((2, 4096, D), dtype=np.float32)
    for core in range(8):
        b, seg = core // 4, core % 4
        out[b, seg * 1024:(seg + 1) * 1024] = res.results[core]["out"].reshape(1024, D)
    if _DBG:
        _last["res"] = res.results
    return out
```
